# Optimizing a Trainium2 kernel written in Bass

```python
import math
import jax, jax.numpy as jnp
from jax import lax
import numpy as np

D_MODEL = 1024
BATCH = 2
SEQ = 8192
DEPTH = 2

GRID_W = 64
CTX_LEN = 256
EPS = 1e-6

D_LRU = 384
LRU_BLOCKS = 6
LRU_BLOCK_DIM = D_LRU // LRU_BLOCKS
LRU_CONV = 4
LRU_C = 8.0

MLA_HEADS = 6
MLA_NOPE = 64
MLA_ROPE = 32
MLA_V = 64
MLA_Q_RANK = 384
MLA_KV_RANK = 256
MLA_SCALE = (MLA_NOPE + MLA_ROPE) ** -0.5
ROPE_PAIRS = MLA_ROPE // 4
ROPE_THETA = 10000.0
ATTN_BLOCK = 128

D_HY = 256
HY_CONV = 3
HY_ORDER = 2
HY_BANDS = 8
HY_EMB = 1 + 2 * HY_BANDS
HY_FFN = 64

D_MIX = D_LRU + MLA_HEADS * MLA_V + D_HY
D_IN = 2 * D_LRU + MLA_Q_RANK + MLA_KV_RANK + MLA_ROPE + 3 * D_HY
IN_SPLITS = [D_LRU, 2 * D_LRU, 2 * D_LRU + MLA_Q_RANK,
             2 * D_LRU + MLA_Q_RANK + MLA_KV_RANK,
             2 * D_LRU + MLA_Q_RANK + MLA_KV_RANK + MLA_ROPE]

PEER_HEADS = 8
PEER_NKEYS = 128
PEER_EXPERTS = PEER_NKEYS * PEER_NKEYS
PEER_DQ = 256
PEER_TOPK = 16
PEER_BLOCK = 128

kernel_name = "hybrid_rglru_mla_hyena_peer_prefix_dit"

F32 = jnp.float32


def rmsnorm(x, g):
    xf = x.astype(F32)
    y = xf * lax.rsqrt(jnp.mean(xf * xf, axis=-1, keepdims=True) + EPS)
    return (y * g.astype(F32)).astype(x.dtype)


def depthwise_conv(x, w, b, pad_l, pad_r):
    y = lax.conv_general_dilated(
        x, w[:, None, :].astype(x.dtype), window_strides=(1,),
        padding=[(pad_l, pad_r)], dimension_numbers=('NWC', 'WIO', 'NWC'),
        feature_group_count=x.shape[-1])
    return y + b.astype(x.dtype)


def linear_scan(a, b, h0):
    b = b.at[:, 0].add(a[:, 0] * h0)

    def combine(e1, e2):
        a1, b1 = e1
        a2, b2 = e2
        return a1 * a2, a2 * b1 + b2

    _, h = lax.associative_scan(combine, (a, b), axis=1)
    return h


def rglru_coeffs(x, wr, br, wi, bi, lam):
    xb = x.reshape(x.shape[:-1] + (LRU_BLOCKS, LRU_BLOCK_DIM))
    r = jax.nn.sigmoid(jnp.einsum('blnd,nde->blne', xb, wr.astype(F32)).reshape(x.shape) + br.astype(F32))
    i = jax.nn.sigmoid(jnp.einsum('blnd,nde->blne', xb, wi.astype(F32)).reshape(x.shape) + bi.astype(F32))
    log_a = -LRU_C * r * jax.nn.softplus(-lam.astype(F32))
    a = jnp.exp(log_a)
    b = jnp.sqrt(-jnp.expm1(2.0 * log_a)) * (i * x)
    return a, b


def rglru_bidir(xx, xc, wr, br, wi, bi, lam):
    xx32, xc32 = xx.astype(F32), xc.astype(F32)
    h0 = jnp.zeros((xx.shape[0], D_LRU), F32)
    ys_x, ys_c = [], []
    for d in range(2):
        flip = (lambda t: t[:, ::-1]) if d == 1 else (lambda t: t)
        ac, bc = rglru_coeffs(flip(xc32), wr[d], br[d], wi[d], bi[d], lam[d])
        ax, bx = rglru_coeffs(flip(xx32), wr[d], br[d], wi[d], bi[d], lam[d])
        hc = linear_scan(ac, bc, h0)
        hx = linear_scan(ax, bx, hc[:, -1])
        ys_c.append(flip(hc))
        ys_x.append(flip(hx))
    return (ys_x[0] + ys_x[1]).astype(xx.dtype), (ys_c[0] + ys_c[1]).astype(xc.dtype)


def rope_axis(x, ang):
    n = x.shape[-1] // 2
    x1, x2 = x[..., :n], x[..., n:]
    cos, sin = jnp.cos(ang).astype(x.dtype), jnp.sin(ang).astype(x.dtype)
    return jnp.concatenate([x1 * cos - x2 * sin, x2 * cos + x1 * sin], axis=-1)


def rope_2d(x, ang_row, ang_col):
    h = MLA_ROPE // 2
    return jnp.concatenate([rope_axis(x[..., :h], ang_row), rope_axis(x[..., h:], ang_col)], axis=-1)


def mla_project(cq, ckv, kr, q_g, wqb, kv_g, wkvb, angles):
    B, L = cq.shape[:2]
    q = (rmsnorm(cq, q_g) @ wqb).reshape(B, L, MLA_HEADS, MLA_NOPE + MLA_ROPE)
    kv = (rmsnorm(ckv, kv_g) @ wkvb).reshape(B, L, MLA_HEADS, MLA_NOPE + MLA_V)
    q_nope, q_rope = q[..., :MLA_NOPE], q[..., MLA_NOPE:]
    k_nope, v = kv[..., :MLA_NOPE], kv[..., MLA_NOPE:]
    if angles is not None:
        ang_r, ang_c = angles
        q_rope = rope_2d(q_rope, ang_r[:, None, :], ang_c[:, None, :])
        kr = rope_2d(kr, ang_r, ang_c)
    k_rope = jnp.broadcast_to(kr[:, :, None, :], (B, L, MLA_HEADS, MLA_ROPE))
    q = jnp.concatenate([q_nope, q_rope], axis=-1)
    k = jnp.concatenate([k_nope, k_rope], axis=-1)
    return q, k, v


def attend(q, k, v):
    s = jnp.einsum('bqhd,bkhd->bhqk', q, k, preferred_element_type=F32) * MLA_SCALE
    p = jax.nn.softmax(s, axis=-1).astype(v.dtype)
    return jnp.einsum('bhqk,bkhd->bqhd', p, v)


def hyena_filter_spectra(L, w1, b1, w2, b2, w3, freq, decay):
    t = jnp.arange(L, dtype=F32) / L
    bands = jnp.linspace(1e-4, HY_BANDS - 1, HY_BANDS, dtype=F32)
    wpos = (2.0 * math.pi) * t[:, None] * bands[None, :]
    z = jnp.concatenate([t[:, None], jnp.cos(wpos), jnp.sin(wpos)], axis=-1)
    fr = freq.astype(F32)
    h = jnp.sin(fr * (z @ w1.astype(F32) + b1.astype(F32)))
    h = jnp.sin(fr * (h @ w2.astype(F32) + b2.astype(F32)))
    k = (h @ w3.astype(F32)).reshape(L, 2 * HY_ORDER, D_HY) * jnp.exp(-t[:, None, None] * decay.astype(F32))
    k = k * lax.rsqrt(jnp.sum(k * k, axis=0, keepdims=True) + EPS)
    k = k.reshape(L, HY_ORDER, 2, D_HY)
    kf, kb = k[:, :, 0], k[:, :, 1]
    k_circ = jnp.concatenate([kf, jnp.zeros((1, HY_ORDER, D_HY), F32), kb[:0:-1]], axis=0)
    return jnp.fft.rfft(k_circ, axis=0)


def fft_conv(z, kspec, skip):
    L = z.shape[1]
    z32 = z.astype(F32)
    zf = jnp.fft.rfft(z32, n=2 * L, axis=1)
    y = jnp.fft.irfft(zf * kspec[None], n=2 * L, axis=1)[:, :L]
    return (y + z32 * skip.astype(F32)).astype(z.dtype)


def hyena(u, conv_w, conv_b, w1, b1, w2, b2, w3, freq, decay, skip):
    L = u.shape[1]
    u = depthwise_conv(u, conv_w, conv_b, 1, 1)
    v, x1, x2 = jnp.split(u, 3, axis=-1)
    kspec = hyena_filter_spectra(L, w1, b1, w2, b2, w3, freq, decay)
    y = x1 * fft_conv(v, kspec[:, 0], skip[0])
    y = x2 * fft_conv(y, kspec[:, 1], skip[1])
    return y


def token_mixer(hx, hc, need_ctx, w_in, w_out,
                lru_conv_w, lru_conv_b, lru_wr, lru_br, lru_wi, lru_bi, lru_lambda,
                mla_q_norm_g, mla_wqb, mla_kv_norm_g, mla_wkvb,
                hy_conv_w, hy_conv_b, hy_f_w1, hy_f_b1, hy_f_w2, hy_f_b2, hy_f_w3,
                hy_f_freq, hy_decay, hy_skip):
    B, L, _ = hx.shape
    ROWS = L // GRID_W
    row = jnp.repeat(jnp.arange(ROWS, dtype=F32), GRID_W)
    col = jnp.tile(jnp.arange(GRID_W, dtype=F32), ROWS)
    inv_freq = ROPE_THETA ** (-jnp.arange(ROPE_PAIRS, dtype=F32) / ROPE_PAIRS)
    angles = (row[:, None] * inv_freq, col[:, None] * inv_freq)

    ux = jnp.split(hx @ w_in, IN_SPLITS, axis=-1)
    uc = jnp.split(hc @ w_in, IN_SPLITS, axis=-1)
    hy_args = (hy_conv_w, hy_conv_b, hy_f_w1, hy_f_b1, hy_f_w2, hy_f_b2, hy_f_w3, hy_f_freq, hy_decay, hy_skip)

    ax_in = depthwise_conv(ux[0], lru_conv_w, lru_conv_b, 2, 1)
    ac_in = depthwise_conv(uc[0], lru_conv_w, lru_conv_b, 2, 1)
    ra_x, ra_c = rglru_bidir(ax_in, ac_in, lru_wr, lru_br, lru_wi, lru_bi, lru_lambda)
    ya_x = ra_x * jax.nn.gelu(ux[1])

    qx, kx, vx = mla_project(ux[2], ux[3], ux[4], mla_q_norm_g, mla_wqb, mla_kv_norm_g, mla_wkvb, angles)
    qc, kc, vc = mla_project(uc[2], uc[3], uc[4], mla_q_norm_g, mla_wqb, mla_kv_norm_g, mla_wkvb, None)
    k_all = jnp.concatenate([kc, kx], axis=1)
    v_all = jnp.concatenate([vc, vx], axis=1)
    nb = L // ATTN_BLOCK
    q_blocks = jnp.moveaxis(qx.reshape(B, nb, ATTN_BLOCK, MLA_HEADS, MLA_NOPE + MLA_ROPE), 1, 0)
    o_blocks = lax.map(lambda qb: attend(qb, k_all, v_all), q_blocks)
    yb_x = jnp.moveaxis(o_blocks, 0, 1).reshape(B, L, MLA_HEADS * MLA_V)

    yc_x = hyena(ux[5], *hy_args)

    out_x = jnp.concatenate([ya_x, yb_x, yc_x], axis=-1) @ w_out
    if not need_ctx:
        return out_x, None

    Lc = hc.shape[1]
    ya_c = ra_c * jax.nn.gelu(uc[1])
    yb_c = attend(qc, kc, vc).reshape(B, Lc, MLA_HEADS * MLA_V)
    yc_c = hyena(uc[5], *hy_args)
    out_c = jnp.concatenate([ya_c, yb_c, yc_c], axis=-1) @ w_out
    return out_x, out_c


def peer(h, wq, keys, u_tab, v_tab):
    B, L, D = h.shape
    tokens = h.reshape(-1, PEER_BLOCK, D)
    half = PEER_DQ // 2

    def block(xb):
        T = xb.shape[0]
        q = (xb @ wq).reshape(T, PEER_HEADS, PEER_DQ)
        s1 = jnp.einsum('thd,kd->thk', q[..., :half], keys[0], preferred_element_type=F32)
        s2 = jnp.einsum('thd,kd->thk', q[..., half:], keys[1], preferred_element_type=F32)
        v1, i1 = lax.top_k(s1, PEER_TOPK)
        v2, i2 = lax.top_k(s2, PEER_TOPK)
        cand = (v1[..., :, None] + v2[..., None, :]).reshape(T, PEER_HEADS, PEER_TOPK * PEER_TOPK)
        cid = (i1[..., :, None] * PEER_NKEYS + i2[..., None, :]).reshape(T, PEER_HEADS, PEER_TOPK * PEER_TOPK)
        top_s, top_p = lax.top_k(cand, PEER_TOPK)
        eid = jnp.take_along_axis(cid, top_p, axis=-1)
        gate = jax.nn.softmax(top_s, axis=-1).astype(xb.dtype)
        act = jax.nn.gelu(jnp.einsum('td,thkd->thk', xb, u_tab[eid]))
        return jnp.einsum('thk,thkd->td', gate * act, v_tab[eid])

    return lax.map(block, tokens).reshape(B, L, D)


def setup_inputs(seed: int = 0) -> dict:
    keys = iter(jax.random.split(jax.random.key(seed), 64))

    def nrm(shape, scale):
        return jax.random.normal(next(keys), shape, F32) * scale

    D = D_MODEL
    lam_u = jax.random.uniform(next(keys), (DEPTH, 2, D_LRU), F32, minval=0.9, maxval=0.999)
    lam_a = lam_u ** (1.0 / LRU_C)
    return {
        "x": nrm((BATCH, SEQ, D), 1.0),
        "c": nrm((BATCH, D), 1.0),
        "ctx": nrm((BATCH, CTX_LEN, D), 1.0),
        "c_ctx": nrm((D,), 1.0),
        "w_mod": nrm((DEPTH, D, 6 * D), 0.5 * D ** -0.5),
        "b_mod": nrm((DEPTH, 6 * D), 0.02),
        "norm1_g": 1.0 + nrm((DEPTH, D), 0.05),
        "norm2_g": 1.0 + nrm((DEPTH, D), 0.05),
        "w_in": nrm((DEPTH, D, D_IN), D ** -0.5),
        "w_out": nrm((DEPTH, D_MIX, D), D_MIX ** -0.5),
        "lru_conv_w": nrm((DEPTH, LRU_CONV, D_LRU), LRU_CONV ** -0.5),
        "lru_conv_b": nrm((DEPTH, D_LRU), 0.02),
        "lru_wr": nrm((DEPTH, 2, LRU_BLOCKS, LRU_BLOCK_DIM, LRU_BLOCK_DIM), LRU_BLOCK_DIM ** -0.5),
        "lru_br": nrm((DEPTH, 2, D_LRU), 0.02),
        "lru_wi": nrm((DEPTH, 2, LRU_BLOCKS, LRU_BLOCK_DIM, LRU_BLOCK_DIM), LRU_BLOCK_DIM ** -0.5),
        "lru_bi": nrm((DEPTH, 2, D_LRU), 0.02),
        "lru_lambda": jnp.log(lam_a) - jnp.log1p(-lam_a),
        "mla_q_norm_g": 1.0 + nrm((DEPTH, MLA_Q_RANK), 0.05),
        "mla_wqb": nrm((DEPTH, MLA_Q_RANK, MLA_HEADS * (MLA_NOPE + MLA_ROPE)), MLA_Q_RANK ** -0.5),
        "mla_kv_norm_g": 1.0 + nrm((DEPTH, MLA_KV_RANK), 0.05),
        "mla_wkvb": nrm((DEPTH, MLA_KV_RANK, MLA_HEADS * (MLA_NOPE + MLA_V)), MLA_KV_RANK ** -0.5),
        "hy_conv_w": nrm((DEPTH, HY_CONV, 3 * D_HY), HY_CONV ** -0.5),
        "hy_conv_b": nrm((DEPTH, 3 * D_HY), 0.02),
        "hy_f_w1": nrm((DEPTH, HY_EMB, HY_FFN), HY_EMB ** -0.5),
        "hy_f_b1": nrm((DEPTH, HY_FFN), 0.1),
        "hy_f_w2": nrm((DEPTH, HY_FFN, HY_FFN), HY_FFN ** -0.5),
        "hy_f_b2": nrm((DEPTH, HY_FFN), 0.1),
        "hy_f_w3": nrm((DEPTH, HY_FFN, 2 * HY_ORDER * D_HY), HY_FFN ** -0.5),
        "hy_f_freq": 1.0 + nrm((DEPTH, HY_FFN), 0.1),
        "hy_decay": jax.random.uniform(next(keys), (DEPTH, 2 * HY_ORDER, D_HY), F32, minval=3.0, maxval=15.0),
        "hy_skip": nrm((DEPTH, HY_ORDER, D_HY), 0.5),
        "peer_wq": nrm((DEPTH, D, PEER_HEADS * PEER_DQ), D ** -0.5),
        "peer_keys": nrm((DEPTH, 2, PEER_NKEYS, PEER_DQ // 2), (PEER_DQ // 2) ** -0.5),
        "peer_u": nrm((DEPTH, PEER_EXPERTS, D), D ** -0.5),
        "peer_v": nrm((DEPTH, PEER_EXPERTS, D), 1.0),
        "final_g": 1.0 + nrm((D,), 0.05),
    }


def reference(x, c, ctx, c_ctx, w_mod, b_mod, norm1_g, norm2_g, w_in, w_out,
              lru_conv_w, lru_conv_b, lru_wr, lru_br, lru_wi, lru_bi, lru_lambda,
              mla_q_norm_g, mla_wqb, mla_kv_norm_g, mla_wkvb,
              hy_conv_w, hy_conv_b, hy_f_w1, hy_f_b1, hy_f_w2, hy_f_b2, hy_f_w3,
              hy_f_freq, hy_decay, hy_skip,
              peer_wq, peer_keys, peer_u, peer_v, final_g):
    for l in range(DEPTH):
        need_ctx = l < DEPTH - 1
        mod_x = jax.nn.silu(c) @ w_mod[l] + b_mod[l]
        mod_c = jax.nn.silu(c_ctx) @ w_mod[l] + b_mod[l]
        sh1, sc1, g1, sh2, sc2, g2 = jnp.split(mod_x[:, None, :], 6, axis=-1)
        csh1, csc1, cg1, csh2, csc2, cg2 = jnp.split(mod_c, 6, axis=-1)

        hx = rmsnorm(x, norm1_g[l]) * (1.0 + sc1) + sh1
        hc = rmsnorm(ctx, norm1_g[l]) * (1.0 + csc1) + csh1
        yx, yc = token_mixer(hx, hc, need_ctx, w_in[l], w_out[l],
                             lru_conv_w[l], lru_conv_b[l], lru_wr[l], lru_br[l], lru_wi[l], lru_bi[l], lru_lambda[l],
                             mla_q_norm_g[l], mla_wqb[l], mla_kv_norm_g[l], mla_wkvb[l],
                             hy_conv_w[l], hy_conv_b[l], hy_f_w1[l], hy_f_b1[l], hy_f_w2[l], hy_f_b2[l], hy_f_w3[l],
                             hy_f_freq[l], hy_decay[l], hy_skip[l])
        x = x + g1 * yx
        hx = rmsnorm(x, norm2_g[l]) * (1.0 + sc2) + sh2
        x = x + g2 * peer(hx, peer_wq[l], peer_keys[l], peer_u[l], peer_v[l])
        if need_ctx:
            ctx = ctx + cg1 * yc
            hc = rmsnorm(ctx, norm2_g[l]) * (1.0 + csc2) + csh2
            ctx = ctx + cg2 * peer(hc, peer_wq[l], peer_keys[l], peer_u[l], peer_v[l])
    return rmsnorm(x, final_g)
```

```python
from contextlib import ExitStack
import numpy as np
import concourse.bass as bass
import concourse.mybir as mybir
from concourse.bass_utils import run_bass_kernel_spmd

F32 = mybir.dt.float32
BF16 = mybir.dt.bfloat16
U32 = mybir.dt.uint32
I32 = mybir.dt.int32
AF = mybir.ActivationFunctionType
ALU = mybir.AluOpType
AX = mybir.AxisListType


def mkap(t, offset, pat):
    return bass.AP(t, offset, [list(p) for p in pat])


class Buf:
    __slots__ = ("name", "t", "last_w", "readers", "is_psum")

    def __init__(self, name, t, init_readers=None):
        self.is_psum = False
        self.name = name
        self.t = t
        self.last_w = None
        self.readers = dict(init_readers or {})


class Prog:
    NDS = 12

    def __init__(self, nc):
        self.nc = nc
        self.stack = ExitStack()
        self.eng = {"pe": nc.tensor, "act": nc.scalar, "dve": nc.vector, "pool": nc.gpsimd, "sp": nc.sync}
        self.csem = {k: self.stack.enter_context(nc.semaphore("c_" + k)) for k in self.eng}
        self.cc = {k: 0 for k in self.eng}
        self.dsem = {k: [self.stack.enter_context(nc.semaphore("d_%s%d" % (k, i))) for i in range(self.NDS)]
                     for k in ("sp", "act", "pool")}
        self.dcount = {k: 0 for k in self.dsem}
        self.waited = {k: {} for k in self.eng}
        self.free_events = {}
        self.nins = 0
        self.scopes = []

    def _track(self, b):
        if self.scopes:
            self.scopes[-1][1].append(b)
        return b

    def sb(self, name, shape, dtype):
        st = self.scopes[-1][0] if self.scopes else self.stack
        self.uid = getattr(self, "uid", 0) + 1
        name = "%s_%d" % (name, self.uid)
        t = st.enter_context(self.nc.sbuf_tensor(name, list(shape), dtype))
        return self._track(Buf(name, t, self.free_events))

    def ps(self, name, shape, dtype):
        st = self.scopes[-1][0] if self.scopes else self.stack
        self.uid = getattr(self, "uid", 0) + 1
        name = "%s_%d" % (name, self.uid)
        t = st.enter_context(self.nc.psum_tensor(name, list(shape), dtype))
        b = self._track(Buf(name, t, self.free_events))
        b.is_psum = True
        return b

    def dram(self, name, shape, dtype, kind="Internal"):
        t = self.nc.dram_tensor(name, list(shape), dtype, kind=kind)
        return Buf(name, t.ap())

    def token(self, name):
        return Buf(name, None)

    class _Scope:
        def __init__(self, p):
            self.p = p

        def __enter__(self):
            self.p.scopes.append((ExitStack(), []))
            return self

        def __exit__(self, *a):
            st, bufs = self.p.scopes.pop()
            fe = self.p.free_events
            for b in bufs:
                evs = list(b.readers.items())
                if b.last_w is not None:
                    evs.append(b.last_w)
                for k, v in evs:
                    if fe.get(k, 0) < v:
                        fe[k] = v
            st.close()
            return False

    def scope(self):
        return Prog._Scope(self)

    def _sem(self, key):
        if key[0] == "x":
            return self.xsem
        return self.csem[key[1]] if key[0] == "c" else self.dsem[key[1]][key[2]]

    def _wait(self, eng, key, val):
        if eng == "pe" and key == ("c", "pe"):
            return
        w = self.waited[eng]
        if w.get(key, 0) >= val:
            return
        w[key] = val
        self.eng[eng].wait_ge(self._sem(key), val)

    def op(self, eng, fn, r=(), w=(), dma=False):
        deps = {}
        for b in r:
            if b.last_w is not None:
                k, v = b.last_w
                if deps.get(k, 0) < v:
                    deps[k] = v
            if b.is_psum:
                for k, v in b.readers.items():
                    if k != ("c", eng) and deps.get(k, 0) < v:
                        deps[k] = v
        for b in w:
            if b.last_w is not None:
                k, v = b.last_w
                if deps.get(k, 0) < v:
                    deps[k] = v
            for k, v in b.readers.items():
                if deps.get(k, 0) < v:
                    deps[k] = v
        for k, v in deps.items():
            self._wait(eng, k, v)
        if dma:
            j = self.dcount[eng]
            self.dcount[eng] = j + 1
            slot = j % self.NDS
            key = ("d", eng, slot)
            if j >= self.NDS:
                self._wait(eng, key, 16 * (j // self.NDS))
            ev = (key, 16 * (j // self.NDS + 1))
            fn(self.eng[eng]).then_inc(self._sem(key), 16)
        else:
            self.cc[eng] += 1
            key = ("c", eng)
            ev = (key, self.cc[eng])
            fn(self.eng[eng]).then_inc(self._sem(key), 1)
        self.nins += 1
        for b in r:
            if b.readers.get(ev[0], 0) < ev[1]:
                b.readers[ev[0]] = ev[1]
        for b in w:
            b.last_w = ev
            b.readers = {}
        return ev

    def coll(self, fn, r=(), w=()):
        if not hasattr(self, "xsem"):
            self.xsem = self.stack.enter_context(self.nc.semaphore("x_coll"))
            self.xcount = 0
        deps = {}
        for b in list(r) + list(w):
            if b.last_w is not None:
                k, v = b.last_w
                deps[k] = max(deps.get(k, 0), v)
        for b in w:
            for k, v in b.readers.items():
                deps[k] = max(deps.get(k, 0), v)
        for k, v in deps.items():
            self._wait("pool", k, v)
        self.xcount += 1
        ev = (("x", "coll"), self.xcount)
        fn(self.eng["pool"]).then_inc(self.xsem)
        for b in r:
            b.readers[ev[0]] = max(b.readers.get(ev[0], 0), ev[1])
        for b in w:
            b.last_w = ev
            b.readers = {}
        return ev

    def dma(self, eng, out, in_, r=(), w=(), **kw):
        return self.op(eng, lambda e: e.dma_start(out=out, in_=in_, **kw), r=r, w=w, dma=True)

    def finish(self, out_bufs=()):
        for b in out_bufs:
            if b.last_w is not None:
                self._wait("sp", b.last_w[0], b.last_w[1])
        for k in self.eng:
            if self.cc[k] > 0:
                self._wait("sp", ("c", k), self.cc[k])
        for k in self.dsem:
            j = self.dcount[k]
            for slot in range(self.NDS):
                n = (j - slot + self.NDS - 1) // self.NDS if j > slot else 0
                if n > 0:
                    self._wait("sp", ("d", k, slot), 16 * n)
        self.stack.close()


D = 1024
NL = 8192
NCTX = 256
NS = NL + NCTX
OWN = 2048
NO = OWN + NCTX
EPS = 1e-6
D_LRU = 384
NFULL = 12
NOWNC = 6
MLA_SCALE = 96.0 ** -0.5
R_LRU, R_CKV, R_HY, R_KR, R_KRS = 0, 384, 640, 1408, 1440


def ins_bc(ap, pos, count):
    pat = [list(p) for p in ap.ap]
    pat.insert(pos, [0, count])
    return bass.AP(ap.tensor, ap.offset, pat)


def full_tiles():
    t = [(0, 256, 1)]
    for i in range(16):
        t.append((256 + 512 * i, 512, 0))
    return t


def own_tiles(need_ctx):
    t = [(0, 256, 1)] if need_ctx else []
    for i in range(4):
        t.append((256 + 512 * i, 512, 0))
    return t


class Ctx:
    pass


def load_cast(P, dst, dst_ap_fn, src_ap, ncols_total, rows=128, piece=2048, eng="pool"):
    with P.scope():
        stg = [P.sb("lc_stg%d" % i, [128, piece], F32) for i in range(2)]
        i = 0
        for c0 in range(0, ncols_total, piece):
            n = min(piece, ncols_total - c0)
            st = stg[i % 2]
            i += 1
            P.dma("sp", st.t[:rows, :n], src_ap[:, c0:c0 + n], w=[st])
            P.op(eng, lambda e, st=st, n=n, c0=c0: e.tensor_copy(dst_ap_fn(c0, n), st.t[:rows, :n]), r=[st], w=[dst])


def phase_mod(P, G, io, l):
    nc = P.nc
    G.mod = P.sb("mod", [128, 48, 2], F32)
    G.s1 = P.sb("s1", [128, 8, 2], F32)
    G.s2 = P.sb("s2", [128, 8, 2], F32)
    with P.scope():
        cv = P.sb("cv", [128, 8, 2], F32)
        sv = P.sb("sv", [128, 8, 2], F32)
        bm = P.sb("bm", [128, 48], F32)
        g12 = P.sb("g12", [128, 16], F32)
        tmp = P.sb("modtmp", [128, 8, 2], F32)
        wm = [P.sb("wm%d" % i, [128, 8, 768], F32) for i in range(2)]
        pm = P.ps("pm", [128, 48, 2], F32)
        P.dma("sp", cv.t[:], io["cvec"], w=[cv])
        P.dma("sp", bm.t[:], io["b_modT"][l], w=[bm])
        P.dma("sp", g12.t[:], io["g12T"][l], w=[g12])
        P.op("act", lambda e: e.activation(sv.t[:], cv.t[:], AF.Silu), r=[cv], w=[sv])
        wmv = io["w_mod"][l].rearrange("(k p) n -> p k n", p=128)
        for cb in range(8):
            w = wm[cb % 2]
            P.dma("sp" if cb % 2 == 0 else "act", w.t[:], wmv[:, :, cb * 768:(cb + 1) * 768], w=[w])
            for m in range(6):
                k6 = cb * 6 + m
                for k in range(8):
                    P.op("pe", lambda e, w=w, m=m, k=k, k6=k6: e.matmul(
                        pm.t[:, k6, :], w.t[:, k, m * 128:(m + 1) * 128], sv.t[:, k, :],
                        start=(k == 0), stop=(k == 7)), r=[w, sv], w=[pm])
        P.op("dve", lambda e: e.tensor_tensor(G.mod.t[:], pm.t[:], ins_bc(bm.t[:], 2, 2), ALU.add), r=[pm, bm], w=[G.mod])
        for (s, sc0, gcol) in ((G.s1, 8, 0), (G.s2, 32, 8)):
            P.op("dve", lambda e, sc0=sc0: e.tensor_scalar_add(tmp.t[:], G.mod.t[:, sc0:sc0 + 8, :], 1.0), r=[G.mod], w=[tmp])
            P.op("dve", lambda e, s=s, gcol=gcol: e.tensor_tensor(s.t[:], tmp.t[:], ins_bc(g12.t[:, gcol:gcol + 8], 2, 2), ALU.mult),
                 r=[tmp, g12], w=[s])


def norm_proj(P, G, name, src_fn, tiles, s_buf, sh_idx, w_bf, nch, dst_fn, dst_toks, out_dt=F32):
    with P.scope():
        xt = [P.sb(name + "xt%d" % i, [128, 8, 512], F32) for i in range(2)]
        sq = P.sb(name + "sq", [128, 8, 512], BF16)
        xn = P.sb(name + "xn", [128, 8, 512], F32)
        hx = [P.sb(name + "hx%d" % i, [128, 8, 512], BF16) for i in range(2)]
        rs = P.sb(name + "rs", [128, 512], F32)
        stage = [P.sb(name + "stg%d" % i, [128, nch, 512], out_dt) for i in range(2)]
        ssp = P.ps(name + "ssp", [128, 512], F32)
        pp = [P.ps(name + "pp%d" % i, [128, 512], F32) for i in range(3)]
        mi = 0
        for ti, (s0, n, j) in enumerate(tiles):
            x = xt[ti % 2]
            h = hx[ti % 2]
            stg = stage[ti % 2]
            P.dma("sp", x.t[:, :, :n], src_fn(s0, n, j), r=G.src["toks"], w=[x])
            P.op("act", lambda e, x=x, n=n: e.activation(sq.t[:, :, :n], x.t[:, :, :n], AF.Square), r=[x], w=[sq])
            for k in range(8):
                P.op("pe", lambda e, k=k, n=n: e.matmul(ssp.t[:, :n], G.ones_bf.t[:, :], sq.t[:, k, :n], start=(k == 0), stop=(k == 7)),
                     r=[sq, G.ones_bf], w=[ssp])
            P.op("act", lambda e, n=n: e.activation(rs.t[:, :n], ssp.t[:, :n], AF.Sqrt, bias=G.eps.t[:, 0:1], scale=1.0 / D), r=[ssp, G.eps], w=[rs])
            P.op("dve", lambda e, n=n: e.reciprocal(rs.t[:, :n], rs.t[:, :n]), r=[rs], w=[rs])
            P.op("dve", lambda e, x=x, n=n: e.tensor_tensor(xn.t[:, :, :n], x.t[:, :, :n], ins_bc(rs.t[:, :n], 1, 8), ALU.mult), r=[x, rs], w=[xn])
            for k in range(8):
                P.op("act", lambda e, h=h, k=k, n=n, j=j: e.activation(
                    h.t[:, k, :n], xn.t[:, k, :n], AF.Identity, scale=s_buf.t[:, k, j:j + 1], bias=G.mod.t[:, sh_idx + k, j:j + 1]),
                    r=[xn, s_buf, G.mod], w=[h])
            for m in range(nch):
                p_ = pp[mi % 3]
                for k in range(8):
                    P.op("pe", lambda e, p_=p_, m=m, k=k, n=n, h=h: e.matmul(
                        p_.t[:, :n], w_bf.t[:, k, m * 128:(m + 1) * 128], h.t[:, k, :n], start=(k == 0), stop=(k == 7)),
                        r=[w_bf, h], w=[p_])
                if mi % 2 == 0:
                    P.op("act", lambda e, p_=p_, m=m, n=n, stg=stg: e.activation(stg.t[:, m, :n], p_.t[:, :n], AF.Identity), r=[p_], w=[stg])
                else:
                    P.op("dve", lambda e, p_=p_, m=m, n=n, stg=stg: e.tensor_copy(stg.t[:, m, :n], p_.t[:, :n]), r=[p_], w=[stg])
                mi += 1
            P.dma("sp" if ti % 2 == 0 else "pool", dst_fn(s0, n), stg.t[:, :, :n], r=[stg], w=[dst_toks[ti]])


ROPE_PERM = list(range(8, 16)) + list(range(0, 8)) + list(range(24, 32)) + list(range(16, 24))


def chunkT(v, nch):
    return np.ascontiguousarray(np.asarray(v, np.float32).reshape(nch, 128).T)


def prep_shared(inp):
    f = lambda a: np.ascontiguousarray(np.asarray(a, np.float32))
    S = {}
    w_in = f(inp["w_in"])
    Lr = w_in.shape[0]
    wf = np.zeros((Lr, D, NFULL * 128), np.float32)
    wf[:, :, R_LRU:R_LRU + 384] = w_in[:, :, 0:384]
    wf[:, :, R_CKV:R_CKV + 256] = w_in[:, :, 1152:1408]
    wf[:, :, R_HY:R_HY + 768] = w_in[:, :, 1440:2208]
    wf[:, :, R_KR:R_KR + 32] = w_in[:, :, 1408:1440]
    wf[:, :, R_KRS:R_KRS + 32] = w_in[:, :, 1408:1440][:, :, ROPE_PERM]
    S["w_inF"] = wf
    S["w_inO"] = np.ascontiguousarray(np.concatenate([w_in[:, :, 384:768], w_in[:, :, 768:1152]], axis=2))
    S["w_mod"] = f(inp["w_mod"])
    S["b_modT"] = np.stack([chunkT(inp["b_mod"][l], 48) for l in range(Lr)])
    prep_lru(inp, S)
    prep_mla(inp, S)
    prep_hyena(inp, S)
    prep_tail(inp, S)
    S["g12T"] = np.stack([np.concatenate([chunkT(inp["norm1_g"][l], 8), chunkT(inp["norm2_g"][l], 8)], axis=1) for l in range(Lr)])
    return S


def prep_core(inp, core):
    b, j = core // 4, core % 4
    x = np.asarray(inp["x"], np.float32)
    C = {}
    C["xT_full"] = np.ascontiguousarray(x[b].T)
    C["xT_own"] = np.ascontiguousarray(x[b, j * OWN:(j + 1) * OWN].T)
    C["cT"] = np.ascontiguousarray(np.asarray(inp["ctx"], np.float32)[b].T)
    cv = np.stack([chunkT(inp["c"][b], 8), chunkT(inp["c_ctx"], 8)], axis=2)
    C["cvec"] = np.ascontiguousarray(cv)
    prep_mla_core(C, j)
    C["idx_own"] = np.ascontiguousarray(((np.arange(8)[None, :] * 128 + np.arange(128)[:, None]) * 4 + j).astype(np.uint32))
    return C


IO_SHAPES = {
    "xT_full": [D, NL], "xT_own": [D, OWN], "cT": [D, NCTX], "cvec": [128, 8, 2],
    "w_mod": [2, D, 6144], "b_modT": [2, 128, 48], "g12T": [2, 128, 16],
    "w_inF": [2, D, NFULL * 128], "w_inO": [2, D, NOWNC * 128],
}


def declare_io(nc, names=None):
    io = {}
    for k, shp in IO_SHAPES.items():
        if names is None or k in names:
            io[k] = nc.dram_tensor(k, shp, IO_DT.get(k, F32), kind="ExternalInput").ap()
    return io


def setup_globals(P, G):
    G.ones_bf = P.sb("ones_bf", [128, 128], BF16)
    G.eps = P.sb("eps", [128, 1], F32)
    P.op("pool", lambda e: e.memset(G.ones_bf.t[:], 1.0), w=[G.ones_bf])
    P.op("pool", lambda e: e.memset(G.eps.t[:], EPS), w=[G.eps])
    G.ones_f = P.sb("ones_f", [128, 64], F32)
    P.op("pool", lambda e: e.memset(G.ones_f.t[:], 1.0), w=[G.ones_f])
    G.one = P.sb("one", [128, 1], F32)
    P.op("pool", lambda e: e.memset(G.one.t[:], 1.0), w=[G.one])
    G.ymix_tok = [P.token("ymix%d" % i) for i in range(8)]


def phase_B(P, G, io, l, need_ctx, uF, uO, dbg=False):
    ft = full_tiles()
    G.uF_tok = [P.token("uF%d" % i) for i in range(len(ft))]
    SRC = G.src
    uFv = uF.t.rearrange("(c p) s -> p c s", p=128)
    uOv = uO.t.rearrange("(c p) s -> p c s", p=128)
    with P.scope():
        wF = P.sb("wF_bf", [128, 8, NFULL * 128], BF16)
        for k in range(8):
            load_cast(P, wF, lambda c0, n, k=k: wF.t[:, k, c0:c0 + n], io["w_inF"][l, k * 128:(k + 1) * 128, :], NFULL * 128)
        norm_proj(P, G, "nf", lambda s0, n, j: (SRC["ctx"](n) if j else SRC["full"](s0 - 256, n)), ft, G.s1, 0, wF, NFULL,
                  lambda s0, n: uFv[:, :, s0:s0 + n], G.uF_tok)
    ot = own_tiles(need_ctx)
    G.uO_tok = [P.token("uO%d" % i) for i in range(len(ot))]
    with P.scope():
        wO = P.sb("wO_bf", [128, 8, NOWNC * 128], BF16)
        for k in range(8):
            load_cast(P, wO, lambda c0, n, k=k: wO.t[:, k, c0:c0 + n], io["w_inO"][l, k * 128:(k + 1) * 128, :], NOWNC * 128)
        norm_proj(P, G, "no", lambda s0, n, j: (SRC["ctx"](n) if j else SRC["own"](s0 - 256, n)), ot, G.s1, 0, wO, NOWNC,
                  lambda s0, n: uOv[:, :, s0:s0 + n], G.uO_tok)


def prep_lru(inp, S):
    f = lambda a: np.asarray(a, np.float32)
    Lr = f(inp["lru_wr"]).shape[0]
    wbd = np.zeros((Lr, 128, 12, 128), np.float32)
    for l in range(Lr):
        for d in range(2):
            for gi, nm in enumerate(("lru_wr", "lru_wi")):
                w = f(inp[nm])[l, d]
                for cc in range(3):
                    q = (d * 2 + gi) * 3 + cc
                    wbd[l, 0:64, q, 0:64] = w[2 * cc]
                    wbd[l, 64:128, q, 64:128] = w[2 * cc + 1]
    S["lru_wbd"] = wbd
    vec = np.zeros((Lr, 128, 6 + 6 + 6 + 12 + 3), np.float32)
    for l in range(Lr):
        for d in range(2):
            vec[l, :, d * 3:d * 3 + 3] = chunkT(inp["lru_br"][l, d], 3)
            vec[l, :, 6 + d * 3:6 + d * 3 + 3] = chunkT(inp["lru_bi"][l, d], 3)
            vec[l, :, 12 + d * 3:12 + d * 3 + 3] = chunkT(inp["lru_lambda"][l, d], 3)
        for k in range(4):
            vec[l, :, 18 + k:18 + 12:4] = chunkT(inp["lru_conv_w"][l, k], 3)
        vec[l, :, 30:33] = chunkT(inp["lru_conv_b"][l], 3)
    S["lru_vec"] = vec


IO_SHAPES.update({"lru_wbd": [2, 128, 12, 128], "lru_vec": [2, 128, 33], "idx_own": [128, 8]})
IO_DT = {"idx_own": U32}


def phase_C(P, G, io, l, need_ctx, uF, uO, ymix):
    ft = full_tiles()
    with P.scope():
        wbd = P.sb("lru_wbd", [128, 12, 128], BF16)
        load_cast(P, wbd, lambda c0, n: wbd.t[:].rearrange("p a b -> p (a b)")[:, c0:c0 + n],
                  io["lru_wbd"][l].rearrange("p a b -> p (a b)"), 12 * 128)
        vec = P.sb("lru_vecs", [128, 33], F32)
        P.dma("sp", vec.t[:], io["lru_vec"][l], w=[vec])
        c8 = P.sb("lru_c8", [128, 12], F32)
        P.op("act", lambda e: e.activation(c8.t[:, 0:6], vec.t[:, 12:18], AF.Exp, scale=-1.0), r=[vec], w=[c8])
        P.op("act", lambda e: e.activation(c8.t[:, 0:6], c8.t[:, 0:6], AF.Ln, bias=G.one.t[:, 0:1], scale=1.0), r=[c8, G.one], w=[c8])
        P.op("dve", lambda e: e.tensor_scalar_mul(c8.t[:, 6:12], c8.t[:, 0:6], -16.0), r=[c8], w=[c8])
        P.op("dve", lambda e: e.tensor_scalar_mul(c8.t[:, 0:6], c8.t[:, 0:6], -8.0), r=[c8], w=[c8])
        idx = P.sb("idx_own", [128, 8], U32)
        P.dma("sp", idx.t[:], io["idx_own"], w=[idx])
        hL = [P.dram("hL%d_%d" % (l, d), [384, NL], F32) for d in range(2)]
        hC = [P.dram("hC%d_%d" % (l, d), [384, NCTX], F32) for d in range(2)]
        for cc in range(3):
            with P.scope():
                X1 = P.sb("lruX1", [128, NS + 8], F32)
                xc = P.sb("lruxc", [128, NS], F32)
                xcb = P.sb("lruxcb", [128, NS], BF16)
                Bf = P.sb("lruB", [128, NS], F32)
                T = P.sb("lruT", [128, NS], F32)
                pr = [P.ps("lrupr%d" % i, [128, 512], F32) for i in range(4)]
                P.op("pool", lambda e: e.memset(X1.t[:, 0:264], 0.0), w=[X1])
                P.op("pool", lambda e: e.memset(X1.t[:, NS:NS + 8], 0.0), w=[X1])
                rows = slice(R_LRU + cc * 128, R_LRU + (cc + 1) * 128)
                P.dma("sp", X1.t[:, 2:258], uF.t[rows, 0:256], r=G.uF_tok, w=[X1])
                P.dma("act", X1.t[:, 261:261 + NL], uF.t[rows, 256:NS], r=G.uF_tok, w=[X1])
                for (o0, n, b0) in ((0, 256, 0), (256, NL, 259)):
                    P.op("dve", lambda e, o0=o0, n=n, b0=b0: e.tensor_scalar(
                        xc.t[:, o0:o0 + n], X1.t[:, b0:b0 + n], vec.t[:, 18 + cc * 4:19 + cc * 4], vec.t[:, 30 + cc:31 + cc], ALU.mult, ALU.add),
                        r=[X1, vec], w=[xc])
                    for k in range(1, 4):
                        P.op("dve", lambda e, o0=o0, n=n, b0=b0, k=k: e.scalar_tensor_tensor(
                            xc.t[:, o0:o0 + n], X1.t[:, b0 + k:b0 + k + n], vec.t[:, 18 + cc * 4 + k:19 + cc * 4 + k], xc.t[:, o0:o0 + n],
                            ALU.mult, ALU.add), r=[X1, vec, xc], w=[xc])
                P.op("pool", lambda e: e.tensor_copy(xcb.t[:], xc.t[:]), r=[xc], w=[xcb])
                A = X1
                for d in range(2):
                    pi = 0
                    for (s0, n, j) in ft:
                        for gi, dst in ((0, A), (1, Bf)):
                            p_ = pr[pi % 4]
                            pi += 1
                            q = (d * 2 + gi) * 3 + cc
                            P.op("pe", lambda e, p_=p_, q=q, s0=s0, n=n: e.matmul(p_.t[:, :n], wbd.t[:, q, :], xcb.t[:, s0:s0 + n], start=True, stop=True),
                                 r=[wbd, xcb], w=[p_])
                            P.op("act", lambda e, p_=p_, dst=dst, s0=s0, n=n, gi=gi, d=d: e.activation(
                                dst.t[:, s0:s0 + n], p_.t[:, :n], AF.Sigmoid, bias=vec.t[:, gi * 6 + d * 3 + cc:gi * 6 + d * 3 + cc + 1]),
                                r=[p_, vec], w=[dst])
                    P.op("act", lambda e, d=d: e.activation(T.t[:], A.t[:, 0:NS], AF.Exp, scale=c8.t[:, 6 + d * 3 + cc:7 + d * 3 + cc]), r=[A, c8], w=[T])
                    P.op("act", lambda e, d=d: e.activation(A.t[:, 0:NS], A.t[:, 0:NS], AF.Exp, scale=c8.t[:, d * 3 + cc:d * 3 + cc + 1]), r=[A, c8], w=[A])
                    P.op("act", lambda e: e.activation(T.t[:], T.t[:], AF.Sqrt, bias=G.one.t[:, 0:1], scale=-1.0), r=[T, G.one], w=[T])
                    P.op("dve", lambda e: e.tensor_tensor(Bf.t[:], Bf.t[:], T.t[:], ALU.mult), r=[Bf, T], w=[Bf])
                    P.op("pool", lambda e: e.tensor_tensor(Bf.t[:], Bf.t[:], xc.t[:], ALU.mult), r=[Bf, xc], w=[Bf])
                    ps_ = Bf.t[:].ap[0][0]
                    if d == 0:
                        P.op("dve", lambda e: e.tensor_tensor_scan(Bf.t[:], A.t[:, 0:NS], Bf.t[:], 0.0, ALU.mult, ALU.add), r=[A, Bf], w=[Bf])
                    else:
                        pa = A.t[:].ap[0][0]
                        rv = lambda t, pst, lo, n: mkap(t.t, lo + n - 1, [[pst, 128], [-1, n]])
                        P.op("dve", lambda e: e.tensor_tensor_scan(rv(Bf, ps_, 0, 256), rv(A, pa, 0, 256), rv(Bf, ps_, 0, 256), 0.0, ALU.mult, ALU.add),
                             r=[A, Bf], w=[Bf])
                        P.op("dve", lambda e: e.tensor_tensor_scan(rv(Bf, ps_, 256, NL), rv(A, pa, 256, NL), rv(Bf, ps_, 256, NL), Bf.t[:, 0:1],
                                                                   ALU.mult, ALU.add), r=[A, Bf], w=[Bf])
                    P.dma("sp", hC[d].t[cc * 128:(cc + 1) * 128, :], Bf.t[:, 0:256], r=[Bf], w=[hC[d]])
                    P.dma("sp", hL[d].t[cc * 128:(cc + 1) * 128, :], Bf.t[:, 256:NS], r=[Bf], w=[hL[d]])
        for cc in range(3):
            with P.scope():
                g0 = P.sb("lrug0", [128, NO], F32)
                g1 = P.sb("lrug1", [128, NO], F32)
                gt = P.sb("lrugt", [128, NO], F32)
                yo = P.sb("lruyo", [128, NO], BF16)
                c0 = 0 if need_ctx else 256
                for d, g in ((0, g0), (1, g1)):
                    if need_ctx:
                        P.dma("sp", g.t[:, 0:256], hC[d].t[cc * 128:(cc + 1) * 128, :], r=[hC[d]], w=[g])
                    P.op("pool", lambda e, g=g, d=d: e.indirect_dma_start(
                        out=g.t[:, 256:NO], out_offset=None, in_=hL[d].t.rearrange("c (q s) -> (c q) s", q=4),
                        in_offset=bass.IndirectOffsetOnAxis(ap=idx.t[:, cc:cc + 1], axis=0)), r=[hL[d], idx], w=[g], dma=True)
                P.dma("sp", gt.t[:, c0:NO], uO.t[cc * 128:(cc + 1) * 128, c0:NO], r=G.uO_tok, w=[gt])
                P.op("act", lambda e: e.activation(gt.t[:, c0:NO], gt.t[:, c0:NO], AF.Gelu_apprx_tanh), r=[gt], w=[gt])
                P.op("dve", lambda e: e.tensor_tensor(g0.t[:, c0:NO], g0.t[:, c0:NO], g1.t[:, c0:NO], ALU.add), r=[g0, g1], w=[g0])
                P.op("dve", lambda e: e.tensor_tensor(yo.t[:, c0:NO], g0.t[:, c0:NO], gt.t[:, c0:NO], ALU.mult), r=[g0, gt], w=[yo])
                P.dma("sp", ymix.t[cc * 128:(cc + 1) * 128, c0:NO], yo.t[:, c0:NO], r=[yo], w=[G.ymix_tok[cc]])


def rope_tables(pos):
    pos = np.asarray(pos)
    inv = (np.float32(10000.0) ** (-np.arange(8, dtype=np.float32) / np.float32(8))).astype(np.float32)
    row = (pos // 64).astype(np.float32)
    col = (pos % 64).astype(np.float32)
    Cc = np.ones((32, len(pos)), np.float32)
    Ss = np.zeros((32, len(pos)), np.float32)
    for r in range(32):
        ang = (row if r < 16 else col) * inv[r % 8]
        c, s = np.cos(ang.astype(np.float32)), np.sin(ang.astype(np.float32))
        Cc[r] = np.where(pos >= 0, c, 1.0)
        Ss[r] = np.where(pos >= 0, (-s if (r % 16) < 8 else s), 0.0)
    return Cc.astype(np.float32), Ss.astype(np.float32)


def prep_mla(inp, S):
    f = lambda a: np.asarray(a, np.float32)
    wkvb = f(inp["mla_wkvb"])
    Lr = wkvb.shape[0]
    w4 = wkvb.reshape(Lr, 256, 6, 2, 64)
    S["wkvb_r"] = np.ascontiguousarray(np.concatenate([w4[:, :, :, 0, :].reshape(Lr, 256, 384), w4[:, :, :, 1, :].reshape(Lr, 256, 384)], axis=2))
    wqb = f(inp["mla_wqb"]).reshape(Lr, 384, 6, 96)
    sw = wqb.copy()
    sw[..., 64:96] = wqb[..., 64:96][..., ROPE_PERM]
    S["wq_ext"] = np.ascontiguousarray(np.stack([wqb, sw], axis=3).reshape(Lr, 384, 6 * 2 * 96))
    S["mla_g"] = np.stack([np.concatenate([chunkT(inp["mla_q_norm_g"][l], 3), chunkT(inp["mla_kv_norm_g"][l], 2)], axis=1) for l in range(Lr)])
    pos = np.concatenate([-np.ones(NCTX, np.int64), np.arange(NL)])
    Cc, Ss = rope_tables(pos)
    S["ropeK"] = np.ascontiguousarray(np.stack([Cc, Ss]))


def prep_mla_core(C, j):
    pos = np.concatenate([-np.ones(NCTX, np.int64), np.arange(j * OWN, (j + 1) * OWN)])
    Cc, Ss = rope_tables(pos)
    Cq = np.ones((96, NO), np.float32); Cq[64:96] = Cc
    Sq = np.zeros((96, NO), np.float32); Sq[64:96] = Ss
    C["ropeQ"] = np.ascontiguousarray(np.stack([Cq, Sq]))


IO_SHAPES.update({"wkvb_r": [2, 256, 768], "wq_ext": [2, 384, 1152], "mla_g": [2, 128, 5], "ropeK": [2, 32, NS], "ropeQ": [2, 96, NO]})


def rms_fm(P, G, name, src_fn, tiles, nk, dim, gains, gcol, dst, src_toks):
    with P.scope():
        xt = [P.sb(name + "x%d" % i, [128, nk, 512], F32) for i in range(2)]
        sq = P.sb(name + "sq", [128, nk, 512], BF16)
        rs = P.sb(name + "rs", [128, 512], F32)
        ssp = P.ps(name + "ss", [128, 512], F32)
        for ti, (s0, n, j) in enumerate(tiles):
            x = xt[ti % 2]
            P.dma("sp" if ti % 2 == 0 else "act", x.t[:, :, :n], src_fn(s0, n), r=src_toks, w=[x])
            P.op("act", lambda e, x=x, n=n: e.activation(sq.t[:, :, :n], x.t[:, :, :n], AF.Square), r=[x], w=[sq])
            for k in range(nk):
                P.op("pe", lambda e, k=k, n=n: e.matmul(ssp.t[:, :n], G.ones_bf.t[:, :], sq.t[:, k, :n], start=(k == 0), stop=(k == nk - 1)),
                     r=[sq, G.ones_bf], w=[ssp])
            P.op("act", lambda e, n=n: e.activation(rs.t[:, :n], ssp.t[:, :n], AF.Sqrt, bias=G.eps.t[:, 0:1], scale=1.0 / dim), r=[ssp, G.eps], w=[rs])
            P.op("dve", lambda e, n=n: e.reciprocal(rs.t[:, :n], rs.t[:, :n]), r=[rs], w=[rs])
            for k in range(nk):
                P.op("dve", lambda e, x=x, k=k, n=n, s0=s0: e.scalar_tensor_tensor(
                    dst.t[:, k, s0:s0 + n], x.t[:, k, :n], gains.t[:, gcol + k:gcol + k + 1], rs.t[:, :n], ALU.mult, ALU.mult),
                    r=[x, gains, rs], w=[dst])


def phase_D(P, G, io, l, need_ctx, uF, uO, ymix):
    ft = full_tiles()
    ot = own_tiles(need_ctx)
    with P.scope():
        mg = P.sb("mla_g", [128, 5], F32)
        P.dma("sp", mg.t[:], io["mla_g"][l], w=[mg])
        wkv = P.sb("wkv_bf", [128, 2, 768], BF16)
        for k in range(2):
            load_cast(P, wkv, lambda c0, n, k=k: wkv.t[:, k, c0:c0 + n], io["wkvb_r"][l, k * 128:(k + 1) * 128, :], 768)
        wq = P.sb("wq_bf", [128, 3, 1152], BF16)
        for k in range(3):
            load_cast(P, wq, lambda c0, n, k=k: wq.t[:, k, c0:c0 + n], io["wq_ext"][l, k * 128:(k + 1) * 128, :], 1152)
        ckvn = P.sb("ckvn", [128, 2, NS], BF16)
        uFv = uF.t.rearrange("(c p) s -> p c s", p=128)
        uOv = uO.t.rearrange("(c p) s -> p c s", p=128)
        rms_fm(P, G, "kvn", lambda s0, n: uFv[:, 3:5, s0:s0 + n], ft, 2, 256, mg, 3, ckvn, G.uF_tok)
        cqn = P.sb("cqn", [128, 3, NO], BF16)
        rms_fm(P, G, "qn", lambda s0, n: uOv[:, 3:6, s0:s0 + n], ot, 3, 384, mg, 0, cqn, G.uO_tok)
        krb = P.sb("krb", [96, NS], BF16)
        with P.scope():
            n = 2112
            a = P.sb("kr_a", [96, n], F32); b_ = P.sb("kr_b", [96, n], F32)
            tc_ = P.sb("kr_c", [96, n], F32); ts_ = P.sb("kr_s", [96, n], F32)
            for c0 in range(0, NS, 2112):
                P.dma("sp", a.t[64:96, :], uF.t[R_KR:R_KR + 32, c0:c0 + n], r=G.uF_tok, w=[a])
                P.dma("act", b_.t[64:96, :], uF.t[R_KRS:R_KRS + 32, c0:c0 + n], r=G.uF_tok, w=[b_])
                P.dma("sp", tc_.t[64:96, :], io["ropeK"][0, :, c0:c0 + n], w=[tc_])
                P.dma("act", ts_.t[64:96, :], io["ropeK"][1, :, c0:c0 + n], w=[ts_])
                P.op("dve", lambda e, a=a, tc_=tc_: e.tensor_tensor(a.t[64:96, :], a.t[64:96, :], tc_.t[64:96, :], ALU.mult), r=[a, tc_], w=[a])
                P.op("pool", lambda e, b_=b_, ts_=ts_: e.tensor_tensor(b_.t[64:96, :], b_.t[64:96, :], ts_.t[64:96, :], ALU.mult), r=[b_, ts_], w=[b_])
                P.op("dve", lambda e, a=a, b_=b_, c0=c0, n=n: e.tensor_tensor(krb.t[64:96, c0:c0 + n], a.t[64:96, :], b_.t[64:96, :], ALU.add), r=[a, b_], w=[krb])
        tq = P.sb("ropeQ", [96, 2, NO], F32)
        P.dma("sp", tq.t[:, 0, :], io["ropeQ"][0], w=[tq])
        P.dma("sp", tq.t[:, 1, :], io["ropeQ"][1], w=[tq])
        for pr in range(3):
            with P.scope():
                Kh = [P.sb("Kh%d" % i, [96, NS], BF16) for i in range(2)]
                Vp = P.sb("Vp", [128, 66, 2, 65], BF16)
                Qh = [P.sb("Qh%d" % i, [96, NO], BF16) for i in range(2)]
                with P.scope():
                    pk = [P.ps("pk%d" % i, [128, 512], F32) for i in range(4)]
                    t1 = P.sb("qt1", [96, 512], F32); t2 = P.sb("qt2", [96, 512], F32)
                    pi = 0
                    for i in range(2):
                        h = 2 * pr + i
                        for (s0, n, j) in ft:
                            p_ = pk[pi % 4]; pi += 1
                            for k in range(2):
                                P.op("pe", lambda e, p_=p_, k=k, h=h, s0=s0, n=n: e.matmul(p_.t[0:64, :n], wkv.t[:, k, h * 64:(h + 1) * 64], ckvn.t[:, k, s0:s0 + n],
                                                                                     start=(k == 0), stop=(k == 1)), r=[wkv, ckvn], w=[p_])
                            if pi % 2:
                                P.op("act", lambda e, p_=p_, i=i, s0=s0, n=n: e.activation(Kh[i].t[0:64, s0:s0 + n], p_.t[0:64, :n], AF.Identity), r=[p_], w=[Kh[i]])
                            else:
                                P.op("dve", lambda e, p_=p_, i=i, s0=s0, n=n: e.tensor_copy(Kh[i].t[0:64, s0:s0 + n], p_.t[0:64, :n]), r=[p_], w=[Kh[i]])
                        P.op("pool", lambda e, i=i: e.tensor_copy(Kh[i].t[64:96, :], krb.t[64:96, :]), r=[krb], w=[Kh[i]])
                        for (s0, n, j) in ot:
                            pa = pk[pi % 4]; pi += 1
                            pb = pk[pi % 4]; pi += 1
                            for v, p_ in ((0, pa), (1, pb)):
                                for k in range(3):
                                    c0 = (h * 2 + v) * 96
                                    P.op("pe", lambda e, p_=p_, k=k, c0=c0, s0=s0, n=n: e.matmul(p_.t[0:96, :n], wq.t[:, k, c0:c0 + 96], cqn.t[:, k, s0:s0 + n],
                                                                                          start=(k == 0), stop=(k == 2)), r=[wq, cqn], w=[p_])
                            P.op("dve", lambda e, pa=pa, s0=s0, n=n: e.tensor_tensor(t1.t[:, :n], pa.t[0:96, :n], tq.t[:, 0, s0:s0 + n], ALU.mult), r=[pa, tq], w=[t1])
                            P.op("dve", lambda e, pb=pb, s0=s0, n=n: e.tensor_tensor(t2.t[:, :n], pb.t[0:96, :n], tq.t[:, 1, s0:s0 + n], ALU.mult), r=[pb, tq], w=[t2])
                            P.op("pool", lambda e, i=i, s0=s0, n=n: e.tensor_tensor(Qh[i].t[:, s0:s0 + n], t1.t[:, :n], t2.t[:, :n], ALU.add), r=[t1, t2], w=[Qh[i]])
                    P.op("pool", lambda e: e.memset(Vp.t[:, :, :, 64:65], 1.0), w=[Vp])
                    for kc4 in range(0, 66, 4):
                        p_ = pk[pi % 4]; pi += 1
                        nk4 = min(4, 66 - kc4)
                        for q in range(nk4):
                            kc = kc4 + q
                            for k in range(2):
                                P.op("pe", lambda e, p_=p_, q=q, k=k, kc=kc: e.matmul(p_.t[:, q * 128:(q + 1) * 128], ckvn.t[:, k, kc * 128:(kc + 1) * 128],
                                                                                wkv.t[:, k, 384 + pr * 128:384 + (pr + 1) * 128], start=(k == 0), stop=(k == 1)),
                                     r=[wkv, ckvn], w=[p_])
                        src4 = p_.t[:, :nk4 * 128].rearrange("p (a b c) -> p a b c", b=2, c=64)
                        if (kc4 // 4) % 2:
                            P.op("act", lambda e, src4=src4, kc4=kc4, nk4=nk4: e.activation(Vp.t[:, kc4:kc4 + nk4, :, 0:64], src4, AF.Identity), r=[p_], w=[Vp])
                        else:
                            P.op("dve", lambda e, src4=src4, kc4=kc4, nk4=nk4: e.tensor_copy(Vp.t[:, kc4:kc4 + nk4, :, 0:64], src4), r=[p_], w=[Vp])
                with P.scope():
                    st = [P.ps("att_st%d" % i, [128, 512], F32) for i in range(4)]
                    pn = [P.ps("att_n%d" % i, [128, 512], F32) for i in range(2)]
                    pbc = P.ps("att_bc", [64, 512], F32)
                    pt = [P.sb("att_pt%d" % i, [128, 512], BF16) for i in range(4)]
                    rec = P.sb("att_rec", [128, 512], F32)
                    rbc = P.sb("att_rbc", [64, 512], F32)
                    ob = [P.sb("att_o%d" % i, [64, 512], BF16) for i in range(2)]
                    ci = 0
                    qi = 0
                    for i in range(2):
                        h = 2 * pr + i
                        for (s0, n, j) in ot:
                            nkc = 2 if j else 66
                            pn_, o_ = pn[qi % 2], ob[qi % 2]
                            qi += 1
                            def emit_pv(kc, p_, pn_=pn_, i=i, n=n, nkc=nkc):
                                P.op("pe", lambda e: e.matmul(pn_.t[0:65, :n], Vp.t[:, kc, i, :], p_.t[:, :n], start=(kc == 0), stop=(kc == nkc - 1)), r=[Vp, p_], w=[pn_])
                            prev = None
                            for kc in range(nkc):
                                if kc % 8 == 7 and G.bg:
                                    G.bg.pop(0)()
                                s_ = st[ci % 4]; p_ = pt[ci % 4]; ci += 1
                                P.op("pe", lambda e, s_=s_, i=i, kc=kc, s0=s0, n=n: e.matmul(s_.t[:, :n], Kh[i].t[0:96, kc * 128:(kc + 1) * 128], Qh[i].t[0:96, s0:s0 + n],
                                                                                       start=True, stop=True), r=[Kh[i], Qh[i]], w=[s_])
                                P.op("act", lambda e, s_=s_, p_=p_, n=n: e.activation(p_.t[:, :n], s_.t[:, :n], AF.Exp, scale=MLA_SCALE), r=[s_], w=[p_])
                                if prev is not None:
                                    emit_pv(*prev)
                                prev = (kc, p_)
                            emit_pv(*prev)
                            P.op("dve", lambda e, pn_=pn_, n=n: e.reciprocal(rec.t[64:65, :n], pn_.t[64:65, :n]), r=[pn_], w=[rec])
                            P.op("pe", lambda e, n=n: e.matmul(pbc.t[:, :n], G.ones_f.t[64:65, 0:64], rec.t[64:65, :n], start=True, stop=True), r=[G.ones_f, rec], w=[pbc])
                            P.op("act", lambda e, n=n: e.activation(rbc.t[:, :n], pbc.t[:, :n], AF.Identity), r=[pbc], w=[rbc])
                            P.op("dve", lambda e, pn_=pn_, o_=o_, n=n: e.tensor_tensor(o_.t[:, :n], pn_.t[0:64, :n], rbc.t[:, :n], ALU.mult), r=[pn_, rbc], w=[o_])
                            P.dma("sp", ymix.t[384 + h * 64:384 + (h + 1) * 64, s0:s0 + n], o_.t[:, :n], r=[o_], w=[G.ymix_tok[3 + pr]])


NFFT = 16384
MAGIC = 12582912.0
TWO_PI = 6.283185307179586


def prep_hyena(inp, S):
    f = lambda a: np.asarray(a, np.float32)
    Lr = f(inp["hy_f_w1"]).shape[0]
    n = np.arange(128)
    ang = 2.0 * np.pi * np.outer(n, n) / 128.0
    Fr, Fi = np.cos(ang), -np.sin(ang)
    S["dft128"] = np.ascontiguousarray(np.stack([Fr, Fi, Fr, -Fi], axis=1).astype(np.float32))
    th = 2.0 * np.pi * np.outer(n, n) / NFFT
    c, s = np.cos(th), np.sin(th)
    S["tw128"] = np.ascontiguousarray(np.stack([c, c, s, -s, s], axis=1).astype(np.float32))
    S["ident"] = np.eye(128, dtype=np.float32)

    def zfeat(L):
        t = (np.arange(L, dtype=np.float32) / np.float32(L)).astype(np.float32)
        bands = np.linspace(1e-4, 7, 8, dtype=np.float32)
        wpos = (np.float32(2.0 * np.pi) * t[:, None] * bands[None, :]).astype(np.float32)
        z = np.concatenate([t[:, None], np.cos(wpos), np.sin(wpos)], axis=-1).astype(np.float32)
        return np.ascontiguousarray(z.T)
    S["zT"] = zfeat(NL)
    S["zTc"] = zfeat(NCTX)
    S["hy_w1"] = f(inp["hy_f_w1"])
    S["hy_w2"] = f(inp["hy_f_w2"])
    S["hy_w3"] = f(inp["hy_f_w3"])
    hv = np.zeros((Lr, 128, 3), np.float32)
    hv[:, 0:64, 0] = f(inp["hy_f_freq"]); hv[:, 0:64, 1] = f(inp["hy_f_b1"]); hv[:, 0:64, 2] = f(inp["hy_f_b2"])
    S["hy_vec"] = hv
    S["hy_decT"] = np.stack([chunkT(f(inp["hy_decay"])[l].reshape(1024), 8) for l in range(Lr)])
    cw = f(inp["hy_conv_w"]); cb = f(inp["hy_conv_b"])
    S["hy_cw"] = np.stack([np.concatenate([np.stack([chunkT(cw[l, k], 6) for k in range(3)], axis=2).reshape(128, 18), chunkT(cb[l], 6)], axis=1) for l in range(Lr)])
    S["hy_skip"] = np.ascontiguousarray(f(inp["hy_skip"]).reshape(Lr, 1, 512))
    k5 = np.arange(512)
    a5 = 2.0 * np.pi * np.outer(k5, k5) / 512.0
    F5 = np.stack([np.cos(a5), -np.sin(a5)], axis=1).astype(np.float32)
    S["dft512"] = np.ascontiguousarray(F5.reshape(4, 128, 2, 512).transpose(1, 0, 2, 3))


IO_SHAPES.update({"dft128": [128, 4, 128], "tw128": [128, 5, 128], "ident": [128, 128], "zT": [17, NL], "zTc": [17, NCTX],
                  "hy_w1": [2, 17, 64], "hy_w2": [2, 64, 64], "hy_w3": [2, 64, 1024], "hy_vec": [2, 128, 3], "hy_decT": [2, 128, 8],
                  "hy_cw": [2, 128, 24], "hy_skip": [2, 1, 512], "dft512": [128, 4, 2, 512]})


def hy_sin_layer(P, G, name, ps_in, n, hv, fcol, dst_ap, tmp, dst_buf):
    a, kq = tmp
    P.op("dve", lambda e: e.tensor_scalar(a.t[0:64, :n], ps_in.t[0:64, :n], hv.t[0:64, 0:1], hv.t[0:64, fcol:fcol + 1], ALU.mult, ALU.add), r=[ps_in, hv], w=[a])
    P.op("dve", lambda e: e.tensor_scalar(kq.t[0:64, :n], a.t[0:64, :n], 1.0 / TWO_PI, MAGIC, ALU.mult, ALU.add), r=[a], w=[kq])
    P.op("dve", lambda e: e.tensor_scalar(kq.t[0:64, :n], kq.t[0:64, :n], MAGIC, -TWO_PI, ALU.subtract, ALU.mult), r=[kq], w=[kq])
    P.op("dve", lambda e: e.tensor_tensor(a.t[0:64, :n], a.t[0:64, :n], kq.t[0:64, :n], ALU.add), r=[a, kq], w=[a])
    P.op("dve", lambda e: e.tensor_scalar(a.t[0:64, :n], a.t[0:64, :n], -3.14159, 3.14159, ALU.max, ALU.min), r=[a], w=[a])
    P.op("act", lambda e: e.activation(dst_ap, a.t[0:64, :n], AF.Sin), r=[a], w=[dst_buf])


def hy_filters(P, G, io, l, L, zT_ap, emit_chunk, junk):
    with P.scope():
        hv = P.sb("hy_vec", [128, 5], F32)
        P.dma("sp", hv.t[:, 0:3], io["hy_vec"][l], w=[hv])
        P.op("dve", lambda e: e.tensor_tensor(hv.t[:, 3:5], hv.t[:, 1:3], mkap(hv.t, 0, [[5, 128], [0, 2]]), ALU.mult), r=[hv], w=[hv])
        dec = P.sb("hy_dec", [128, 8], F32)
        P.dma("sp", dec.t[:], io["hy_decT"][l], w=[dec])
        P.op("dve", lambda e: e.tensor_scalar_mul(dec.t[:], dec.t[:], -1.0), r=[dec], w=[dec])
        w1 = P.sb("hy_w1", [17, 64], F32); w2 = P.sb("hy_w2", [64, 64], F32); w3 = P.sb("hy_w3", [64, 1024], F32)
        P.dma("sp", w1.t[:], io["hy_w1"][l], w=[w1]); P.dma("sp", w2.t[:], io["hy_w2"][l], w=[w2]); P.dma("sp", w3.t[:], io["hy_w3"][l], w=[w3])
        zts = [P.sb("hy_zt%d" % i, [17, 512], F32) for i in range(2)]
        tb = P.sb("hy_tb", [128, L], F32)
        P.dma("sp", tb.t[:], zT_ap[0:1, :].broadcast_to([128, L]), w=[tb])
        h2 = P.sb("hy_h2", [64, L], F32)
        tiles = [(s0, min(512, L - s0)) for s0 in range(0, L, 512)]
        with P.scope():
            h1 = P.sb("hy_h1", [64, L], F32)
            a = P.sb("hy_a", [64, 512], F32); kq = P.sb("hy_kq", [64, 512], F32)
            pp = [P.ps("hy_pp%d" % i, [64, 512], F32) for i in range(2)]
            for ti, (s0, n) in enumerate(tiles):
                p_ = pp[ti % 2]
                zt = zts[ti % 2]
                P.dma("sp", zt.t[:, :n], zT_ap[:, s0:s0 + n], w=[zt])
                P.op("pe", lambda e, p_=p_, zt=zt, n=n: e.matmul(p_.t[:, :n], w1.t[:, :], zt.t[:, :n], start=True, stop=True), r=[w1, zt], w=[p_])
                hy_sin_layer(P, G, "s1", p_, n, hv, 3, h1.t[:, s0:s0 + n], (a, kq), h1)
            for ti, (s0, n) in enumerate(tiles):
                p_ = pp[ti % 2]
                P.op("pe", lambda e, p_=p_, s0=s0, n=n: e.matmul(p_.t[:, :n], w2.t[:, :], h1.t[:, s0:s0 + n], start=True, stop=True), r=[w2, h1], w=[p_])
                hy_sin_layer(P, G, "s2", p_, n, hv, 4, h2.t[:, s0:s0 + n], (a, kq), h2)
        with P.scope():
            kk = [P.sb("hy_kk%d" % i, [128, L], F32) for i in range(2)]
            et = [P.sb("hy_et%d" % i, [128, 512], F32) for i in range(2)]
            ss = P.sb("hy_ss", [128, 1], F32)
            pk = [P.ps("hy_pk%d" % i, [128, 512], F32) for i in range(2)]
            for q in range(8):
                k_ = kk[q % 2]
                for ti, (s0, n) in enumerate(tiles):
                    p_ = pk[ti % 2]; e_ = et[ti % 2]
                    P.op("pe", lambda e, p_=p_, s0=s0, n=n, q=q: e.matmul(p_.t[:, :n], w3.t[:, q * 128:(q + 1) * 128], h2.t[:, s0:s0 + n], start=True, stop=True), r=[w3, h2], w=[p_])
                    P.op("act", lambda e, e_=e_, s0=s0, n=n, q=q: e.activation(e_.t[:, :n], tb.t[:, s0:s0 + n], AF.Exp, scale=dec.t[:, q:q + 1]), r=[tb, dec], w=[e_])
                    P.op("dve", lambda e, p_=p_, e_=e_, k_=k_, s0=s0, n=n: e.tensor_tensor(k_.t[:, s0:s0 + n], p_.t[:, :n], e_.t[:, :n], ALU.mult), r=[p_, e_], w=[k_])
                P.op("act", lambda e, k_=k_: e.activation(junk.t[:], k_.t[:], AF.Square, accum_out=ss.t[:, 0:1]), r=[k_], w=[junk, ss])
                P.op("act", lambda e: e.activation(ss.t[:], ss.t[:], AF.Sqrt, bias=G.eps.t[:, 0:1], scale=1.0), r=[ss, G.eps], w=[ss])
                P.op("dve", lambda e: e.reciprocal(ss.t[:], ss.t[:]), r=[ss], w=[ss])
                P.op("dve", lambda e, k_=k_: e.tensor_scalar(k_.t[:], k_.t[:], ss.t[:, 0:1], None, ALU.mult), r=[k_, ss], w=[k_])
                emit_chunk(q, k_)


def fft_consts(P, G, io):
    G.DF = P.sb("dft_bf", [128, 4, 128], BF16)
    load_cast(P, G.DF, lambda c0, n: G.DF.t[:].rearrange("p a b -> p (a b)")[:, c0:c0 + n], io["dft128"].rearrange("p a b -> p (a b)"), 512)
    G.TW = P.sb("tw", [128, 5, 128], F32)
    P.dma("sp", G.TW.t[:], io["tw128"], w=[G.TW])


def cplx_mul_tab(P, G, src_ps, nch, tab, i1, i2, t1, t2, dst):
    tp = tab.t[:].ap[0][0]
    sp_ = src_ps.t[:].ap[0][0]
    tabA = mkap(tab.t, i1 * 128, [[tp, 128], [0, nch], [128, 2], [1, 128]])
    tabB = mkap(tab.t, i2 * 128, [[tp, 128], [0, nch], [128, 2], [1, 128]])
    srcA = mkap(src_ps.t, 0, [[sp_, 128], [256, nch], [128, 2], [1, 128]])
    srcSw = mkap(src_ps.t, 128, [[sp_, 128], [256, nch], [-128, 2], [1, 128]])
    P.op("dve", lambda e: e.tensor_tensor(t1.t[:, 0:nch], srcA, tabA, ALU.mult), r=[src_ps, tab], w=[t1])
    P.op("dve", lambda e: e.tensor_tensor(t2.t[:, 0:nch], srcSw, tabB, ALU.mult), r=[src_ps, tab], w=[t2])
    P.op("pool", lambda e: e.tensor_tensor(dst.t[:, 0:nch], t1.t[:, 0:nch], t2.t[:, 0:nch], ALU.add), r=[t1, t2], w=[dst])


def fft_fwd_group(P, G, xin_fn, K, nch, PA, PXr, PXi, t1, t2, Ab):
    DF = G.DF
    for c in range(nch):
        ap, bufs = xin_fn(c)
        P.op("pe", lambda e, c=c, ap=ap: e.matmul(PA.t[:, c].rearrange("p a b -> p (a b)"), ap, DF.t[0:K, 0:2, :].rearrange("p a b -> p (a b)"), start=True, stop=True),
             r=bufs + [DF], w=[PA])
    cplx_mul_tab(P, G, PA, nch, G.TW, 0, 2, t1, t2, Ab)
    Ar = Ab.t[:, 0:nch, 0, :]
    Ai = Ab.t[:, 0:nch, 1, :]
    P.op("pe", lambda e: e.matmul(PXr.t[:, 0:nch, :], DF.t[:, 0, :], Ar, start=True, stop=False), r=[DF, Ab], w=[PXr])
    P.op("pe", lambda e: e.matmul(PXr.t[:, 0:nch, :], DF.t[:, 3, :], Ai, start=False, stop=True), r=[DF, Ab], w=[PXr])
    P.op("pe", lambda e: e.matmul(PXi.t[:, 0:nch, :], DF.t[:, 1, :], Ar, start=True, stop=False), r=[DF, Ab], w=[PXi])
    P.op("pe", lambda e: e.matmul(PXi.t[:, 0:nch, :], DF.t[:, 0, :], Ai, start=False, stop=True), r=[DF, Ab], w=[PXi])


def phase_E_filters(P, G, io, l, KS, KSW):
    kc = P.dram("hy_kc%d" % l, [512, NFFT], BF16)
    kc_tok = [P.token("kc%d" % q) for q in range(8)]
    with P.scope():
        ob = [P.sb("hy_ob0", [128, NL], BF16)] * 2

        def emit(q, k_):
            g, half = q // 2, q % 2
            o, dr = g // 2, g % 2
            o_ = ob[q % 2]
            rows = slice(o * 256 + half * 128, o * 256 + (half + 1) * 128)
            if dr == 0:
                P.op("pool", lambda e: e.tensor_copy(o_.t[:], k_.t[:]), r=[k_], w=[o_])
                P.dma("sp", kc.t[rows, 0:NL], o_.t[:], r=[o_], w=[kc_tok[q]])
            else:
                kp = k_.t[:].ap[0][0]
                P.op("pool", lambda e: e.memset(o_.t[:, 0:1], 0.0), w=[o_])
                P.op("pool", lambda e: e.tensor_copy(o_.t[:, 1:NL], mkap(k_.t, NL - 1, [[kp, 128], [-1, NL - 1]])), r=[k_], w=[o_])
                P.dma("sp", kc.t[rows, NL:2 * NL], o_.t[:], r=[o_], w=[kc_tok[q]])
        hy_filters(P, G, io, l, NL, io["zT"], emit, ob[0])
    G.KS_tok = [P.token("KS%d" % i) for i in range(32)]
    with P.scope():
        kin = [P.sb("hyf_kin%d" % i, [128, 16, 128], BF16) for i in range(2)]
        PA = P.ps("hyf_PA", [128, 4, 2, 128], F32)
        PXr = P.ps("hyf_PXr", [128, 4, 128], F32)
        PXi = P.ps("hyf_PXi", [128, 4, 128], F32)
        t1 = P.sb("hyf_t1", [128, 4, 2, 128], F32); t2 = P.sb("hyf_t2", [128, 4, 2, 128], F32)
        Ab = P.sb("hyf_Ab", [128, 4, 2, 128], BF16)
        ks = [P.sb("hyf_ks%d" % i, [128, 16, 2, 128], F32) for i in range(2)]
        ksw = [P.sb("hyf_ksw%d" % i, [128, 16, 2, 128], F32) for i in range(2)]
        kcv = kc.t.rearrange("r (a b) -> a r b", b=128)
        for g16 in range(32):
            ki = kin[g16 % 2]; ks_ = ks[g16 % 2]; ksw_ = ksw[g16 % 2]
            P.dma("sp" if g16 % 2 == 0 else "act", ki.t[:], kcv[:, g16 * 16:(g16 + 1) * 16, :], r=kc_tok, w=[ki])
            for g4 in range(4):
                fft_fwd_group(P, G, lambda c, g4=g4, ki=ki: (ki.t[:, g4 * 4 + c, :], [ki]), 128, 4, PA, PXr, PXi, t1, t2, Ab)
                sl = slice(g4 * 4, g4 * 4 + 4)
                sc = 1.0 / NFFT
                P.op("act", lambda e, sl=sl, ks_=ks_: e.activation(ks_.t[:, sl, 0, :], PXr.t[:], AF.Identity, scale=sc), r=[PXr], w=[ks_])
                P.op("act", lambda e, sl=sl, ks_=ks_: e.activation(ks_.t[:, sl, 1, :], PXi.t[:], AF.Identity, scale=sc), r=[PXi], w=[ks_])
                P.op("act", lambda e, sl=sl, ksw_=ksw_: e.activation(ksw_.t[:, sl, 0, :], PXi.t[:], AF.Identity, scale=-sc), r=[PXi], w=[ksw_])
                P.op("act", lambda e, sl=sl, ksw_=ksw_: e.activation(ksw_.t[:, sl, 1, :], PXr.t[:], AF.Identity, scale=sc), r=[PXr], w=[ksw_])
            P.dma("sp", KS.t[:, g16 * 16:(g16 + 1) * 16], ks_.t[:], r=[ks_], w=[G.KS_tok[g16]])
            P.dma("act", KSW.t[:, g16 * 16:(g16 + 1) * 16], ksw_.t[:], r=[ksw_], w=[G.KS_tok[g16]])


def phase_E_latent(P, G, io, l, uF, ymix, KS, KSW):
    hyU = P.dram("hyU%d" % l, [768, NL], F32)
    hyU_tok = [P.token("hyU%d" % i) for i in range(6)]
    hyY = P.dram("hyY%d" % l, [256, NL], BF16)
    hyY_tok = [P.token("hyY")]
    with P.scope():
        cw = P.sb("hy_cw", [128, 24], F32)
        P.dma("sp", cw.t[:], io["hy_cw"][l], w=[cw])
        for ch in range(6):
            with P.scope():
                xi = P.sb("hyc_x", [128, NL + 2], F32)
                xo = P.sb("hyc_o", [128, NL], F32)
                P.op("pool", lambda e: e.memset(xi.t[:, 0:1], 0.0), w=[xi])
                P.op("pool", lambda e: e.memset(xi.t[:, NL + 1:NL + 2], 0.0), w=[xi])
                P.dma("sp", xi.t[:, 1:NL + 1], uF.t[R_HY + ch * 128:R_HY + (ch + 1) * 128, 256:NS], r=G.uF_tok, w=[xi])
                P.op("dve", lambda e: e.tensor_scalar(xo.t[:], xi.t[:, 0:NL], cw.t[:, ch * 3:ch * 3 + 1], cw.t[:, 18 + ch:19 + ch], ALU.mult, ALU.add), r=[xi, cw], w=[xo])
                for k in (1, 2):
                    P.op("dve", lambda e, k=k: e.scalar_tensor_tensor(xo.t[:], xi.t[:, k:k + NL], cw.t[:, ch * 3 + k:ch * 3 + k + 1], xo.t[:], ALU.mult, ALU.add), r=[xi, cw, xo], w=[xo])
                P.dma("sp", hyU.t[ch * 128:(ch + 1) * 128, :], xo.t[:], r=[xo], w=[hyU_tok[ch]])
    with P.scope():
        skp = P.sb("hy_skip", [64, 512], F32)
        P.dma("sp", skp.t[:], io["hy_skip"][l].broadcast_to([64, 512]), w=[skp])
        uv = hyU.t.rearrange("(b c) (a n) -> a b c n", b=3, n=128)
        yv = hyY.t.rearrange("c (a n) -> a c n", n=128)
        NG = 8
        xin = [P.sb("hye_xin%d" % i, [64, 3, NG, 128], F32) for i in range(2)]
        yo = [P.sb("hye_yo%d" % i, [64, NG, 128], BF16) for i in range(2)]
        kt = [P.sb("hye_k%d" % i, [128, 2, NG, 2, 128], F32) for i in range(2)]
        kwt = [P.sb("hye_kw%d" % i, [128, 2, NG, 2, 128], F32) for i in range(2)]
        DF = G.DF

        class HS:
            pass
        sets = []
        for si in range(2):
            h_ = HS()
            h_.vb = P.sb("hye_vb%d" % si, [64, 4, 128], BF16)
            h_.y1f = P.sb("hye_y1f%d" % si, [64, 4, 128], F32)
            h_.y1b = P.sb("hye_y1b%d" % si, [64, 4, 128], BF16)
            h_.tt = P.sb("hye_tt%d" % si, [64, 4, 128], F32)
            h_.t1 = P.sb("hye_t1%d" % si, [128, 4, 2, 128], F32)
            h_.t2 = P.sb("hye_t2%d" % si, [128, 4, 2, 128], F32)
            h_.Ab = P.sb("hye_Ab%d" % si, [128, 4, 2, 128], BF16)
            h_.Yb = P.sb("hye_Yb%d" % si, [128, 4, 2, 128], BF16)
            h_.Bb = P.sb("hye_Bb%d" % si, [128, 4, 2, 128], BF16)
            h_.P2 = P.ps("hye_P2%d" % si, [128, 4, 2, 128], F32)
            h_.PXr = P.ps("hye_PXr%d" % si, [128, 4, 128], F32)
            h_.PXi = P.ps("hye_PXi%d" % si, [128, 4, 128], F32)
            sets.append(h_)

        def chain(h_, x_, k_, kw_, yo_, c0, g4):
            sl = slice(g4 * 4, g4 * 4 + 4)
            P2, PXr, PXi, t1, t2 = h_.P2, h_.PXr, h_.PXi, h_.t1, h_.t2
            PY = P2.t[0:64, 0:2].rearrange("p a b c -> p (a b) c")
            P.op("pool", lambda e: e.tensor_copy(h_.vb.t[:], x_.t[:, 0, sl, :]), r=[x_], w=[h_.vb])
            yield
            for o in range(2):
                src = h_.vb if o == 0 else h_.y1b
                for c in range(4):
                    P.op("pe", lambda e, c=c: e.matmul(P2.t[:, c].rearrange("p a b -> p (a b)"), src.t[:, c, :], DF.t[0:64, 0:2, :].rearrange("p a b -> p (a b)"), start=True, stop=True),
                         r=[src, DF], w=[P2])
                yield
                cplx_mul_tab(P, G, P2, 4, G.TW, 0, 2, t1, t2, h_.Ab)
                yield
                Ar = h_.Ab.t[:, :, 0, :]
                Ai = h_.Ab.t[:, :, 1, :]
                P.op("pe", lambda e: e.matmul(PXr.t[:], DF.t[:, 0, :], Ar, start=True, stop=False), r=[DF, h_.Ab], w=[PXr])
                P.op("pe", lambda e: e.matmul(PXr.t[:], DF.t[:, 3, :], Ai, start=False, stop=True), r=[DF, h_.Ab], w=[PXr])
                P.op("pe", lambda e: e.matmul(PXi.t[:], DF.t[:, 1, :], Ar, start=True, stop=False), r=[DF, h_.Ab], w=[PXi])
                P.op("pe", lambda e: e.matmul(PXi.t[:], DF.t[:, 0, :], Ai, start=False, stop=True), r=[DF, h_.Ab], w=[PXi])
                yield
                XrB = mkap(PXr.t, 0, [[PXr.t[:].ap[0][0], 128], [128, 4], [0, 2], [1, 128]])
                XiB = mkap(PXi.t, 0, [[PXi.t[:].ap[0][0], 128], [128, 4], [0, 2], [1, 128]])
                P.op("dve", lambda e, o=o: e.tensor_tensor(t1.t[:], XrB, k_.t[:, o, sl], ALU.mult), r=[PXr, k_], w=[t1])
                P.op("dve", lambda e, o=o: e.tensor_tensor(t2.t[:], XiB, kw_.t[:, o, sl], ALU.mult), r=[PXi, kw_], w=[t2])
                P.op("pool", lambda e: e.tensor_tensor(h_.Yb.t[:], t1.t[:], t2.t[:], ALU.add), r=[t1, t2], w=[h_.Yb])
                yield
                for c in range(4):
                    P.op("pe", lambda e, c=c: e.matmul(P2.t[:, c].rearrange("p a b -> p (a b)"), h_.Yb.t[:, c, 0, :], DF.t[:, 2:4, :].rearrange("p a b -> p (a b)"),
                                                      start=True, stop=False), r=[h_.Yb, DF], w=[P2])
                    P.op("pe", lambda e, c=c: e.matmul(P2.t[:, c].rearrange("p a b -> p (a b)"), h_.Yb.t[:, c, 1, :], DF.t[:, 1:3, :].rearrange("p a b -> p (a b)"),
                                                      start=False, stop=True), r=[h_.Yb, DF], w=[P2])
                yield
                cplx_mul_tab(P, G, P2, 4, G.TW, 0, 3, t1, t2, h_.Bb)
                yield
                P.op("pe", lambda e: e.matmul(PY, DF.t[:, 0, 0:64], h_.Bb.t[:, :, 0, :], start=True, stop=False), r=[DF, h_.Bb], w=[P2])
                P.op("pe", lambda e: e.matmul(PY, DF.t[:, 1, 0:64], h_.Bb.t[:, :, 1, :], start=False, stop=True), r=[DF, h_.Bb], w=[P2])
                yield
                zsrc = (x_, x_.t[:, 0, sl, :]) if o == 0 else (h_.y1f, h_.y1f.t[:])
                skb = mkap(skp.t, o * 256 + c0 + g4 * 4, [[512, 64], [1, 4], [0, 128]])
                P.op("pool", lambda e, zsrc=zsrc, skb=skb: e.tensor_tensor(h_.tt.t[:], zsrc[1], skb, ALU.mult), r=[zsrc[0], skp], w=[h_.tt])
                P.op("dve", lambda e: e.tensor_tensor(h_.tt.t[:], PY, h_.tt.t[:], ALU.add), r=[P2, h_.tt], w=[h_.tt])
                if o == 0:
                    P.op("pool", lambda e: e.tensor_tensor(h_.y1f.t[:], h_.tt.t[:], x_.t[:, 1, sl, :], ALU.mult), r=[h_.tt, x_], w=[h_.y1f])
                    P.op("act", lambda e: e.activation(h_.y1b.t[:], h_.y1f.t[:], AF.Identity), r=[h_.y1f], w=[h_.y1b])
                else:
                    P.op("pool", lambda e: e.tensor_tensor(yo_.t[:, sl, :], h_.tt.t[:], x_.t[:, 2, sl, :], ALU.mult), r=[h_.tt, x_], w=[yo_])
                yield

        for gi in range(256 // NG):
            c0 = gi * NG
            x_ = xin[gi % 2]; k_ = kt[gi % 2]; kw_ = kwt[gi % 2]; yo_ = yo[gi % 2]
            for b in range(3):
                P.dma("sp" if b != 1 else "act", x_.t[:, b], uv[:, b, c0:c0 + NG, :], r=hyU_tok, w=[x_])
            for o in range(2):
                P.dma("sp", k_.t[:, o], KS.t[:, o * 256 + c0:o * 256 + c0 + NG], r=G.KS_tok, w=[k_])
                P.dma("act", kw_.t[:, o], KSW.t[:, o * 256 + c0:o * 256 + c0 + NG], r=G.KS_tok, w=[kw_])
            gens = [chain(sets[g4], x_, k_, kw_, yo_, c0, g4) for g4 in range(2)]
            live = list(gens)
            while live:
                for g_ in list(live):
                    try:
                        next(g_)
                    except StopIteration:
                        live.remove(g_)
            P.dma("sp", yv[:, c0:c0 + NG, :], yo_.t[:], r=[yo_], w=hyY_tok)
    with P.scope():
        idx = P.sb("idx_own_e", [128, 8], U32)
        P.dma("sp", idx.t[:], io["idx_own"], w=[idx])
        for cc in range(2):
            g = P.sb("hye_g%d" % cc, [128, OWN], BF16)
            P.op("pool", lambda e, g=g, cc=cc: e.indirect_dma_start(
                out=g.t[:], out_offset=None, in_=hyY.t.rearrange("c (q s) -> (c q) s", q=4),
                in_offset=bass.IndirectOffsetOnAxis(ap=idx.t[:, cc:cc + 1], axis=0)), r=hyY_tok + [idx], w=[g], dma=True)
            P.dma("sp", ymix.t[768 + cc * 128:768 + (cc + 1) * 128, 256:NO], g.t[:], r=[g], w=[G.ymix_tok[6 + cc]])


def phase_E_ctx(P, G, io, l, uF, ymix):
    Lc = NCTX
    with P.scope():
        F5 = P.sb("F512", [128, 4, 2, 512], BF16)
        load_cast(P, F5, lambda c0, n: F5.t[:].rearrange("p a b c -> p (a b c)")[:, c0:c0 + n], io["dft512"].rearrange("p a b c -> p (a b c)"), 4096)
        idn = P.sb("ident", [128, 128], F32)
        P.dma("sp", idn.t[:], io["ident"], w=[idn])
        kcf = P.sb("hc_kcf", [128, 4, 512], F32)
        junk = P.sb("hc_junk", [128, Lc], BF16)

        def emit(q, k_):
            g, half = q // 2, q % 2
            o, dr = g // 2, g % 2
            rc = o * 2 + half
            if dr == 0:
                P.op("pool", lambda e: e.tensor_copy(kcf.t[:, rc, 0:Lc], k_.t[:]), r=[k_], w=[kcf])
            else:
                kp = k_.t[:].ap[0][0]
                P.op("pool", lambda e: e.memset(kcf.t[:, rc, Lc:Lc + 1], 0.0), w=[kcf])
                P.op("pool", lambda e: e.tensor_copy(kcf.t[:, rc, Lc + 1:2 * Lc], mkap(k_.t, Lc - 1, [[kp, 128], [-1, Lc - 1]])), r=[k_], w=[kcf])
        hy_filters(P, G, io, l, Lc, io["zTc"], emit, junk)
        kct = P.sb("hc_kct", [128, 4, 512], BF16)
        Ksp = P.sb("hc_Ksp", [128, 4, 2, 512], F32)
        Kspw = P.sb("hc_Kspw", [128, 4, 2, 512], F32)
        xct = P.sb("hc_xct", [128, 2, 768], F32)
        with P.scope():
            pt = [P.ps("hc_pt%d" % i, [128, 512], F32) for i in range(4)]
            pi = 0
            for nc_ in range(4):
                p_ = pt[pi % 4]; pi += 1
                for rc in range(4):
                    P.op("pe", lambda e, p_=p_, rc=rc, nc_=nc_: e.matmul(p_.t[:, rc * 128:(rc + 1) * 128], kcf.t[:, rc, nc_ * 128:(nc_ + 1) * 128], idn.t[:], start=True, stop=True), r=[kcf, idn], w=[p_])
                P.op("act", lambda e, p_=p_, nc_=nc_: e.activation(kct.t[:, nc_, :], p_.t[:], AF.Identity), r=[p_], w=[kct])
            for kc in range(4):
                for ri in range(2):
                    p_ = pt[pi % 4]; pi += 1
                    for nc_ in range(4):
                        P.op("pe", lambda e, p_=p_, kc=kc, ri=ri, nc_=nc_: e.matmul(p_.t[:], F5.t[:, nc_, ri, kc * 128:(kc + 1) * 128], kct.t[:, nc_, :], start=(nc_ == 0), stop=(nc_ == 3)),
                             r=[F5, kct], w=[p_])
                    sc = 1.0 / 512.0
                    P.op("act", lambda e, p_=p_, kc=kc, ri=ri: e.activation(Ksp.t[:, kc, ri, :], p_.t[:], AF.Identity, scale=sc), r=[p_], w=[Ksp])
                    P.op("dve", lambda e, kc=kc, ri=ri: e.tensor_scalar_mul(Kspw.t[:, kc, 1 - ri, :], Ksp.t[:, kc, ri, :], (1.0 if ri == 0 else -1.0)), r=[Ksp], w=[Kspw])
            cw = P.sb("hc_cw", [128, 24], F32)
            P.dma("sp", cw.t[:], io["hy_cw"][l], w=[cw])
            xi = P.sb("hc_xi", [128, 6, Lc + 2], F32)
            xo = P.sb("hc_xo", [128, 6, Lc], F32)
            P.op("pool", lambda e: e.memset(xi.t[:], 0.0), w=[xi])
            P.dma("sp", xi.t[:, :, 1:Lc + 1], uF.t.rearrange("(c p) s -> p c s", p=128)[:, 5:11, 0:Lc], r=G.uF_tok, w=[xi])
            for ch in range(6):
                P.op("dve", lambda e, ch=ch: e.tensor_scalar(xo.t[:, ch, :], xi.t[:, ch, 0:Lc], cw.t[:, ch * 3:ch * 3 + 1], cw.t[:, 18 + ch:19 + ch], ALU.mult, ALU.add), r=[xi, cw], w=[xo])
                for k in (1, 2):
                    P.op("dve", lambda e, ch=ch, k=k: e.scalar_tensor_tensor(xo.t[:, ch, :], xi.t[:, ch, k:k + Lc], cw.t[:, ch * 3 + k:ch * 3 + k + 1], xo.t[:, ch, :], ALU.mult, ALU.add),
                         r=[xi, cw, xo], w=[xo])
            for nc_ in range(2):
                for hh in range(2):
                    p_ = pt[pi % 4]; pi += 1
                    for c3 in range(3):
                        ch = hh * 3 + c3
                        P.op("pe", lambda e, p_=p_, c3=c3, ch=ch, nc_=nc_: e.matmul(p_.t[:, c3 * 128:(c3 + 1) * 128], xo.t[:, ch, nc_ * 128:(nc_ + 1) * 128], idn.t[:], start=True, stop=True), r=[xo, idn], w=[p_])
                    P.op("act", lambda e, p_=p_, nc_=nc_, hh=hh: e.activation(xct.t[:, nc_, hh * 384:(hh + 1) * 384], p_.t[:, 0:384], AF.Identity), r=[p_], w=[xct])
        with P.scope():
            skp = P.sb("hc_skip", [128, 512], F32)
            P.dma("sp", skp.t[:], io["hy_skip"][l].broadcast_to([128, 512]), w=[skp])
            px = [P.ps("hc_px%d" % i, [128, 2, 256], F32) for i in range(2)]
            py = [P.ps("hc_py%d" % i, [128, 512], F32) for i in range(2)]
            Ysp = P.sb("hc_Ysp", [128, 4, 2, 256], BF16)
            z16 = P.sb("hc_z16", [128, 2, 256], BF16)
            t1 = P.sb("hc_t1", [128, 2, 256], F32); t2 = P.sb("hc_t2", [128, 2, 256], F32)
            y1 = P.sb("hc_y1", [128, 2, 256], F32)
            y2 = P.sb("hc_y2", [128, 2, 256], F32)
            tt = P.sb("hc_tt", [128, 256], F32)
            for o in range(2):
                zb = xct if o == 0 else y1
                zap = (lambda nc_: xct.t[:, nc_, 0:256]) if o == 0 else (lambda nc_: y1.t[:, nc_, :])
                for nc_ in range(2):
                    P.op("act", lambda e, nc_=nc_, zap=zap: e.activation(z16.t[:, nc_, :], zap(nc_), AF.Identity), r=[zb], w=[z16])
                for kc in range(4):
                    p_ = px[kc % 2]
                    for ri in range(2):
                        for nc_ in range(2):
                            P.op("pe", lambda e, p_=p_, ri=ri, nc_=nc_, kc=kc: e.matmul(p_.t[:, ri, :], F5.t[:, nc_, ri, kc * 128:(kc + 1) * 128], z16.t[:, nc_, :], start=(nc_ == 0), stop=(nc_ == 1)),
                                 r=[F5, z16], w=[p_])
                    pp_ = p_.t[:].ap[0][0]
                    XrB = mkap(p_.t, 0, [[pp_, 128], [0, 2], [1, 256]])
                    XiB = mkap(p_.t, 256, [[pp_, 128], [0, 2], [1, 256]])
                    P.op("dve", lambda e, XrB=XrB, kc=kc, o=o: e.tensor_tensor(t1.t[:], XrB, Ksp.t[:, kc, :, o * 256:(o + 1) * 256], ALU.mult), r=[p_, Ksp], w=[t1])
                    P.op("dve", lambda e, XiB=XiB, kc=kc, o=o: e.tensor_tensor(t2.t[:], XiB, Kspw.t[:, kc, :, o * 256:(o + 1) * 256], ALU.mult), r=[p_, Kspw], w=[t2])
                    P.op("pool", lambda e, kc=kc: e.tensor_tensor(Ysp.t[:, kc], t1.t[:], t2.t[:], ALU.add), r=[t1, t2], w=[Ysp])
                for nc_ in range(2):
                    p_ = py[nc_]
                    i = 0
                    for kc in range(4):
                        for ri in range(2):
                            P.op("pe", lambda e, p_=p_, kc=kc, ri=ri, nc_=nc_, i=i: e.matmul(p_.t[:, 0:256], F5.t[:, kc, ri, nc_ * 128:(nc_ + 1) * 128], Ysp.t[:, kc, ri, :], start=(i == 0), stop=(i == 7)),
                                 r=[F5, Ysp], w=[p_])
                            i += 1
                    P.op("pool", lambda e, nc_=nc_, o=o, zap=zap: e.tensor_tensor(tt.t[:], zap(nc_), skp.t[:, o * 256:(o + 1) * 256], ALU.mult), r=[zb, skp], w=[tt])
                    P.op("dve", lambda e, p_=p_: e.tensor_tensor(tt.t[:], p_.t[:, 0:256], tt.t[:], ALU.add), r=[p_, tt], w=[tt])
                    dst = y1 if o == 0 else y2
                    P.op("pool", lambda e, nc_=nc_, o=o, dst=dst: e.tensor_tensor(dst.t[:, nc_, :], tt.t[:], xct.t[:, nc_, 256 * (o + 1):256 * (o + 2)], ALU.mult), r=[tt, xct], w=[dst])
            yb = P.sb("hc_yb", [128, 2, 256], BF16)
            for cc in range(2):
                p_ = py[cc]
                for nc_ in range(2):
                    P.op("pe", lambda e, p_=p_, cc=cc, nc_=nc_: e.matmul(p_.t[:, nc_ * 128:(nc_ + 1) * 128], y2.t[:, nc_, cc * 128:(cc + 1) * 128], idn.t[:], start=True, stop=True), r=[y2, idn], w=[p_])
                P.op("act", lambda e, p_=p_, cc=cc: e.activation(yb.t[:, cc, :], p_.t[:, 0:256], AF.Identity), r=[p_], w=[yb])
                P.dma("sp", ymix.t[768 + cc * 128:768 + (cc + 1) * 128, 0:256], yb.t[:, cc, :], r=[yb], w=[G.ymix_tok[6 + cc]])


def prep_tail(inp, S):
    f = lambda a: np.ascontiguousarray(np.asarray(a, np.float32))
    S["w_out"] = f(inp["w_out"])
    wq = f(inp["peer_wq"])
    Lr = wq.shape[0]
    S["wqT_r"] = np.ascontiguousarray(wq.reshape(Lr, D, 16, 128).transpose(0, 3, 2, 1))
    S["keysT_r"] = np.ascontiguousarray(f(inp["peer_keys"]).transpose(0, 3, 1, 2))
    pu = f(inp["peer_u"]); pv = f(inp["peer_v"])
    for l in range(Lr):
        S["peer_u%d" % l] = pu[l]
        S["peer_v%d" % l] = pv[l]
    S["final_gT"] = chunkT(inp["final_g"], 8)
    S["iota16"] = np.ascontiguousarray(np.tile(np.arange(16, dtype=np.float32)[None, :], (128, 1)))


IO_SHAPES.update({"w_out": [2, D, D], "wqT_r": [2, 128, 16, D], "keysT_r": [2, 128, 2, 128], "peer_u0": [16384, D], "peer_v0": [16384, D], "peer_u1": [16384, D], "peer_v1": [16384, D],
                  "final_gT": [128, 8], "iota16": [128, 16]})


def phase_F(P, G, io, l, need_ctx, ymix, x1T):
    ot = own_tiles(need_ctx)
    c0 = 0 if need_ctx else 256
    SRC = G.src
    x1v = x1T.t.rearrange("(k p) s -> p k s", p=128)
    G.x1_tok = [P.token("x1T%d" % i) for i in range(len(ot))]
    with P.scope():
        wo = P.sb("wo_bf", [128, 8, D], BF16)
        for k in range(8):
            load_cast(P, wo, lambda c0_, n, k=k: wo.t[:, k, c0_:c0_ + n], io["w_out"][l, k * 128:(k + 1) * 128, :], D)
        ym = P.sb("ym", [128, 8, NO], BF16)
        P.dma("sp", ym.t[:, :, c0:NO], ymix.t.rearrange("(k p) s -> p k s", p=128)[:, :, c0:NO], r=G.ymix_tok, w=[ym])
        xt = [P.sb("f_xt%d" % i, [128, 8, 512], F32) for i in range(2)]
        xo = [P.sb("f_xo%d" % i, [128, 8, 512], F32) for i in range(2)]
        pp = [P.ps("f_pp%d" % i, [128, 512], F32) for i in range(3)]
        pi = 0
        for ti, (s0, n, j) in enumerate(ot):
            x_ = xt[ti % 2]; o_ = xo[ti % 2]
            P.dma("act", x_.t[:, :, :n], (SRC["ctx"](n) if j else SRC["own"](s0 - 256, n)), r=SRC["toks"], w=[x_])
            for dm in range(8):
                p_ = pp[pi % 3]; pi += 1
                for k in range(8):
                    P.op("pe", lambda e, p_=p_, k=k, dm=dm, s0=s0, n=n: e.matmul(p_.t[:, :n], wo.t[:, k, dm * 128:(dm + 1) * 128], ym.t[:, k, s0:s0 + n], start=(k == 0), stop=(k == 7)),
                         r=[wo, ym], w=[p_])
                P.op("dve", lambda e, p_=p_, dm=dm, n=n, j=j, x_=x_, o_=o_: e.scalar_tensor_tensor(
                    o_.t[:, dm, :n], p_.t[:, :n], G.mod.t[:, 16 + dm, j:j + 1], x_.t[:, dm, :n], ALU.mult, ALU.add), r=[p_, G.mod, x_], w=[o_])
            P.dma("sp", x1v[:, :, s0:s0 + n], o_.t[:, :, :n], r=[o_], w=[G.x1_tok[ti]])


def cast_peer_tables(P, G, io, l):
    G.Ub = P.dram("peerUb%d" % l, [16384, D], BF16)
    G.Vb = P.dram("peerVb%d" % l, [16384, D], BF16)
    G.tab_tok = [P.token("ptab")]
    with P.scope():
        st = [P.sb("pc_st%d" % i, [128, 2, D], F32) for i in range(2)]
        ob = [P.sb("pc_ob%d" % i, [128, 2, D], BF16) for i in range(2)]
        i = 0
        for nm, dst in (("peer_u%d" % l, G.Ub), ("peer_v%d" % l, G.Vb)):
            src = io[nm].rearrange("(p r) d -> p r d", p=128)
            dv = dst.t.rearrange("(p r) d -> p r d", p=128)
            for r0 in range(0, 128, 2):
                def piece(i=i, src=src, dv=dv, r0=r0):
                    s_ = st[i % 2]; o_ = ob[i % 2]
                    P.dma("sp", s_.t[:], src[:, r0:r0 + 2, :], w=[s_])
                    eng = ("pool", "dve", "pool")[i % 3]
                    P.op(eng, lambda e: e.tensor_copy(o_.t[:], s_.t[:]), r=[s_], w=[o_])
                    P.dma("pool", dv[:, r0:r0 + 2, :], o_.t[:], r=[o_], w=G.tab_tok)
                G.bg.append(piece)
                i += 1
        yield


def phase_G(P, G, io, l, need_ctx, final, x1T, xoT, coT, tile_limit=None):
    x1v = x1T.t.rearrange("(k p) s -> p k s", p=128)
    xov = xoT.t.rearrange("(k p) s -> p k s", p=128)
    cov = coT.t.rearrange("(k p) s -> p k s", p=128) if coT is not None else None
    NEG = -1.0e30
    with P.scope():
        idn = P.sb("g_ident", [128, 128], F32)
        P.dma("sp", idn.t[:], io["ident"], w=[idn])
        io16 = P.sb("g_iota16", [128, 16], F32)
        P.dma("sp", io16.t[:], io["iota16"], w=[io16])
        fg = P.sb("g_fg", [128, 8], F32)
        P.dma("sp", fg.t[:], io["final_gT"], w=[fg])
        Wk = P.sb("g_Wk", [128, 8, 2048], F32)
        with P.scope():
            kT = P.sb("g_keysT", [128, 2, 128], F32)
            P.dma("sp", kT.t[:], io["keysT_r"][l], w=[kT])
            wqs = [P.sb("g_wq%d" % i, [128, D], F32) for i in range(2)]
            pw = [P.ps("g_pw%d" % i, [128, 512], F32) for i in range(8)]
            for g4 in range(4):
                for gg in range(4):
                    g = g4 * 4 + gg
                    wq_ = wqs[g % 2]
                    P.dma("sp" if g % 2 == 0 else "act", wq_.t[:], io["wqT_r"][l, :, g, :], w=[wq_])
                    for dc in range(8):
                        P.op("pe", lambda e, dc=dc, gg=gg, g=g, wq_=wq_: e.matmul(pw[dc].t[:, gg * 128:(gg + 1) * 128], wq_.t[:, dc * 128:(dc + 1) * 128], kT.t[:, g % 2, :], start=True, stop=True),
                             r=[wq_, kT], w=[pw[dc]])
                for dc in range(8):
                    if dc % 2:
                        P.op("act", lambda e, dc=dc, g4=g4: e.activation(Wk.t[:, dc, g4 * 512:(g4 + 1) * 512], pw[dc].t[:], AF.Identity), r=[pw[dc]], w=[Wk])
                    else:
                        P.op("dve", lambda e, dc=dc, g4=g4: e.tensor_copy(Wk.t[:, dc, g4 * 512:(g4 + 1) * 512], pw[dc].t[:]), r=[pw[dc]], w=[Wk])
        NB = 8
        Dg = P.sb("g_Dg", [128, 64, 128], BF16)
        gU = [P.sb("g_gU%d" % i, [128, D], BF16) for i in range(NB)]
        gV = [P.sb("g_gV%d" % i, [128, D], BF16) for i in range(NB)]
        xt = P.sb("g_xt", [128, 8, 128], F32)
        sq = P.sb("g_sq", [128, 8, 128], BF16)
        xn = P.sb("g_xn", [128, 8, 128], F32)
        hx = P.sb("g_hx", [128, 8, 128], F32)
        rs = P.sb("g_rs", [128, 128], F32)
        hxt = P.sb("g_hxt", [128, D], F32)
        junk = P.sb("g_junk", [128, D], BF16)
        sc = P.sb("g_sc", [128, 16, 128], F32)
        sc2 = P.sb("g_sc2", [128, 16, 128], F32)
        v16 = P.sb("g_v16", [128, 16, 16], F32)
        i16u = P.sb("g_i16u", [128, 16, 16], U32)
        if16 = P.sb("g_if16", [128, 16, 16], F32)
        cand = P.sb("g_cand", [128, 8, 256], F32)
        cand2 = sc2
        cs16 = P.sb("g_cs16", [128, 8, 16], F32)
        cp16 = P.sb("g_cp16", [128, 8, 16], U32)
        au = P.sb("g_au", [128, 8, 16], U32); bu = P.sb("g_bu", [128, 8, 16], U32)
        af = P.sb("g_af", [128, 8, 16], F32); bf = P.sb("g_bf", [128, 8, 16], F32)
        eq = P.sb("g_eq", [128, 8, 16, 16], F32)
        s1 = P.sb("g_s1", [128, 8, 16], F32); s2_ = P.sb("g_s2", [128, 8, 16], F32)
        eidu = P.sb("g_eidu", [128, 128], U32)
        gate = P.sb("g_gate", [128, 8, 16], F32)
        gsum = P.sb("g_gsum", [128, 8], F32)
        dots = P.sb("g_dots", [128, 128], F32)
        wts = P.sb("g_wts", [128, 128], F32)
        acc = P.sb("g_acc", [128, D], F32)
        xo = P.sb("g_xo", [128, 8, 128], F32)
        yo = P.sb("g_yo", [128, 8, 128], F32)
        pss = P.ps("g_pss", [128, 512], F32)
        psc = [P.ps("g_psc%d" % i, [128, 512], F32) for i in range(4)]
        ptr = [P.ps("g_ptr%d" % i, [128, 512], F32) for i in range(2)]
        ntile = NO // 128
        t0 = 0 if need_ctx else 2
        out_tok = [P.token("xo%d" % i) for i in range(ntile)]
        G.out_tok = out_tok
        u_tab = G.Ub.t
        v_tab = G.Vb.t
        for tt in range(t0, ntile if tile_limit is None else tile_limit):
            o0 = tt * 128
            j = 1 if o0 < 256 else 0
            P.dma("sp", xt.t[:], x1v[:, :, o0:o0 + 128], r=G.x1_tok, w=[xt])
            P.op("act", lambda e: e.activation(sq.t[:], xt.t[:], AF.Square), r=[xt], w=[sq])
            for k in range(8):
                P.op("pe", lambda e, k=k: e.matmul(pss.t[:, 0:128], G.ones_bf.t[:, :], sq.t[:, k, :], start=(k == 0), stop=(k == 7)), r=[sq, G.ones_bf], w=[pss])
            P.op("act", lambda e: e.activation(rs.t[:], pss.t[:, 0:128], AF.Sqrt, bias=G.eps.t[:, 0:1], scale=1.0 / D), r=[pss, G.eps], w=[rs])
            P.op("dve", lambda e: e.reciprocal(rs.t[:], rs.t[:]), r=[rs], w=[rs])
            P.op("dve", lambda e: e.tensor_tensor(xn.t[:], xt.t[:], ins_bc(rs.t[:], 1, 8), ALU.mult), r=[xt, rs], w=[xn])
            for k in range(8):
                P.op("act", lambda e, k=k, j=j: e.activation(hx.t[:, k, :], xn.t[:, k, :], AF.Identity, scale=G.s2.t[:, k, j:j + 1], bias=G.mod.t[:, 24 + k, j:j + 1]),
                     r=[xn, G.s2, G.mod], w=[hx])
            for c4 in range(4):
                for k in range(8):
                    P.op("pe", lambda e, c4=c4, k=k: e.matmul(psc[c4].t[:], hx.t[:, k, :], Wk.t[:, k, c4 * 512:(c4 + 1) * 512], start=(k == 0), stop=(k == 7)), r=[hx, Wk], w=[psc[c4]])
                P.op("act", lambda e, c4=c4: e.activation(sc.t[:, c4 * 4:(c4 + 1) * 4, :].rearrange("p a b -> p (a b)"), psc[c4].t[:], AF.Identity), r=[psc[c4]], w=[sc])
            for h2 in range(2):
                for k4 in range(4):
                    k = h2 * 4 + k4
                    P.op("pe", lambda e, k=k, k4=k4, h2=h2: e.matmul(ptr[h2].t[:, k4 * 128:(k4 + 1) * 128], hx.t[:, k, :], idn.t[:], start=True, stop=True), r=[hx, idn], w=[ptr[h2]])
                P.op("act", lambda e, h2=h2: e.activation(hxt.t[:, h2 * 512:(h2 + 1) * 512], ptr[h2].t[:], AF.Identity), r=[ptr[h2]], w=[hxt])
            for g in range(16):
                P.op("dve", lambda e, g=g: e.max(v16.t[:, g, 0:8], sc.t[:, g, :]), r=[sc], w=[v16])
                P.op("dve", lambda e, g=g: e.max_index(i16u.t[:, g, 0:8], v16.t[:, g, 0:8], sc.t[:, g, :]), r=[sc, v16], w=[i16u])
                P.op("dve", lambda e, g=g: e.match_replace(sc2.t[:, g, :], v16.t[:, g, 0:8], sc.t[:, g, :], NEG), r=[sc, v16], w=[sc2])
                P.op("dve", lambda e, g=g: e.max(v16.t[:, g, 8:16], sc2.t[:, g, :]), r=[sc2], w=[v16])
                P.op("dve", lambda e, g=g: e.max_index(i16u.t[:, g, 8:16], v16.t[:, g, 8:16], sc2.t[:, g, :]), r=[sc2, v16], w=[i16u])
            P.op("dve", lambda e: e.tensor_copy(if16.t[:], i16u.t[:]), r=[i16u], w=[if16])
            vp = v16.t[:].ap[0][0]
            bcA = lambda t, off: mkap(t.t, off, [[vp, 128], [32, 8], [1, 16], [0, 16]])
            bcB = lambda t, off: mkap(t.t, off, [[vp, 128], [32, 8], [0, 16], [1, 16]])
            c4d = cand.t[:].rearrange("p h (a b) -> p h a b", b=16)
            P.op("dve", lambda e: e.tensor_tensor(c4d, bcA(v16, 0), bcB(v16, 16), ALU.add), r=[v16], w=[cand])
            for h in range(8):
                P.op("dve", lambda e, h=h: e.max(cs16.t[:, h, 0:8], cand.t[:, h, :]), r=[cand], w=[cs16])
                P.op("dve", lambda e, h=h: e.max_index(cp16.t[:, h, 0:8], cs16.t[:, h, 0:8], cand.t[:, h, :]), r=[cand, cs16], w=[cp16])
                P.op("dve", lambda e, h=h: e.match_replace(cand2.t[:].rearrange("p a b -> p (a b)")[:, h * 256:(h + 1) * 256], cs16.t[:, h, 0:8], cand.t[:, h, :], NEG), r=[cand, cs16], w=[cand2])
                P.op("dve", lambda e, h=h: e.max(cs16.t[:, h, 8:16], cand2.t[:].rearrange("p a b -> p (a b)")[:, h * 256:(h + 1) * 256]), r=[cand2], w=[cs16])
                P.op("dve", lambda e, h=h: e.max_index(cp16.t[:, h, 8:16], cs16.t[:, h, 8:16], cand2.t[:].rearrange("p a b -> p (a b)")[:, h * 256:(h + 1) * 256]), r=[cand2, cs16], w=[cp16])
            P.op("dve", lambda e: e.tensor_single_scalar(au.t[:], cp16.t[:], 4, ALU.logical_shift_right), r=[cp16], w=[au])
            P.op("dve", lambda e: e.tensor_single_scalar(bu.t[:], cp16.t[:], 15, ALU.bitwise_and), r=[cp16], w=[bu])
            P.op("dve", lambda e: e.tensor_copy(af.t[:], au.t[:]), r=[au], w=[af])
            P.op("dve", lambda e: e.tensor_copy(bf.t[:], bu.t[:]), r=[bu], w=[bf])
            ep = eq.t[:].ap[0][0]
            for (sel, posf, off) in ((s1, af, 0), (s2_, bf, 16)):
                pf = posf.t[:].ap[0][0]
                posB = mkap(posf.t, 0, [[pf, 128], [16, 8], [1, 16], [0, 16]])
                iotB = mkap(io16.t, 0, [[16, 128], [0, 8], [0, 16], [1, 16]])
                valB = mkap(if16.t, off, [[vp, 128], [32, 8], [0, 16], [1, 16]])
                P.op("dve", lambda e, posB=posB, iotB=iotB: e.tensor_tensor(eq.t[:], posB, iotB, ALU.is_equal), r=[posf, io16], w=[eq])
                P.op("dve", lambda e, valB=valB: e.tensor_tensor(eq.t[:], eq.t[:], valB, ALU.mult), r=[eq, if16], w=[eq])
                P.op("dve", lambda e, sel=sel: e.tensor_reduce(sel.t[:], eq.t[:], AX.X, ALU.add), r=[eq], w=[sel])
            P.op("dve", lambda e: e.scalar_tensor_tensor(s1.t[:], s1.t[:], 128.0, s2_.t[:], ALU.mult, ALU.add), r=[s1, s2_], w=[s1])
            P.op("dve", lambda e: e.tensor_copy(eidu.t[:], s1.t[:].rearrange("p a b -> p (a b)")), r=[s1], w=[eidu])
            cp_ = cs16.t[:].ap[0][0]
            mxB = mkap(cs16.t, 0, [[cp_, 128], [16, 8], [0, 16]])
            P.op("dve", lambda e, mxB=mxB: e.tensor_tensor(gate.t[:], cs16.t[:], mxB, ALU.subtract), r=[cs16], w=[gate])
            P.op("act", lambda e: e.activation(gate.t[:], gate.t[:], AF.Exp), r=[gate], w=[gate])
            P.op("dve", lambda e: e.tensor_reduce(gsum.t[:], gate.t[:], AX.X, ALU.add), r=[gate], w=[gsum])
            P.op("dve", lambda e: e.reciprocal(gsum.t[:], gsum.t[:]), r=[gsum], w=[gsum])
            P.op("dve", lambda e: e.tensor_tensor(gate.t[:], gate.t[:], ins_bc(gsum.t[:], 2, 16), ALU.mult), r=[gate, gsum], w=[gate])
            for s in range(128):
                ug = gU[s % NB]
                P.op("pool", lambda e, ug=ug, s=s: e.indirect_dma_start(out=ug.t[:], out_offset=None, in_=u_tab,
                                                                        in_offset=bass.IndirectOffsetOnAxis(ap=eidu.t[:, s:s + 1], axis=0)), r=[eidu] + G.tab_tok, w=[ug], dma=True)
                P.op("dve", lambda e, ug=ug, s=s: e.scalar_tensor_tensor(junk.t[:], ug.t[:], 1.0, hxt.t[:], ALU.mult, ALU.mult, accum_out=dots.t[:, s:s + 1]),
                     r=[ug, hxt], w=[junk, dots])
            P.op("act", lambda e: e.activation(dots.t[:], dots.t[:], AF.Gelu_apprx_tanh), r=[dots], w=[dots])
            P.op("dve", lambda e: e.tensor_tensor(wts.t[:], dots.t[:], gate.t[:].rearrange("p a b -> p (a b)"), ALU.mult), r=[dots, gate], w=[wts])
            wp = wts.t[:].ap[0][0]
            for s in range(128):
                if s % 64 == 0:
                    P.op("dve", lambda e, s=s: e.tensor_tensor(Dg.t[:], mkap(wts.t, s, [[wp, 128], [1, 64], [0, 128]]), mkap(idn.t, 0, [[128, 128], [0, 64], [1, 128]]), ALU.mult),
                         r=[wts, idn], w=[Dg])
                vg = gV[s % NB]
                P.op("pool", lambda e, vg=vg, s=s: e.indirect_dma_start(out=vg.t[:], out_offset=None, in_=v_tab,
                                                                        in_offset=bass.IndirectOffsetOnAxis(ap=eidu.t[:, s:s + 1], axis=0)), r=[eidu] + G.tab_tok, w=[vg], dma=True)
                for hh in range(2):
                    P.op("pe", lambda e, vg=vg, s=s, hh=hh: e.matmul(psc[hh].t[:], Dg.t[:, s % 64, :], vg.t[:, hh * 512:(hh + 1) * 512], start=(s == 0), stop=(s == 127)),
                         r=[Dg, vg], w=[psc[hh]])
            for hh in range(2):
                P.op("act", lambda e, hh=hh: e.activation(acc.t[:, hh * 512:(hh + 1) * 512], psc[hh].t[:], AF.Identity), r=[psc[hh]], w=[acc])
            if getattr(G, "dbgo", None) is not None and tt == 2:
                dd = G.dbgo
                P.dma("sp", dd["d_sc"], sc.t[:].rearrange("p a b -> p (a b)"), r=[sc])
                P.dma("sp", dd["d_v16"], v16.t[:].rearrange("p a b -> p (a b)"), r=[v16])
                P.dma("sp", dd["d_if16"], if16.t[:].rearrange("p a b -> p (a b)"), r=[if16])
                P.dma("sp", dd["d_cs16"], cs16.t[:].rearrange("p a b -> p (a b)"), r=[cs16])
                P.dma("sp", dd["d_eid"], s1.t[:].rearrange("p a b -> p (a b)"), r=[s1])
                P.dma("sp", dd["d_gate"], gate.t[:].rearrange("p a b -> p (a b)"), r=[gate])
                P.dma("sp", dd["d_dots"], dots.t[:], r=[dots])
                P.dma("sp", dd["d_acc"], acc.t[:], r=[acc])
                P.dma("sp", dd["d_hxt"], hxt.t[:], r=[hxt])
            for h2 in range(2):
                for k4 in range(4):
                    k = h2 * 4 + k4
                    P.op("pe", lambda e, k=k, k4=k4, h2=h2: e.matmul(ptr[h2].t[:, k4 * 128:(k4 + 1) * 128], acc.t[:, k * 128:(k + 1) * 128], idn.t[:], start=True, stop=True), r=[acc, idn], w=[ptr[h2]])
                for k4 in range(4):
                    k = h2 * 4 + k4
                    P.op("dve", lambda e, k=k, k4=k4, h2=h2, j=j: e.scalar_tensor_tensor(xo.t[:, k, :], ptr[h2].t[:, k4 * 128:(k4 + 1) * 128], G.mod.t[:, 40 + k, j:j + 1], xt.t[:, k, :],
                                                                                   ALU.mult, ALU.add), r=[ptr[h2], G.mod, xt], w=[xo])
            src = xo
            if final:
                P.op("act", lambda e: e.activation(sq.t[:], xo.t[:], AF.Square), r=[xo], w=[sq])
                for k in range(8):
                    P.op("pe", lambda e, k=k: e.matmul(pss.t[:, 0:128], G.ones_bf.t[:, :], sq.t[:, k, :], start=(k == 0), stop=(k == 7)), r=[sq, G.ones_bf], w=[pss])
                P.op("act", lambda e: e.activation(rs.t[:], pss.t[:, 0:128], AF.Sqrt, bias=G.eps.t[:, 0:1], scale=1.0 / D), r=[pss, G.eps], w=[rs])
                P.op("dve", lambda e: e.reciprocal(rs.t[:], rs.t[:]), r=[rs], w=[rs])
                for k in range(8):
                    P.op("dve", lambda e, k=k: e.scalar_tensor_tensor(yo.t[:, k, :], xo.t[:, k, :], fg.t[:, k:k + 1], rs.t[:], ALU.mult, ALU.mult), r=[xo, fg, rs], w=[yo])
                src = yo
            if j:
                P.dma("sp", cov[:, :, o0:o0 + 128], src.t[:], r=[src], w=[out_tok[tt]])
            else:
                P.dma("sp", xov[:, :, o0 - 256:o0 - 256 + 128], src.t[:], r=[src], w=[out_tok[tt]])


def build_layer(P, G, io, l, need_ctx, final, xoT, coT, dbg=None):
    uF = P.dram("uF%d" % l, [NFULL * 128, NS], F32)
    uO = P.dram("uO%d" % l, [NOWNC * 128, NO], F32)
    ymix = dbg["ymix"] if dbg and "ymix" in dbg else P.dram("ymix%d" % l, [D, NO], BF16)
    x1T = dbg["x1T"] if dbg and "x1T" in dbg else P.dram("x1T%d" % l, [D, NO], F32)
    KS = P.dram("KS%d" % l, [128, 512, 2, 128], F32)
    KSW = P.dram("KSW%d" % l, [128, 512, 2, 128], F32)
    phase_mod(P, G, io, l)
    phase_B(P, G, io, l, need_ctx, uF, uO)
    phase_C(P, G, io, l, need_ctx, uF, uO, ymix)
    G.bg = []
    cg = cast_peer_tables(P, G, io, l)
    next(cg)
    phase_D(P, G, io, l, need_ctx, uF, uO, ymix)
    while G.bg:
        G.bg.pop(0)()
    for _ in cg:
        pass
    phase_E_filters(P, G, io, l, KS, KSW)
    phase_E_latent(P, G, io, l, uF, ymix, KS, KSW)
    if need_ctx:
        phase_E_ctx(P, G, io, l, uF, ymix)
    phase_F(P, G, io, l, need_ctx, ymix, x1T)
    phase_G(P, G, io, l, need_ctx, final, x1T, xoT, coT)


def src_from_inputs(io):
    xfv = io["xT_full"].rearrange("(k p) s -> p k s", p=128)
    xov = io["xT_own"].rearrange("(k p) s -> p k s", p=128)
    cv = io["cT"].rearrange("(k p) s -> p k s", p=128)
    return {"full": lambda t0, n: xfv[:, :, t0:t0 + n], "own": lambda t0, n: xov[:, :, t0:t0 + n], "ctx": lambda n: cv[:, :, 0:n], "toks": []}


def build_fused_program():
    nc = bass.Bass("TRN2", target_bir_lowering=False)
    io = declare_io(nc)
    P = Prog(nc)
    G = Ctx()
    xoT = Buf("xoT", nc.dram_tensor("xoT", [D, OWN], F32, kind="ExternalOutput").ap())
    setup_globals(P, G)
    fft_consts(P, G, io)
    xo0 = P.dram("xo0", [D, OWN], F32)
    co0 = P.dram("co0", [D, NCTX], F32)
    G.src = src_from_inputs(io)
    build_layer(P, G, io, 0, True, False, xo0, co0)
    toks0 = list(G.out_tok)
    xg = P.dram("xg", [4 * D, OWN], F32)
    for k in range(8):
        P.coll(lambda e, k=k: e.collective_compute("AllGather", ALU.bypass, replica_groups=[[0, 1, 2, 3], [4, 5, 6, 7]],
                                                   ins=[xo0.t[k * 128:(k + 1) * 128, :].opt()], outs=[xg.t[k * 512:(k + 1) * 512, :].opt()]), r=toks0, w=[xg])
    xgv = xg.t.rearrange("(k r p) s -> r p k s", r=4, p=128)
    xo0v = xo0.t.rearrange("(k p) s -> p k s", p=128)
    co0v = co0.t.rearrange("(k p) s -> p k s", p=128)
    G.src = {"full": lambda t0, n: xgv[t0 // OWN][:, :, (t0 % OWN):(t0 % OWN) + n], "own": lambda t0, n: xo0v[:, :, t0:t0 + n],
             "ctx": lambda n: co0v[:, :, 0:n], "toks": [xg] + toks0}
    build_layer(P, G, io, 1, False, True, xoT, None)
    P.finish(G.out_tok)
    return nc


def kernel(**inputs):
    inp = {k: np.asarray(v) for k, v in inputs.items()}
    S = prep_shared(inp)
    nc = build_fused_program()
    maps = []
    for c in range(8):
        m = dict(S)
        m.update(prep_core(inp, c))
        maps.append(m)
    res = run_bass_kernel_spmd(nc, maps, core_ids=list(range(8)))
    x = np.asarray(inp["x"], np.float32)
    out = np.empty_like(x)
    for c in range(8):
        b, j = c // 4, c % 4
        out[b, j * OWN:(j + 1) * OWN] = res.results[c]["xoT"].T
    return out.astype(np.float32)
```

```python
from contextlib import ExitStack
import numpy as np
import concourse.bass as bass
import concourse.mybir as mybir
from concourse.bass_utils import run_bass_kernel_spmd

F32 = mybir.dt.float32
BF16 = mybir.dt.bfloat16
U32 = mybir.dt.uint32
I32 = mybir.dt.int32
AF = mybir.ActivationFunctionType
ALU = mybir.AluOpType
AX = mybir.AxisListType


def mkap(t, offset, pat):
    return bass.AP(t, offset, [list(p) for p in pat])


class Buf:
    __slots__ = ("name", "t", "last_w", "readers", "is_psum")

    def __init__(self, name, t, init_readers=None):
        self.is_psum = False
        self.name = name
        self.t = t
        self.last_w = None
        self.readers = dict(init_readers or {})


class Prog:
    NDS = 24

    def __init__(self, nc):
        self.nc = nc
        self.stack = ExitStack()
        self.eng = {"pe": nc.tensor, "act": nc.scalar, "dve": nc.vector, "pool": nc.gpsimd, "sp": nc.sync}
        self.csem = {k: self.stack.enter_context(nc.semaphore("c_" + k)) for k in self.eng}
        self.cc = {k: 0 for k in self.eng}
        self.dsem = {k: [self.stack.enter_context(nc.semaphore("d_%s%d" % (k, i))) for i in range(self.NDS)]
                     for k in ("sp", "act", "pool")}
        self.dcount = {k: 0 for k in self.dsem}
        self.waited = {k: {} for k in self.eng}
        self.free_events = {}
        self.nins = 0
        self.scopes = []

    def _track(self, b):
        if self.scopes:
            self.scopes[-1][1].append(b)
        return b

    def sb(self, name, shape, dtype):
        st = self.scopes[-1][0] if self.scopes else self.stack
        self.uid = getattr(self, "uid", 0) + 1
        name = "%s_%d" % (name, self.uid)
        t = st.enter_context(self.nc.sbuf_tensor(name, list(shape), dtype))
        return self._track(Buf(name, t, self.free_events))

    def ps(self, name, shape, dtype):
        st = self.scopes[-1][0] if self.scopes else self.stack
        self.uid = getattr(self, "uid", 0) + 1
        name = "%s_%d" % (name, self.uid)
        t = st.enter_context(self.nc.psum_tensor(name, list(shape), dtype))
        b = self._track(Buf(name, t, self.free_events))
        b.is_psum = True
        return b

    def dram(self, name, shape, dtype, kind="Internal"):
        t = self.nc.dram_tensor(name, list(shape), dtype, kind=kind)
        return Buf(name, t.ap())

    def token(self, name):
        return Buf(name, None)

    class _Scope:
        def __init__(self, p):
            self.p = p

        def __enter__(self):
            self.p.scopes.append((ExitStack(), []))
            return self

        def __exit__(self, *a):
            st, bufs = self.p.scopes.pop()
            fe = self.p.free_events
            for b in bufs:
                evs = list(b.readers.items())
                if b.last_w is not None:
                    evs.append(b.last_w)
                for k, v in evs:
                    if fe.get(k, 0) < v:
                        fe[k] = v
            st.close()
            return False

    def scope(self):
        return Prog._Scope(self)

    def _sem(self, key):
        if key[0] == "x":
            return self.xsem
        return self.csem[key[1]] if key[0] == "c" else self.dsem[key[1]][key[2]]

    def _wait(self, eng, key, val):
        if eng == "pe" and key == ("c", "pe"):
            return
        w = self.waited[eng]
        if w.get(key, 0) >= val:
            return
        w[key] = val
        self.eng[eng].wait_ge(self._sem(key), val)

    def op(self, eng, fn, r=(), w=(), dma=False):
        deps = {}
        for b in r:
            if b.last_w is not None:
                k, v = b.last_w
                if deps.get(k, 0) < v:
                    deps[k] = v
            if b.is_psum:
                for k, v in b.readers.items():
                    if k != ("c", eng) and deps.get(k, 0) < v:
                        deps[k] = v
        for b in w:
            if b.last_w is not None:
                k, v = b.last_w
                if deps.get(k, 0) < v:
                    deps[k] = v
            for k, v in b.readers.items():
                if deps.get(k, 0) < v:
                    deps[k] = v
        for k, v in deps.items():
            self._wait(eng, k, v)
        if dma:
            j = self.dcount[eng]
            self.dcount[eng] = j + 1
            slot = j % self.NDS
            key = ("d", eng, slot)
            if j >= self.NDS:
                self._wait(eng, key, 16 * (j // self.NDS))
            ev = (key, 16 * (j // self.NDS + 1))
            fn(self.eng[eng]).then_inc(self._sem(key), 16)
        else:
            self.cc[eng] += 1
            key = ("c", eng)
            ev = (key, self.cc[eng])
            fn(self.eng[eng]).then_inc(self._sem(key), 1)
        self.nins += 1
        for b in r:
            if b.readers.get(ev[0], 0) < ev[1]:
                b.readers[ev[0]] = ev[1]
        for b in w:
            b.last_w = ev
            b.readers = {}
        return ev

    def coll(self, fn, r=(), w=()):
        if not hasattr(self, "xsem"):
            self.xsem = self.stack.enter_context(self.nc.semaphore("x_coll"))
            self.xcount = 0
        deps = {}
        for b in list(r) + list(w):
            if b.last_w is not None:
                k, v = b.last_w
                deps[k] = max(deps.get(k, 0), v)
        for b in w:
            for k, v in b.readers.items():
                deps[k] = max(deps.get(k, 0), v)
        for k, v in deps.items():
            self._wait("pool", k, v)
        self.xcount += 1
        ev = (("x", "coll"), self.xcount)
        fn(self.eng["pool"]).then_inc(self.xsem)
        for b in r:
            b.readers[ev[0]] = max(b.readers.get(ev[0], 0), ev[1])
        for b in w:
            b.last_w = ev
            b.readers = {}
        return ev

    def dma(self, eng, out, in_, r=(), w=(), **kw):
        return self.op(eng, lambda e: e.dma_start(out=out, in_=in_, **kw), r=r, w=w, dma=True)

    def finish(self, out_bufs=()):
        for b in out_bufs:
            if b.last_w is not None:
                self._wait("sp", b.last_w[0], b.last_w[1])
        for k in self.eng:
            if self.cc[k] > 0:
                self._wait("sp", ("c", k), self.cc[k])
        for k in self.dsem:
            j = self.dcount[k]
            for slot in range(self.NDS):
                n = (j - slot + self.NDS - 1) // self.NDS if j > slot else 0
                if n > 0:
                    self._wait("sp", ("d", k, slot), 16 * n)
        self.stack.close()


D = 1024
NL = 8192
NCTX = 256
NS = NL + NCTX
OWN = 2048
NO = OWN + NCTX
EPS = 1e-6
D_LRU = 384
NFULL = 12
NOWNC = 6
MLA_SCALE = 96.0 ** -0.5
R_LRU, R_CKV, R_HY, R_KR, R_KRS = 0, 384, 640, 1408, 1440


def ins_bc(ap, pos, count):
    pat = [list(p) for p in ap.ap]
    pat.insert(pos, [0, count])
    return bass.AP(ap.tensor, ap.offset, pat)


def full_tiles():
    t = [(0, 256, 1)]
    for i in range(16):
        t.append((256 + 512 * i, 512, 0))
    return t


def own_tiles(need_ctx):
    t = [(0, 256, 1)] if need_ctx else []
    for i in range(4):
        t.append((256 + 512 * i, 512, 0))
    return t


class Ctx:
    pass


def load_cast(P, dst, dst_ap_fn, src_ap, ncols_total, rows=128, piece=2048, eng="pool"):
    with P.scope():
        stg = [P.sb("lc_stg%d" % i, [128, piece], F32) for i in range(2)]
        i = 0
        for c0 in range(0, ncols_total, piece):
            n = min(piece, ncols_total - c0)
            st = stg[i % 2]
            i += 1
            P.dma("sp", st.t[:rows, :n], src_ap[:, c0:c0 + n], w=[st])
            P.op(eng, lambda e, st=st, n=n, c0=c0: e.tensor_copy(dst_ap_fn(c0, n), st.t[:rows, :n]), r=[st], w=[dst])


def phase_mod(P, G, io, l):
    nc = P.nc
    G.mod = P.sb("mod", [128, 48, 2], F32)
    G.s1 = P.sb("s1", [128, 8, 2], F32)
    G.s2 = P.sb("s2", [128, 8, 2], F32)
    with P.scope():
        cv = P.sb("cv", [128, 8, 2], F32)
        sv = P.sb("sv", [128, 8, 2], F32)
        bm = P.sb("bm", [128, 48], F32)
        g12 = P.sb("g12", [128, 16], F32)
        tmp = P.sb("modtmp", [128, 8, 2], F32)
        wm = [P.sb("wm%d" % i, [128, 8, 768], F32) for i in range(2)]
        pm = P.ps("pm", [128, 48, 2], F32)
        P.dma("sp", cv.t[:], io["cvec"], w=[cv])
        P.dma("sp", bm.t[:], io["b_modT"][l], w=[bm])
        P.dma("sp", g12.t[:], io["g12T"][l], w=[g12])
        P.op("act", lambda e: e.activation(sv.t[:], cv.t[:], AF.Silu), r=[cv], w=[sv])
        wmv = io["w_mod"][l].rearrange("(k p) n -> p k n", p=128)
        for cb in range(8):
            w = wm[cb % 2]
            P.dma("sp" if cb % 2 == 0 else "act", w.t[:], wmv[:, :, cb * 768:(cb + 1) * 768], w=[w])
            for m in range(6):
                k6 = cb * 6 + m
                for k in range(8):
                    P.op("pe", lambda e, w=w, m=m, k=k, k6=k6: e.matmul(
                        pm.t[:, k6, :], w.t[:, k, m * 128:(m + 1) * 128], sv.t[:, k, :],
                        start=(k == 0), stop=(k == 7)), r=[w, sv], w=[pm])
        P.op("dve", lambda e: e.tensor_tensor(G.mod.t[:], pm.t[:], ins_bc(bm.t[:], 2, 2), ALU.add), r=[pm, bm], w=[G.mod])
        for (s, sc0, gcol) in ((G.s1, 8, 0), (G.s2, 32, 8)):
            P.op("dve", lambda e, sc0=sc0: e.tensor_scalar_add(tmp.t[:], G.mod.t[:, sc0:sc0 + 8, :], 1.0), r=[G.mod], w=[tmp])
            P.op("dve", lambda e, s=s, gcol=gcol: e.tensor_tensor(s.t[:], tmp.t[:], ins_bc(g12.t[:, gcol:gcol + 8], 2, 2), ALU.mult),
                 r=[tmp, g12], w=[s])


def norm_proj(P, G, name, src_fn, tiles, s_buf, sh_idx, w_bf, nch, dst_fn, dst_toks, out_dt=F32):
    with P.scope():
        xt = [P.sb(name + "xt%d" % i, [128, 8, 512], F32) for i in range(2)]
        sq = P.sb(name + "sq", [128, 8, 512], BF16)
        xn = P.sb(name + "xn", [128, 8, 512], F32)
        hx = [P.sb(name + "hx%d" % i, [128, 8, 512], BF16) for i in range(2)]
        rs = P.sb(name + "rs", [128, 512], F32)
        stage = [P.sb(name + "stg%d" % i, [128, nch, 512], out_dt) for i in range(2)]
        ssp = P.ps(name + "ssp", [128, 512], F32)
        pp = [P.ps(name + "pp%d" % i, [128, 512], F32) for i in range(3)]
        mi = 0
        for ti, (s0, n, j) in enumerate(tiles):
            x = xt[ti % 2]
            h = hx[ti % 2]
            stg = stage[ti % 2]
            P.dma("sp", x.t[:, :, :n], src_fn(s0, n, j), r=G.src["toks"], w=[x])
            P.op("act", lambda e, x=x, n=n: e.activation(sq.t[:, :, :n], x.t[:, :, :n], AF.Square), r=[x], w=[sq])
            for k in range(8):
                P.op("pe", lambda e, k=k, n=n: e.matmul(ssp.t[:, :n], G.ones_bf.t[:, :], sq.t[:, k, :n], start=(k == 0), stop=(k == 7)),
                     r=[sq, G.ones_bf], w=[ssp])
            P.op("act", lambda e, n=n: e.activation(rs.t[:, :n], ssp.t[:, :n], AF.Sqrt, bias=G.eps.t[:, 0:1], scale=1.0 / D), r=[ssp, G.eps], w=[rs])
            P.op("dve", lambda e, n=n: e.reciprocal(rs.t[:, :n], rs.t[:, :n]), r=[rs], w=[rs])
            P.op("dve", lambda e, x=x, n=n: e.tensor_tensor(xn.t[:, :, :n], x.t[:, :, :n], ins_bc(rs.t[:, :n], 1, 8), ALU.mult), r=[x, rs], w=[xn])
            for k in range(8):
                P.op("act", lambda e, h=h, k=k, n=n, j=j: e.activation(
                    h.t[:, k, :n], xn.t[:, k, :n], AF.Identity, scale=s_buf.t[:, k, j:j + 1], bias=G.mod.t[:, sh_idx + k, j:j + 1]),
                    r=[xn, s_buf, G.mod], w=[h])
            for m in range(nch):
                p_ = pp[mi % 3]
                for k in range(8):
                    P.op("pe", lambda e, p_=p_, m=m, k=k, n=n, h=h: e.matmul(
                        p_.t[:, :n], w_bf.t[:, k, m * 128:(m + 1) * 128], h.t[:, k, :n], start=(k == 0), stop=(k == 7)),
                        r=[w_bf, h], w=[p_])
                if mi % 2 == 0:
                    P.op("act", lambda e, p_=p_, m=m, n=n, stg=stg: e.activation(stg.t[:, m, :n], p_.t[:, :n], AF.Identity), r=[p_], w=[stg])
                else:
                    P.op("dve", lambda e, p_=p_, m=m, n=n, stg=stg: e.tensor_copy(stg.t[:, m, :n], p_.t[:, :n]), r=[p_], w=[stg])
                mi += 1
            P.dma("sp" if ti % 2 == 0 else "pool", dst_fn(s0, n), stg.t[:, :, :n], r=[stg], w=[dst_toks[ti]])


ROPE_PERM = list(range(8, 16)) + list(range(0, 8)) + list(range(24, 32)) + list(range(16, 24))


def chunkT(v, nch):
    return np.ascontiguousarray(np.asarray(v, np.float32).reshape(nch, 128).T)


def prep_shared(inp):
    f = lambda a: np.ascontiguousarray(np.asarray(a, np.float32))
    S = {}
    w_in = f(inp["w_in"])
    Lr = w_in.shape[0]
    wf = np.zeros((Lr, D, NFULL * 128), np.float32)
    wf[:, :, R_LRU:R_LRU + 384] = w_in[:, :, 0:384]
    wf[:, :, R_CKV:R_CKV + 256] = w_in[:, :, 1152:1408]
    wf[:, :, R_HY:R_HY + 768] = w_in[:, :, 1440:2208]
    wf[:, :, R_KR:R_KR + 32] = w_in[:, :, 1408:1440]
    wf[:, :, R_KRS:R_KRS + 32] = w_in[:, :, 1408:1440][:, :, ROPE_PERM]
    S["w_inF"] = wf
    S["w_inO"] = np.ascontiguousarray(np.concatenate([w_in[:, :, 384:768], w_in[:, :, 768:1152]], axis=2))
    S["w_mod"] = f(inp["w_mod"])
    S["b_modT"] = np.stack([chunkT(inp["b_mod"][l], 48) for l in range(Lr)])
    prep_lru(inp, S)
    prep_mla(inp, S)
    prep_hyena(inp, S)
    prep_tail(inp, S)
    S["g12T"] = np.stack([np.concatenate([chunkT(inp["norm1_g"][l], 8), chunkT(inp["norm2_g"][l], 8)], axis=1) for l in range(Lr)])
    return S


def prep_core(inp, core):
    b, j = core // 4, core % 4
    x = np.asarray(inp["x"], np.float32)
    C = {}
    C["xT_full"] = np.ascontiguousarray(x[b].T)
    C["xT_own"] = np.ascontiguousarray(x[b, j * OWN:(j + 1) * OWN].T)
    C["cT"] = np.ascontiguousarray(np.asarray(inp["ctx"], np.float32)[b].T)
    cv = np.stack([chunkT(inp["c"][b], 8), chunkT(inp["c_ctx"], 8)], axis=2)
    C["cvec"] = np.ascontiguousarray(cv)
    prep_mla_core(C, j)
    C["idx_own"] = np.ascontiguousarray(((np.arange(8)[None, :] * 128 + np.arange(128)[:, None]) * 4 + j).astype(np.uint32))
    return C


IO_SHAPES = {
    "xT_full": [D, NL], "xT_own": [D, OWN], "cT": [D, NCTX], "cvec": [128, 8, 2],
    "w_mod": [2, D, 6144], "b_modT": [2, 128, 48], "g12T": [2, 128, 16],
    "w_inF": [2, D, NFULL * 128], "w_inO": [2, D, NOWNC * 128],
}


def declare_io(nc, names=None):
    io = {}
    for k, shp in IO_SHAPES.items():
        if names is None or k in names:
            io[k] = nc.dram_tensor(k, shp, IO_DT.get(k, F32), kind="ExternalInput").ap()
    return io


def setup_globals(P, G):
    G.ones_bf = P.sb("ones_bf", [128, 128], BF16)
    G.eps = P.sb("eps", [128, 1], F32)
    P.op("pool", lambda e: e.memset(G.ones_bf.t[:], 1.0), w=[G.ones_bf])
    P.op("pool", lambda e: e.memset(G.eps.t[:], EPS), w=[G.eps])
    G.ones_f = P.sb("ones_f", [128, 64], F32)
    P.op("pool", lambda e: e.memset(G.ones_f.t[:], 1.0), w=[G.ones_f])
    G.one = P.sb("one", [128, 1], F32)
    P.op("pool", lambda e: e.memset(G.one.t[:], 1.0), w=[G.one])
    G.ymix_tok = [P.token("ymix%d" % i) for i in range(8)]


def phase_B(P, G, io, l, need_ctx, uF, uO, dbg=False):
    ft = full_tiles()
    G.uF_tok = [P.token("uF%d" % i) for i in range(len(ft))]
    SRC = G.src
    uFv = uF.t.rearrange("(c p) s -> p c s", p=128)
    uOv = uO.t.rearrange("(c p) s -> p c s", p=128)
    with P.scope():
        wF = P.sb("wF_bf", [128, 8, NFULL * 128], BF16)
        for k in range(8):
            load_cast(P, wF, lambda c0, n, k=k: wF.t[:, k, c0:c0 + n], io["w_inF"][l, k * 128:(k + 1) * 128, :], NFULL * 128)
        norm_proj(P, G, "nf", lambda s0, n, j: (SRC["ctx"](n) if j else SRC["full"](s0 - 256, n)), ft, G.s1, 0, wF, NFULL,
                  lambda s0, n: uFv[:, :, s0:s0 + n], G.uF_tok)
    ot = own_tiles(need_ctx)
    G.uO_tok = [P.token("uO%d" % i) for i in range(len(ot))]
    with P.scope():
        wO = P.sb("wO_bf", [128, 8, NOWNC * 128], BF16)
        for k in range(8):
            load_cast(P, wO, lambda c0, n, k=k: wO.t[:, k, c0:c0 + n], io["w_inO"][l, k * 128:(k + 1) * 128, :], NOWNC * 128)
        norm_proj(P, G, "no", lambda s0, n, j: (SRC["ctx"](n) if j else SRC["own"](s0 - 256, n)), ot, G.s1, 0, wO, NOWNC,
                  lambda s0, n: uOv[:, :, s0:s0 + n], G.uO_tok)


def prep_lru(inp, S):
    f = lambda a: np.asarray(a, np.float32)
    Lr = f(inp["lru_wr"]).shape[0]
    wbd = np.zeros((Lr, 128, 12, 128), np.float32)
    for l in range(Lr):
        for d in range(2):
            for gi, nm in enumerate(("lru_wr", "lru_wi")):
                w = f(inp[nm])[l, d]
                for cc in range(3):
                    q = (d * 2 + gi) * 3 + cc
                    wbd[l, 0:64, q, 0:64] = w[2 * cc]
                    wbd[l, 64:128, q, 64:128] = w[2 * cc + 1]
    S["lru_wbd"] = wbd
    vec = np.zeros((Lr, 128, 6 + 6 + 6 + 12 + 3), np.float32)
    for l in range(Lr):
        for d in range(2):
            vec[l, :, d * 3:d * 3 + 3] = chunkT(inp["lru_br"][l, d], 3)
            vec[l, :, 6 + d * 3:6 + d * 3 + 3] = chunkT(inp["lru_bi"][l, d], 3)
            vec[l, :, 12 + d * 3:12 + d * 3 + 3] = chunkT(inp["lru_lambda"][l, d], 3)
        for k in range(4):
            vec[l, :, 18 + k:18 + 12:4] = chunkT(inp["lru_conv_w"][l, k], 3)
        vec[l, :, 30:33] = chunkT(inp["lru_conv_b"][l], 3)
    S["lru_vec"] = vec


IO_SHAPES.update({"lru_wbd": [2, 128, 12, 128], "lru_vec": [2, 128, 33], "idx_own": [128, 8]})
IO_DT = {"idx_own": U32}


def phase_C(P, G, io, l, need_ctx, uF, uO, ymix):
    ft = full_tiles()
    with P.scope():
        wbd = P.sb("lru_wbd", [128, 12, 128], BF16)
        load_cast(P, wbd, lambda c0, n: wbd.t[:].rearrange("p a b -> p (a b)")[:, c0:c0 + n],
                  io["lru_wbd"][l].rearrange("p a b -> p (a b)"), 12 * 128)
        vec = P.sb("lru_vecs", [128, 33], F32)
        P.dma("sp", vec.t[:], io["lru_vec"][l], w=[vec])
        c8 = P.sb("lru_c8", [128, 12], F32)
        P.op("act", lambda e: e.activation(c8.t[:, 0:6], vec.t[:, 12:18], AF.Exp, scale=-1.0), r=[vec], w=[c8])
        P.op("act", lambda e: e.activation(c8.t[:, 0:6], c8.t[:, 0:6], AF.Ln, bias=G.one.t[:, 0:1], scale=1.0), r=[c8, G.one], w=[c8])
        P.op("dve", lambda e: e.tensor_scalar_mul(c8.t[:, 6:12], c8.t[:, 0:6], -16.0), r=[c8], w=[c8])
        P.op("dve", lambda e: e.tensor_scalar_mul(c8.t[:, 0:6], c8.t[:, 0:6], -8.0), r=[c8], w=[c8])
        idx = P.sb("idx_own", [128, 8], U32)
        P.dma("sp", idx.t[:], io["idx_own"], w=[idx])
        hL = [P.dram("hL%d_%d" % (l, d), [384, NL], F32) for d in range(2)]
        hC = [P.dram("hC%d_%d" % (l, d), [384, NCTX], F32) for d in range(2)]
        for cc in range(3):
            with P.scope():
                X1 = P.sb("lruX1", [128, NS + 8], F32)
                xc = P.sb("lruxc", [128, NS], F32)
                xcb = P.sb("lruxcb", [128, NS], BF16)
                Bf = P.sb("lruB", [128, NS], F32)
                T = P.sb("lruT", [128, NS], F32)
                pr = [P.ps("lrupr%d" % i, [128, 512], F32) for i in range(4)]
                P.op("pool", lambda e: e.memset(X1.t[:, 0:264], 0.0), w=[X1])
                P.op("pool", lambda e: e.memset(X1.t[:, NS:NS + 8], 0.0), w=[X1])
                rows = slice(R_LRU + cc * 128, R_LRU + (cc + 1) * 128)
                P.dma("sp", X1.t[:, 2:258], uF.t[rows, 0:256], r=G.uF_tok, w=[X1])
                P.dma("act", X1.t[:, 261:261 + NL], uF.t[rows, 256:NS], r=G.uF_tok, w=[X1])
                for (o0, n, b0) in ((0, 256, 0), (256, NL, 259)):
                    P.op("dve", lambda e, o0=o0, n=n, b0=b0: e.tensor_scalar(
                        xc.t[:, o0:o0 + n], X1.t[:, b0:b0 + n], vec.t[:, 18 + cc * 4:19 + cc * 4], vec.t[:, 30 + cc:31 + cc], ALU.mult, ALU.add),
                        r=[X1, vec], w=[xc])
                    for k in range(1, 4):
                        P.op("dve", lambda e, o0=o0, n=n, b0=b0, k=k: e.scalar_tensor_tensor(
                            xc.t[:, o0:o0 + n], X1.t[:, b0 + k:b0 + k + n], vec.t[:, 18 + cc * 4 + k:19 + cc * 4 + k], xc.t[:, o0:o0 + n],
                            ALU.mult, ALU.add), r=[X1, vec, xc], w=[xc])
                P.op("pool", lambda e: e.tensor_copy(xcb.t[:], xc.t[:]), r=[xc], w=[xcb])
                A = X1
                for d in range(2):
                    pi = 0
                    for (s0, n, j) in ft:
                        for gi, dst in ((0, A), (1, Bf)):
                            p_ = pr[pi % 4]
                            pi += 1
                            q = (d * 2 + gi) * 3 + cc
                            P.op("pe", lambda e, p_=p_, q=q, s0=s0, n=n: e.matmul(p_.t[:, :n], wbd.t[:, q, :], xcb.t[:, s0:s0 + n], start=True, stop=True),
                                 r=[wbd, xcb], w=[p_])
                            P.op("act", lambda e, p_=p_, dst=dst, s0=s0, n=n, gi=gi, d=d: e.activation(
                                dst.t[:, s0:s0 + n], p_.t[:, :n], AF.Sigmoid, bias=vec.t[:, gi * 6 + d * 3 + cc:gi * 6 + d * 3 + cc + 1]),
                                r=[p_, vec], w=[dst])
                    P.op("act", lambda e, d=d: e.activation(T.t[:], A.t[:, 0:NS], AF.Exp, scale=c8.t[:, 6 + d * 3 + cc:7 + d * 3 + cc]), r=[A, c8], w=[T])
                    P.op("act", lambda e, d=d: e.activation(A.t[:, 0:NS], A.t[:, 0:NS], AF.Exp, scale=c8.t[:, d * 3 + cc:d * 3 + cc + 1]), r=[A, c8], w=[A])
                    P.op("act", lambda e: e.activation(T.t[:], T.t[:], AF.Sqrt, bias=G.one.t[:, 0:1], scale=-1.0), r=[T, G.one], w=[T])
                    P.op("dve", lambda e: e.tensor_tensor(Bf.t[:], Bf.t[:], T.t[:], ALU.mult), r=[Bf, T], w=[Bf])
                    P.op("pool", lambda e: e.tensor_tensor(Bf.t[:], Bf.t[:], xc.t[:], ALU.mult), r=[Bf, xc], w=[Bf])
                    ps_ = Bf.t[:].ap[0][0]
                    if d == 0:
                        P.op("dve", lambda e: e.tensor_tensor_scan(Bf.t[:], A.t[:, 0:NS], Bf.t[:], 0.0, ALU.mult, ALU.add), r=[A, Bf], w=[Bf])
                    else:
                        pa = A.t[:].ap[0][0]
                        rv = lambda t, pst, lo, n: mkap(t.t, lo + n - 1, [[pst, 128], [-1, n]])
                        P.op("dve", lambda e: e.tensor_tensor_scan(rv(Bf, ps_, 0, 256), rv(A, pa, 0, 256), rv(Bf, ps_, 0, 256), 0.0, ALU.mult, ALU.add),
                             r=[A, Bf], w=[Bf])
                        P.op("dve", lambda e: e.tensor_tensor_scan(rv(Bf, ps_, 256, NL), rv(A, pa, 256, NL), rv(Bf, ps_, 256, NL), Bf.t[:, 0:1],
                                                                   ALU.mult, ALU.add), r=[A, Bf], w=[Bf])
                    P.dma("sp", hC[d].t[cc * 128:(cc + 1) * 128, :], Bf.t[:, 0:256], r=[Bf], w=[hC[d]])
                    P.dma("sp", hL[d].t[cc * 128:(cc + 1) * 128, :], Bf.t[:, 256:NS], r=[Bf], w=[hL[d]])
        for cc in range(3):
            with P.scope():
                g0 = P.sb("lrug0", [128, NO], F32)
                g1 = P.sb("lrug1", [128, NO], F32)
                gt = P.sb("lrugt", [128, NO], F32)
                yo = P.sb("lruyo", [128, NO], BF16)
                c0 = 0 if need_ctx else 256
                for d, g in ((0, g0), (1, g1)):
                    if need_ctx:
                        P.dma("sp", g.t[:, 0:256], hC[d].t[cc * 128:(cc + 1) * 128, :], r=[hC[d]], w=[g])
                    P.op("pool", lambda e, g=g, d=d: e.indirect_dma_start(
                        out=g.t[:, 256:NO], out_offset=None, in_=hL[d].t.rearrange("c (q s) -> (c q) s", q=4),
                        in_offset=bass.IndirectOffsetOnAxis(ap=idx.t[:, cc:cc + 1], axis=0)), r=[hL[d], idx], w=[g], dma=True)
                P.dma("sp", gt.t[:, c0:NO], uO.t[cc * 128:(cc + 1) * 128, c0:NO], r=G.uO_tok, w=[gt])
                P.op("act", lambda e: e.activation(gt.t[:, c0:NO], gt.t[:, c0:NO], AF.Gelu_apprx_tanh), r=[gt], w=[gt])
                P.op("dve", lambda e: e.tensor_tensor(g0.t[:, c0:NO], g0.t[:, c0:NO], g1.t[:, c0:NO], ALU.add), r=[g0, g1], w=[g0])
                P.op("dve", lambda e: e.tensor_tensor(yo.t[:, c0:NO], g0.t[:, c0:NO], gt.t[:, c0:NO], ALU.mult), r=[g0, gt], w=[yo])
                P.dma("sp", ymix.t[cc * 128:(cc + 1) * 128, c0:NO], yo.t[:, c0:NO], r=[yo], w=[G.ymix_tok[cc]])


def rope_tables(pos):
    pos = np.asarray(pos)
    inv = (np.float32(10000.0) ** (-np.arange(8, dtype=np.float32) / np.float32(8))).astype(np.float32)
    row = (pos // 64).astype(np.float32)
    col = (pos % 64).astype(np.float32)
    Cc = np.ones((32, len(pos)), np.float32)
    Ss = np.zeros((32, len(pos)), np.float32)
    for r in range(32):
        ang = (row if r < 16 else col) * inv[r % 8]
        c, s = np.cos(ang.astype(np.float32)), np.sin(ang.astype(np.float32))
        Cc[r] = np.where(pos >= 0, c, 1.0)
        Ss[r] = np.where(pos >= 0, (-s if (r % 16) < 8 else s), 0.0)
    return Cc.astype(np.float32), Ss.astype(np.float32)


def prep_mla(inp, S):
    f = lambda a: np.asarray(a, np.float32)
    wkvb = f(inp["mla_wkvb"])
    Lr = wkvb.shape[0]
    w4 = wkvb.reshape(Lr, 256, 6, 2, 64)
    S["wkvb_r"] = np.ascontiguousarray(np.concatenate([w4[:, :, :, 0, :].reshape(Lr, 256, 384), w4[:, :, :, 1, :].reshape(Lr, 256, 384)], axis=2))
    wqb = f(inp["mla_wqb"]).reshape(Lr, 384, 6, 96)
    sw = wqb.copy()
    sw[..., 64:96] = wqb[..., 64:96][..., ROPE_PERM]
    S["wq_ext"] = np.ascontiguousarray(np.stack([wqb, sw], axis=3).reshape(Lr, 384, 6 * 2 * 96))
    S["mla_g"] = np.stack([np.concatenate([chunkT(inp["mla_q_norm_g"][l], 3), chunkT(inp["mla_kv_norm_g"][l], 2)], axis=1) for l in range(Lr)])
    pos = np.concatenate([-np.ones(NCTX, np.int64), np.arange(NL)])
    Cc, Ss = rope_tables(pos)
    S["ropeK"] = np.ascontiguousarray(np.stack([Cc, Ss]))


def prep_mla_core(C, j):
    pos = np.concatenate([-np.ones(NCTX, np.int64), np.arange(j * OWN, (j + 1) * OWN)])
    Cc, Ss = rope_tables(pos)
    Cq = np.ones((96, NO), np.float32); Cq[64:96] = Cc
    Sq = np.zeros((96, NO), np.float32); Sq[64:96] = Ss
    C["ropeQ"] = np.ascontiguousarray(np.stack([Cq, Sq]))


IO_SHAPES.update({"wkvb_r": [2, 256, 768], "wq_ext": [2, 384, 1152], "mla_g": [2, 128, 5], "ropeK": [2, 32, NS], "ropeQ": [2, 96, NO]})


def rms_fm(P, G, name, src_fn, tiles, nk, dim, gains, gcol, dst, src_toks):
    with P.scope():
        xt = [P.sb(name + "x%d" % i, [128, nk, 512], F32) for i in range(2)]
        sq = P.sb(name + "sq", [128, nk, 512], BF16)
        rs = P.sb(name + "rs", [128, 512], F32)
        ssp = P.ps(name + "ss", [128, 512], F32)
        for ti, (s0, n, j) in enumerate(tiles):
            x = xt[ti % 2]
            P.dma("sp" if ti % 2 == 0 else "act", x.t[:, :, :n], src_fn(s0, n), r=src_toks, w=[x])
            P.op("act", lambda e, x=x, n=n: e.activation(sq.t[:, :, :n], x.t[:, :, :n], AF.Square), r=[x], w=[sq])
            for k in range(nk):
                P.op("pe", lambda e, k=k, n=n: e.matmul(ssp.t[:, :n], G.ones_bf.t[:, :], sq.t[:, k, :n], start=(k == 0), stop=(k == nk - 1)),
                     r=[sq, G.ones_bf], w=[ssp])
            P.op("act", lambda e, n=n: e.activation(rs.t[:, :n], ssp.t[:, :n], AF.Sqrt, bias=G.eps.t[:, 0:1], scale=1.0 / dim), r=[ssp, G.eps], w=[rs])
            P.op("dve", lambda e, n=n: e.reciprocal(rs.t[:, :n], rs.t[:, :n]), r=[rs], w=[rs])
            for k in range(nk):
                P.op("dve", lambda e, x=x, k=k, n=n, s0=s0: e.scalar_tensor_tensor(
                    dst.t[:, k, s0:s0 + n], x.t[:, k, :n], gains.t[:, gcol + k:gcol + k + 1], rs.t[:, :n], ALU.mult, ALU.mult),
                    r=[x, gains, rs], w=[dst])


def phase_D(P, G, io, l, need_ctx, uF, uO, ymix):
    ft = full_tiles()
    ot = own_tiles(need_ctx)
    with P.scope():
        mg = P.sb("mla_g", [128, 5], F32)
        P.dma("sp", mg.t[:], io["mla_g"][l], w=[mg])
        wkv = P.sb("wkv_bf", [128, 2, 768], BF16)
        for k in range(2):
            load_cast(P, wkv, lambda c0, n, k=k: wkv.t[:, k, c0:c0 + n], io["wkvb_r"][l, k * 128:(k + 1) * 128, :], 768)
        wq = P.sb("wq_bf", [128, 3, 1152], BF16)
        for k in range(3):
            load_cast(P, wq, lambda c0, n, k=k: wq.t[:, k, c0:c0 + n], io["wq_ext"][l, k * 128:(k + 1) * 128, :], 1152)
        ckvn = P.sb("ckvn", [128, 2, NS], BF16)
        uFv = uF.t.rearrange("(c p) s -> p c s", p=128)
        uOv = uO.t.rearrange("(c p) s -> p c s", p=128)
        rms_fm(P, G, "kvn", lambda s0, n: uFv[:, 3:5, s0:s0 + n], ft, 2, 256, mg, 3, ckvn, G.uF_tok)
        cqn = P.sb("cqn", [128, 3, NO], BF16)
        rms_fm(P, G, "qn", lambda s0, n: uOv[:, 3:6, s0:s0 + n], ot, 3, 384, mg, 0, cqn, G.uO_tok)
        krb = P.sb("krb", [96, NS], BF16)
        with P.scope():
            n = 2112
            a = P.sb("kr_a", [96, n], F32); b_ = P.sb("kr_b", [96, n], F32)
            tc_ = P.sb("kr_c", [96, n], F32); ts_ = P.sb("kr_s", [96, n], F32)
            for c0 in range(0, NS, 2112):
                P.dma("sp", a.t[64:96, :], uF.t[R_KR:R_KR + 32, c0:c0 + n], r=G.uF_tok, w=[a])
                P.dma("act", b_.t[64:96, :], uF.t[R_KRS:R_KRS + 32, c0:c0 + n], r=G.uF_tok, w=[b_])
                P.dma("sp", tc_.t[64:96, :], io["ropeK"][0, :, c0:c0 + n], w=[tc_])
                P.dma("act", ts_.t[64:96, :], io["ropeK"][1, :, c0:c0 + n], w=[ts_])
                P.op("dve", lambda e, a=a, tc_=tc_: e.tensor_tensor(a.t[64:96, :], a.t[64:96, :], tc_.t[64:96, :], ALU.mult), r=[a, tc_], w=[a])
                P.op("pool", lambda e, b_=b_, ts_=ts_: e.tensor_tensor(b_.t[64:96, :], b_.t[64:96, :], ts_.t[64:96, :], ALU.mult), r=[b_, ts_], w=[b_])
                P.op("dve", lambda e, a=a, b_=b_, c0=c0, n=n: e.tensor_tensor(krb.t[64:96, c0:c0 + n], a.t[64:96, :], b_.t[64:96, :], ALU.add), r=[a, b_], w=[krb])
        tq = P.sb("ropeQ", [96, 2, NO], F32)
        P.dma("sp", tq.t[:, 0, :], io["ropeQ"][0], w=[tq])
        P.dma("sp", tq.t[:, 1, :], io["ropeQ"][1], w=[tq])
        for pr in range(3):
            with P.scope():
                Kh = [P.sb("Kh%d" % i, [96, NS], BF16) for i in range(2)]
                Vp = P.sb("Vp", [128, 66, 2, 65], BF16)
                Qh = [P.sb("Qh%d" % i, [96, NO], BF16) for i in range(2)]
                with P.scope():
                    pk = [P.ps("pk%d" % i, [128, 512], F32) for i in range(4)]
                    t1 = P.sb("qt1", [96, 512], F32); t2 = P.sb("qt2", [96, 512], F32)
                    pi = 0
                    for i in range(2):
                        h = 2 * pr + i
                        for (s0, n, j) in ft:
                            p_ = pk[pi % 4]; pi += 1
                            for k in range(2):
                                P.op("pe", lambda e, p_=p_, k=k, h=h, s0=s0, n=n: e.matmul(p_.t[0:64, :n], wkv.t[:, k, h * 64:(h + 1) * 64], ckvn.t[:, k, s0:s0 + n],
                                                                                     start=(k == 0), stop=(k == 1)), r=[wkv, ckvn], w=[p_])
                            if pi % 2:
                                P.op("act", lambda e, p_=p_, i=i, s0=s0, n=n: e.activation(Kh[i].t[0:64, s0:s0 + n], p_.t[0:64, :n], AF.Identity), r=[p_], w=[Kh[i]])
                            else:
                                P.op("dve", lambda e, p_=p_, i=i, s0=s0, n=n: e.tensor_copy(Kh[i].t[0:64, s0:s0 + n], p_.t[0:64, :n]), r=[p_], w=[Kh[i]])
                        P.op("pool", lambda e, i=i: e.tensor_copy(Kh[i].t[64:96, :], krb.t[64:96, :]), r=[krb], w=[Kh[i]])
                        for (s0, n, j) in ot:
                            pa = pk[pi % 4]; pi += 1
                            pb = pk[pi % 4]; pi += 1
                            for v, p_ in ((0, pa), (1, pb)):
                                for k in range(3):
                                    c0 = (h * 2 + v) * 96
                                    P.op("pe", lambda e, p_=p_, k=k, c0=c0, s0=s0, n=n: e.matmul(p_.t[0:96, :n], wq.t[:, k, c0:c0 + 96], cqn.t[:, k, s0:s0 + n],
                                                                                          start=(k == 0), stop=(k == 2)), r=[wq, cqn], w=[p_])
                            P.op("dve", lambda e, pa=pa, s0=s0, n=n: e.tensor_tensor(t1.t[:, :n], pa.t[0:96, :n], tq.t[:, 0, s0:s0 + n], ALU.mult), r=[pa, tq], w=[t1])
                            P.op("dve", lambda e, pb=pb, s0=s0, n=n: e.tensor_tensor(t2.t[:, :n], pb.t[0:96, :n], tq.t[:, 1, s0:s0 + n], ALU.mult), r=[pb, tq], w=[t2])
                            P.op("pool", lambda e, i=i, s0=s0, n=n: e.tensor_tensor(Qh[i].t[:, s0:s0 + n], t1.t[:, :n], t2.t[:, :n], ALU.add), r=[t1, t2], w=[Qh[i]])
                    P.op("pool", lambda e: e.memset(Vp.t[:, :, :, 64:65], 1.0), w=[Vp])
                    for kc4 in range(0, 66, 4):
                        p_ = pk[pi % 4]; pi += 1
                        nk4 = min(4, 66 - kc4)
                        for q in range(nk4):
                            kc = kc4 + q
                            for k in range(2):
                                P.op("pe", lambda e, p_=p_, q=q, k=k, kc=kc: e.matmul(p_.t[:, q * 128:(q + 1) * 128], ckvn.t[:, k, kc * 128:(kc + 1) * 128],
                                                                                wkv.t[:, k, 384 + pr * 128:384 + (pr + 1) * 128], start=(k == 0), stop=(k == 1)),
                                     r=[wkv, ckvn], w=[p_])
                        src4 = p_.t[:, :nk4 * 128].rearrange("p (a b c) -> p a b c", b=2, c=64)
                        if (kc4 // 4) % 2:
                            P.op("act", lambda e, src4=src4, kc4=kc4, nk4=nk4: e.activation(Vp.t[:, kc4:kc4 + nk4, :, 0:64], src4, AF.Identity), r=[p_], w=[Vp])
                        else:
                            P.op("dve", lambda e, src4=src4, kc4=kc4, nk4=nk4: e.tensor_copy(Vp.t[:, kc4:kc4 + nk4, :, 0:64], src4), r=[p_], w=[Vp])
                with P.scope():
                    st = [P.ps("att_st%d" % i, [128, 512], F32) for i in range(4)]
                    pn = [P.ps("att_n%d" % i, [128, 512], F32) for i in range(2)]
                    pbc = P.ps("att_bc", [64, 512], F32)
                    pt = [P.sb("att_pt%d" % i, [128, 512], BF16) for i in range(4)]
                    rec = P.sb("att_rec", [128, 512], F32)
                    rbc = P.sb("att_rbc", [64, 512], F32)
                    ob = [P.sb("att_o%d" % i, [64, 512], BF16) for i in range(2)]
                    ci = 0
                    qi = 0
                    for i in range(2):
                        h = 2 * pr + i
                        for (s0, n, j) in ot:
                            nkc = 2 if j else 66
                            pn_, o_ = pn[qi % 2], ob[qi % 2]
                            qi += 1
                            def emit_pv(kc, p_, pn_=pn_, i=i, n=n, nkc=nkc):
                                P.op("pe", lambda e: e.matmul(pn_.t[0:65, :n], Vp.t[:, kc, i, :], p_.t[:, :n], start=(kc == 0), stop=(kc == nkc - 1)), r=[Vp, p_], w=[pn_])
                            prev = None
                            for kc in range(nkc):
                                if kc % 8 == 7 and G.bg:
                                    G.bg.pop(0)()
                                s_ = st[ci % 4]; p_ = pt[ci % 4]; ci += 1
                                P.op("pe", lambda e, s_=s_, i=i, kc=kc, s0=s0, n=n: e.matmul(s_.t[:, :n], Kh[i].t[0:96, kc * 128:(kc + 1) * 128], Qh[i].t[0:96, s0:s0 + n],
                                                                                       start=True, stop=True), r=[Kh[i], Qh[i]], w=[s_])
                                P.op("act", lambda e, s_=s_, p_=p_, n=n: e.activation(p_.t[:, :n], s_.t[:, :n], AF.Exp, scale=MLA_SCALE), r=[s_], w=[p_])
                                if prev is not None:
                                    emit_pv(*prev)
                                prev = (kc, p_)
                            emit_pv(*prev)
                            P.op("dve", lambda e, pn_=pn_, n=n: e.reciprocal(rec.t[64:65, :n], pn_.t[64:65, :n]), r=[pn_], w=[rec])
                            P.op("pe", lambda e, n=n: e.matmul(pbc.t[:, :n], G.ones_f.t[64:65, 0:64], rec.t[64:65, :n], start=True, stop=True), r=[G.ones_f, rec], w=[pbc])
                            P.op("act", lambda e, n=n: e.activation(rbc.t[:, :n], pbc.t[:, :n], AF.Identity), r=[pbc], w=[rbc])
                            P.op("dve", lambda e, pn_=pn_, o_=o_, n=n: e.tensor_tensor(o_.t[:, :n], pn_.t[0:64, :n], rbc.t[:, :n], ALU.mult), r=[pn_, rbc], w=[o_])
                            P.dma("sp", ymix.t[384 + h * 64:384 + (h + 1) * 64, s0:s0 + n], o_.t[:, :n], r=[o_], w=[G.ymix_tok[3 + pr]])


NFFT = 16384
MAGIC = 12582912.0
TWO_PI = 6.283185307179586


def prep_hyena(inp, S):
    f = lambda a: np.asarray(a, np.float32)
    Lr = f(inp["hy_f_w1"]).shape[0]
    n = np.arange(128)
    ang = 2.0 * np.pi * np.outer(n, n) / 128.0
    Fr, Fi = np.cos(ang), -np.sin(ang)
    S["dft128"] = np.ascontiguousarray(np.stack([Fr, Fi, Fr, -Fi], axis=1).astype(np.float32))
    th = 2.0 * np.pi * np.outer(n, n) / NFFT
    c, s = np.cos(th), np.sin(th)
    S["tw128"] = np.ascontiguousarray(np.stack([c, c, s, -s, s], axis=1).astype(np.float32))
    S["ident"] = np.eye(128, dtype=np.float32)

    def zfeat(L):
        t = (np.arange(L, dtype=np.float32) / np.float32(L)).astype(np.float32)
        bands = np.linspace(1e-4, 7, 8, dtype=np.float32)
        wpos = (np.float32(2.0 * np.pi) * t[:, None] * bands[None, :]).astype(np.float32)
        z = np.concatenate([t[:, None], np.cos(wpos), np.sin(wpos)], axis=-1).astype(np.float32)
        return np.ascontiguousarray(z.T)
    S["zT"] = zfeat(NL)
    S["zTc"] = zfeat(NCTX)
    S["hy_w1"] = f(inp["hy_f_w1"])
    S["hy_w2"] = f(inp["hy_f_w2"])
    S["hy_w3"] = f(inp["hy_f_w3"])
    hv = np.zeros((Lr, 128, 3), np.float32)
    hv[:, 0:64, 0] = f(inp["hy_f_freq"]); hv[:, 0:64, 1] = f(inp["hy_f_b1"]); hv[:, 0:64, 2] = f(inp["hy_f_b2"])
    S["hy_vec"] = hv
    S["hy_decT"] = np.stack([chunkT(f(inp["hy_decay"])[l].reshape(1024), 8) for l in range(Lr)])
    cw = f(inp["hy_conv_w"]); cb = f(inp["hy_conv_b"])
    S["hy_cw"] = np.stack([np.concatenate([np.stack([chunkT(cw[l, k], 6) for k in range(3)], axis=2).reshape(128, 18), chunkT(cb[l], 6)], axis=1) for l in range(Lr)])
    S["hy_skip"] = np.ascontiguousarray(f(inp["hy_skip"]).reshape(Lr, 1, 512))
    k5 = np.arange(512)
    a5 = 2.0 * np.pi * np.outer(k5, k5) / 512.0
    F5 = np.stack([np.cos(a5), -np.sin(a5)], axis=1).astype(np.float32)
    S["dft512"] = np.ascontiguousarray(F5.reshape(4, 128, 2, 512).transpose(1, 0, 2, 3))


IO_SHAPES.update({"dft128": [128, 4, 128], "tw128": [128, 5, 128], "ident": [128, 128], "zT": [17, NL], "zTc": [17, NCTX],
                  "hy_w1": [2, 17, 64], "hy_w2": [2, 64, 64], "hy_w3": [2, 64, 1024], "hy_vec": [2, 128, 3], "hy_decT": [2, 128, 8],
                  "hy_cw": [2, 128, 24], "hy_skip": [2, 1, 512], "dft512": [128, 4, 2, 512]})


def hy_sin_layer(P, G, name, ps_in, n, hv, fcol, dst_ap, tmp, dst_buf):
    a, kq = tmp
    P.op("dve", lambda e: e.tensor_scalar(a.t[0:64, :n], ps_in.t[0:64, :n], hv.t[0:64, 0:1], hv.t[0:64, fcol:fcol + 1], ALU.mult, ALU.add), r=[ps_in, hv], w=[a])
    P.op("dve", lambda e: e.tensor_scalar(kq.t[0:64, :n], a.t[0:64, :n], 1.0 / TWO_PI, MAGIC, ALU.mult, ALU.add), r=[a], w=[kq])
    P.op("dve", lambda e: e.tensor_scalar(kq.t[0:64, :n], kq.t[0:64, :n], MAGIC, -TWO_PI, ALU.subtract, ALU.mult), r=[kq], w=[kq])
    P.op("dve", lambda e: e.tensor_tensor(a.t[0:64, :n], a.t[0:64, :n], kq.t[0:64, :n], ALU.add), r=[a, kq], w=[a])
    P.op("dve", lambda e: e.tensor_scalar(a.t[0:64, :n], a.t[0:64, :n], -3.14159, 3.14159, ALU.max, ALU.min), r=[a], w=[a])
    P.op("act", lambda e: e.activation(dst_ap, a.t[0:64, :n], AF.Sin), r=[a], w=[dst_buf])


def hy_filters(P, G, io, l, L, zT_ap, emit_chunk, junk):
    with P.scope():
        hv = P.sb("hy_vec", [128, 5], F32)
        P.dma("sp", hv.t[:, 0:3], io["hy_vec"][l], w=[hv])
        P.op("dve", lambda e: e.tensor_tensor(hv.t[:, 3:5], hv.t[:, 1:3], mkap(hv.t, 0, [[5, 128], [0, 2]]), ALU.mult), r=[hv], w=[hv])
        dec = P.sb("hy_dec", [128, 8], F32)
        P.dma("sp", dec.t[:], io["hy_decT"][l], w=[dec])
        P.op("dve", lambda e: e.tensor_scalar_mul(dec.t[:], dec.t[:], -1.0), r=[dec], w=[dec])
        w1 = P.sb("hy_w1", [17, 64], F32); w2 = P.sb("hy_w2", [64, 64], F32); w3 = P.sb("hy_w3", [64, 1024], F32)
        P.dma("sp", w1.t[:], io["hy_w1"][l], w=[w1]); P.dma("sp", w2.t[:], io["hy_w2"][l], w=[w2]); P.dma("sp", w3.t[:], io["hy_w3"][l], w=[w3])
        zts = [P.sb("hy_zt%d" % i, [17, 512], F32) for i in range(2)]
        tb = P.sb("hy_tb", [128, L], F32)
        P.dma("sp", tb.t[:], zT_ap[0:1, :].broadcast_to([128, L]), w=[tb])
        h2 = P.sb("hy_h2", [64, L], F32)
        tiles = [(s0, min(512, L - s0)) for s0 in range(0, L, 512)]
        with P.scope():
            h1 = P.sb("hy_h1", [64, L], F32)
            a = P.sb("hy_a", [64, 512], F32); kq = P.sb("hy_kq", [64, 512], F32)
            pp = [P.ps("hy_pp%d" % i, [64, 512], F32) for i in range(2)]
            for ti, (s0, n) in enumerate(tiles):
                p_ = pp[ti % 2]
                zt = zts[ti % 2]
                P.dma("sp", zt.t[:, :n], zT_ap[:, s0:s0 + n], w=[zt])
                P.op("pe", lambda e, p_=p_, zt=zt, n=n: e.matmul(p_.t[:, :n], w1.t[:, :], zt.t[:, :n], start=True, stop=True), r=[w1, zt], w=[p_])
                hy_sin_layer(P, G, "s1", p_, n, hv, 3, h1.t[:, s0:s0 + n], (a, kq), h1)
            for ti, (s0, n) in enumerate(tiles):
                p_ = pp[ti % 2]
                P.op("pe", lambda e, p_=p_, s0=s0, n=n: e.matmul(p_.t[:, :n], w2.t[:, :], h1.t[:, s0:s0 + n], start=True, stop=True), r=[w2, h1], w=[p_])
                hy_sin_layer(P, G, "s2", p_, n, hv, 4, h2.t[:, s0:s0 + n], (a, kq), h2)
        with P.scope():
            kk = [P.sb("hy_kk%d" % i, [128, L], F32) for i in range(2)]
            et = [P.sb("hy_et%d" % i, [128, 512], F32) for i in range(2)]
            ss = P.sb("hy_ss", [128, 1], F32)
            pk = [P.ps("hy_pk%d" % i, [128, 512], F32) for i in range(2)]
            for q in range(8):
                k_ = kk[q % 2]
                for ti, (s0, n) in enumerate(tiles):
                    p_ = pk[ti % 2]; e_ = et[ti % 2]
                    P.op("pe", lambda e, p_=p_, s0=s0, n=n, q=q: e.matmul(p_.t[:, :n], w3.t[:, q * 128:(q + 1) * 128], h2.t[:, s0:s0 + n], start=True, stop=True), r=[w3, h2], w=[p_])
                    P.op("act", lambda e, e_=e_, s0=s0, n=n, q=q: e.activation(e_.t[:, :n], tb.t[:, s0:s0 + n], AF.Exp, scale=dec.t[:, q:q + 1]), r=[tb, dec], w=[e_])
                    P.op("dve", lambda e, p_=p_, e_=e_, k_=k_, s0=s0, n=n: e.tensor_tensor(k_.t[:, s0:s0 + n], p_.t[:, :n], e_.t[:, :n], ALU.mult), r=[p_, e_], w=[k_])
                P.op("act", lambda e, k_=k_: e.activation(junk.t[:], k_.t[:], AF.Square, accum_out=ss.t[:, 0:1]), r=[k_], w=[junk, ss])
                P.op("act", lambda e: e.activation(ss.t[:], ss.t[:], AF.Sqrt, bias=G.eps.t[:, 0:1], scale=1.0), r=[ss, G.eps], w=[ss])
                P.op("dve", lambda e: e.reciprocal(ss.t[:], ss.t[:]), r=[ss], w=[ss])
                P.op("dve", lambda e, k_=k_: e.tensor_scalar(k_.t[:], k_.t[:], ss.t[:, 0:1], None, ALU.mult), r=[k_, ss], w=[k_])
                emit_chunk(q, k_)


def fft_consts(P, G, io):
    G.DF = P.sb("dft_bf", [128, 4, 128], BF16)
    load_cast(P, G.DF, lambda c0, n: G.DF.t[:].rearrange("p a b -> p (a b)")[:, c0:c0 + n], io["dft128"].rearrange("p a b -> p (a b)"), 512)
    G.TW = P.sb("tw", [128, 5, 128], F32)
    P.dma("sp", G.TW.t[:], io["tw128"], w=[G.TW])


def cplx_mul_tab(P, G, src_ps, nch, tab, i1, i2, t1, t2, dst):
    tp = tab.t[:].ap[0][0]
    sp_ = src_ps.t[:].ap[0][0]
    tabA = mkap(tab.t, i1 * 128, [[tp, 128], [0, nch], [128, 2], [1, 128]])
    tabB = mkap(tab.t, i2 * 128, [[tp, 128], [0, nch], [128, 2], [1, 128]])
    srcA = mkap(src_ps.t, 0, [[sp_, 128], [256, nch], [128, 2], [1, 128]])
    srcSw = mkap(src_ps.t, 128, [[sp_, 128], [256, nch], [-128, 2], [1, 128]])
    P.op("dve", lambda e: e.tensor_tensor(t1.t[:, 0:nch], srcA, tabA, ALU.mult), r=[src_ps, tab], w=[t1])
    P.op("dve", lambda e: e.tensor_tensor(t2.t[:, 0:nch], srcSw, tabB, ALU.mult), r=[src_ps, tab], w=[t2])
    P.op("pool", lambda e: e.tensor_tensor(dst.t[:, 0:nch], t1.t[:, 0:nch], t2.t[:, 0:nch], ALU.add), r=[t1, t2], w=[dst])


def fft_fwd_group(P, G, xin_fn, K, nch, PA, PXr, PXi, t1, t2, Ab):
    DF = G.DF
    for c in range(nch):
        ap, bufs = xin_fn(c)
        P.op("pe", lambda e, c=c, ap=ap: e.matmul(PA.t[:, c].rearrange("p a b -> p (a b)"), ap, DF.t[0:K, 0:2, :].rearrange("p a b -> p (a b)"), start=True, stop=True),
             r=bufs + [DF], w=[PA])
    cplx_mul_tab(P, G, PA, nch, G.TW, 0, 2, t1, t2, Ab)
    Ar = Ab.t[:, 0:nch, 0, :]
    Ai = Ab.t[:, 0:nch, 1, :]
    P.op("pe", lambda e: e.matmul(PXr.t[:, 0:nch, :], DF.t[:, 0, :], Ar, start=True, stop=False), r=[DF, Ab], w=[PXr])
    P.op("pe", lambda e: e.matmul(PXr.t[:, 0:nch, :], DF.t[:, 3, :], Ai, start=False, stop=True), r=[DF, Ab], w=[PXr])
    P.op("pe", lambda e: e.matmul(PXi.t[:, 0:nch, :], DF.t[:, 1, :], Ar, start=True, stop=False), r=[DF, Ab], w=[PXi])
    P.op("pe", lambda e: e.matmul(PXi.t[:, 0:nch, :], DF.t[:, 0, :], Ai, start=False, stop=True), r=[DF, Ab], w=[PXi])


def phase_E_filters(P, G, io, l, KS, KSW):
    kc = P.dram("hy_kc%d" % l, [512, NFFT], BF16)
    kc_tok = [P.token("kc%d" % q) for q in range(8)]
    with P.scope():
        ob = [P.sb("hy_ob0", [128, NL], BF16)] * 2

        def emit(q, k_):
            g, half = q // 2, q % 2
            o, dr = g // 2, g % 2
            o_ = ob[q % 2]
            rows = slice(o * 256 + half * 128, o * 256 + (half + 1) * 128)
            if dr == 0:
                P.op("pool", lambda e: e.tensor_copy(o_.t[:], k_.t[:]), r=[k_], w=[o_])
                P.dma("sp", kc.t[rows, 0:NL], o_.t[:], r=[o_], w=[kc_tok[q]])
            else:
                kp = k_.t[:].ap[0][0]
                P.op("pool", lambda e: e.memset(o_.t[:, 0:1], 0.0), w=[o_])
                P.op("pool", lambda e: e.tensor_copy(o_.t[:, 1:NL], mkap(k_.t, NL - 1, [[kp, 128], [-1, NL - 1]])), r=[k_], w=[o_])
                P.dma("sp", kc.t[rows, NL:2 * NL], o_.t[:], r=[o_], w=[kc_tok[q]])
        hy_filters(P, G, io, l, NL, io["zT"], emit, ob[0])
    G.KS_tok = [P.token("KS%d" % i) for i in range(32)]
    with P.scope():
        kin = [P.sb("hyf_kin%d" % i, [128, 16, 128], BF16) for i in range(2)]
        PA = P.ps("hyf_PA", [128, 4, 2, 128], F32)
        PXr = P.ps("hyf_PXr", [128, 4, 128], F32)
        PXi = P.ps("hyf_PXi", [128, 4, 128], F32)
        t1 = P.sb("hyf_t1", [128, 4, 2, 128], F32); t2 = P.sb("hyf_t2", [128, 4, 2, 128], F32)
        Ab = P.sb("hyf_Ab", [128, 4, 2, 128], BF16)
        ks = [P.sb("hyf_ks%d" % i, [128, 16, 2, 128], F32) for i in range(2)]
        ksw = [P.sb("hyf_ksw%d" % i, [128, 16, 2, 128], F32) for i in range(2)]
        kcv = kc.t.rearrange("r (a b) -> a r b", b=128)
        for g16 in range(32):
            ki = kin[g16 % 2]; ks_ = ks[g16 % 2]; ksw_ = ksw[g16 % 2]
            P.dma("sp" if g16 % 2 == 0 else "act", ki.t[:], kcv[:, g16 * 16:(g16 + 1) * 16, :], r=kc_tok, w=[ki])
            for g4 in range(4):
                fft_fwd_group(P, G, lambda c, g4=g4, ki=ki: (ki.t[:, g4 * 4 + c, :], [ki]), 128, 4, PA, PXr, PXi, t1, t2, Ab)
                sl = slice(g4 * 4, g4 * 4 + 4)
                sc = 1.0 / NFFT
                P.op("act", lambda e, sl=sl, ks_=ks_: e.activation(ks_.t[:, sl, 0, :], PXr.t[:], AF.Identity, scale=sc), r=[PXr], w=[ks_])
                P.op("act", lambda e, sl=sl, ks_=ks_: e.activation(ks_.t[:, sl, 1, :], PXi.t[:], AF.Identity, scale=sc), r=[PXi], w=[ks_])
                P.op("act", lambda e, sl=sl, ksw_=ksw_: e.activation(ksw_.t[:, sl, 0, :], PXi.t[:], AF.Identity, scale=-sc), r=[PXi], w=[ksw_])
                P.op("act", lambda e, sl=sl, ksw_=ksw_: e.activation(ksw_.t[:, sl, 1, :], PXr.t[:], AF.Identity, scale=sc), r=[PXr], w=[ksw_])
            P.dma("sp", KS.t[:, g16 * 16:(g16 + 1) * 16], ks_.t[:], r=[ks_], w=[G.KS_tok[g16]])
            P.dma("act", KSW.t[:, g16 * 16:(g16 + 1) * 16], ksw_.t[:], r=[ksw_], w=[G.KS_tok[g16]])


def phase_E_latent(P, G, io, l, uF, ymix, KS, KSW):
    hyU = P.dram("hyU%d" % l, [768, NL], F32)
    hyU_tok = [P.token("hyU%d" % i) for i in range(6)]
    hyY = P.dram("hyY%d" % l, [256, NL], BF16)
    hyY_tok = [P.token("hyY")]
    with P.scope():
        cw = P.sb("hy_cw", [128, 24], F32)
        P.dma("sp", cw.t[:], io["hy_cw"][l], w=[cw])
        for ch in range(6):
            with P.scope():
                xi = P.sb("hyc_x", [128, NL + 2], F32)
                xo = P.sb("hyc_o", [128, NL], F32)
                P.op("pool", lambda e: e.memset(xi.t[:, 0:1], 0.0), w=[xi])
                P.op("pool", lambda e: e.memset(xi.t[:, NL + 1:NL + 2], 0.0), w=[xi])
                P.dma("sp", xi.t[:, 1:NL + 1], uF.t[R_HY + ch * 128:R_HY + (ch + 1) * 128, 256:NS], r=G.uF_tok, w=[xi])
                P.op("dve", lambda e: e.tensor_scalar(xo.t[:], xi.t[:, 0:NL], cw.t[:, ch * 3:ch * 3 + 1], cw.t[:, 18 + ch:19 + ch], ALU.mult, ALU.add), r=[xi, cw], w=[xo])
                for k in (1, 2):
                    P.op("dve", lambda e, k=k: e.scalar_tensor_tensor(xo.t[:], xi.t[:, k:k + NL], cw.t[:, ch * 3 + k:ch * 3 + k + 1], xo.t[:], ALU.mult, ALU.add), r=[xi, cw, xo], w=[xo])
                P.dma("sp", hyU.t[ch * 128:(ch + 1) * 128, :], xo.t[:], r=[xo], w=[hyU_tok[ch]])
    with P.scope():
        skp = P.sb("hy_skip", [64, 512], F32)
        P.dma("sp", skp.t[:], io["hy_skip"][l].broadcast_to([64, 512]), w=[skp])
        uv = hyU.t.rearrange("(b c) (a n) -> a b c n", b=3, n=128)
        yv = hyY.t.rearrange("c (a n) -> a c n", n=128)
        NG = 8
        xin = [P.sb("hye_xin%d" % i, [64, 3, NG, 128], F32) for i in range(2)]
        yo = [P.sb("hye_yo%d" % i, [64, NG, 128], BF16) for i in range(2)]
        kt = [P.sb("hye_k%d" % i, [128, 2, NG, 2, 128], F32) for i in range(2)]
        kwt = [P.sb("hye_kw%d" % i, [128, 2, NG, 2, 128], F32) for i in range(2)]
        DF = G.DF

        class HS:
            pass
        sets = []
        for si in range(2):
            h_ = HS()
            h_.vb = P.sb("hye_vb%d" % si, [64, 4, 128], BF16)
            h_.y1f = P.sb("hye_y1f%d" % si, [64, 4, 128], F32)
            h_.y1b = P.sb("hye_y1b%d" % si, [64, 4, 128], BF16)
            h_.tt = P.sb("hye_tt%d" % si, [64, 4, 128], F32)
            h_.t1 = P.sb("hye_t1%d" % si, [128, 4, 2, 128], F32)
            h_.t2 = P.sb("hye_t2%d" % si, [128, 4, 2, 128], F32)
            h_.Ab = P.sb("hye_Ab%d" % si, [128, 4, 2, 128], BF16)
            h_.Yb = P.sb("hye_Yb%d" % si, [128, 4, 2, 128], BF16)
            h_.Bb = P.sb("hye_Bb%d" % si, [128, 4, 2, 128], BF16)
            h_.P2 = P.ps("hye_P2%d" % si, [128, 4, 2, 128], F32)
            h_.PXr = P.ps("hye_PXr%d" % si, [128, 4, 128], F32)
            h_.PXi = P.ps("hye_PXi%d" % si, [128, 4, 128], F32)
            sets.append(h_)

        def chain(h_, x_, k_, kw_, yo_, c0, g4):
            sl = slice(g4 * 4, g4 * 4 + 4)
            P2, PXr, PXi, t1, t2 = h_.P2, h_.PXr, h_.PXi, h_.t1, h_.t2
            PY = P2.t[0:64, 0:2].rearrange("p a b c -> p (a b) c")
            P.op("pool", lambda e: e.tensor_copy(h_.vb.t[:], x_.t[:, 0, sl, :]), r=[x_], w=[h_.vb])
            yield
            for o in range(2):
                src = h_.vb if o == 0 else h_.y1b
                for c in range(4):
                    P.op("pe", lambda e, c=c: e.matmul(P2.t[:, c].rearrange("p a b -> p (a b)"), src.t[:, c, :], DF.t[0:64, 0:2, :].rearrange("p a b -> p (a b)"), start=True, stop=True),
                         r=[src, DF], w=[P2])
                yield
                cplx_mul_tab(P, G, P2, 4, G.TW, 0, 2, t1, t2, h_.Ab)
                yield
                Ar = h_.Ab.t[:, :, 0, :]
                Ai = h_.Ab.t[:, :, 1, :]
                P.op("pe", lambda e: e.matmul(PXr.t[:], DF.t[:, 0, :], Ar, start=True, stop=False), r=[DF, h_.Ab], w=[PXr])
                P.op("pe", lambda e: e.matmul(PXr.t[:], DF.t[:, 3, :], Ai, start=False, stop=True), r=[DF, h_.Ab], w=[PXr])
                P.op("pe", lambda e: e.matmul(PXi.t[:], DF.t[:, 1, :], Ar, start=True, stop=False), r=[DF, h_.Ab], w=[PXi])
                P.op("pe", lambda e: e.matmul(PXi.t[:], DF.t[:, 0, :], Ai, start=False, stop=True), r=[DF, h_.Ab], w=[PXi])
                yield
                XrB = mkap(PXr.t, 0, [[PXr.t[:].ap[0][0], 128], [128, 4], [0, 2], [1, 128]])
                XiB = mkap(PXi.t, 0, [[PXi.t[:].ap[0][0], 128], [128, 4], [0, 2], [1, 128]])
                P.op("dve", lambda e, o=o: e.tensor_tensor(t1.t[:], XrB, k_.t[:, o, sl], ALU.mult), r=[PXr, k_], w=[t1])
                P.op("dve", lambda e, o=o: e.tensor_tensor(t2.t[:], XiB, kw_.t[:, o, sl], ALU.mult), r=[PXi, kw_], w=[t2])
                P.op("pool", lambda e: e.tensor_tensor(h_.Yb.t[:], t1.t[:], t2.t[:], ALU.add), r=[t1, t2], w=[h_.Yb])
                yield
                for c in range(4):
                    P.op("pe", lambda e, c=c: e.matmul(P2.t[:, c].rearrange("p a b -> p (a b)"), h_.Yb.t[:, c, 0, :], DF.t[:, 2:4, :].rearrange("p a b -> p (a b)"),
                                                      start=True, stop=False), r=[h_.Yb, DF], w=[P2])
                    P.op("pe", lambda e, c=c: e.matmul(P2.t[:, c].rearrange("p a b -> p (a b)"), h_.Yb.t[:, c, 1, :], DF.t[:, 1:3, :].rearrange("p a b -> p (a b)"),
                                                      start=False, stop=True), r=[h_.Yb, DF], w=[P2])
                yield
                cplx_mul_tab(P, G, P2, 4, G.TW, 0, 3, t1, t2, h_.Bb)
                yield
                P.op("pe", lambda e: e.matmul(PY, DF.t[:, 0, 0:64], h_.Bb.t[:, :, 0, :], start=True, stop=False), r=[DF, h_.Bb], w=[P2])
                P.op("pe", lambda e: e.matmul(PY, DF.t[:, 1, 0:64], h_.Bb.t[:, :, 1, :], start=False, stop=True), r=[DF, h_.Bb], w=[P2])
                yield
                zsrc = (x_, x_.t[:, 0, sl, :]) if o == 0 else (h_.y1f, h_.y1f.t[:])
                skb = mkap(skp.t, o * 256 + c0 + g4 * 4, [[512, 64], [1, 4], [0, 128]])
                P.op("pool", lambda e, zsrc=zsrc, skb=skb: e.tensor_tensor(h_.tt.t[:], zsrc[1], skb, ALU.mult), r=[zsrc[0], skp], w=[h_.tt])
                P.op("dve", lambda e: e.tensor_tensor(h_.tt.t[:], PY, h_.tt.t[:], ALU.add), r=[P2, h_.tt], w=[h_.tt])
                if o == 0:
                    P.op("pool", lambda e: e.tensor_tensor(h_.y1f.t[:], h_.tt.t[:], x_.t[:, 1, sl, :], ALU.mult), r=[h_.tt, x_], w=[h_.y1f])
                    P.op("act", lambda e: e.activation(h_.y1b.t[:], h_.y1f.t[:], AF.Identity), r=[h_.y1f], w=[h_.y1b])
                else:
                    P.op("pool", lambda e: e.tensor_tensor(yo_.t[:, sl, :], h_.tt.t[:], x_.t[:, 2, sl, :], ALU.mult), r=[h_.tt, x_], w=[yo_])
                yield

        for gi in range(256 // NG):
            c0 = gi * NG
            x_ = xin[gi % 2]; k_ = kt[gi % 2]; kw_ = kwt[gi % 2]; yo_ = yo[gi % 2]
            for b in range(3):
                P.dma("sp" if b != 1 else "act", x_.t[:, b], uv[:, b, c0:c0 + NG, :], r=hyU_tok, w=[x_])
            for o in range(2):
                P.dma("sp", k_.t[:, o], KS.t[:, o * 256 + c0:o * 256 + c0 + NG], r=G.KS_tok, w=[k_])
                P.dma("act", kw_.t[:, o], KSW.t[:, o * 256 + c0:o * 256 + c0 + NG], r=G.KS_tok, w=[kw_])
            gens = [chain(sets[g4], x_, k_, kw_, yo_, c0, g4) for g4 in range(2)]
            live = list(gens)
            while live:
                for g_ in list(live):
                    try:
                        next(g_)
                    except StopIteration:
                        live.remove(g_)
            P.dma("sp", yv[:, c0:c0 + NG, :], yo_.t[:], r=[yo_], w=hyY_tok)
    with P.scope():
        idx = P.sb("idx_own_e", [128, 8], U32)
        P.dma("sp", idx.t[:], io["idx_own"], w=[idx])
        for cc in range(2):
            g = P.sb("hye_g%d" % cc, [128, OWN], BF16)
            P.op("pool", lambda e, g=g, cc=cc: e.indirect_dma_start(
                out=g.t[:], out_offset=None, in_=hyY.t.rearrange("c (q s) -> (c q) s", q=4),
                in_offset=bass.IndirectOffsetOnAxis(ap=idx.t[:, cc:cc + 1], axis=0)), r=hyY_tok + [idx], w=[g], dma=True)
            P.dma("sp", ymix.t[768 + cc * 128:768 + (cc + 1) * 128, 256:NO], g.t[:], r=[g], w=[G.ymix_tok[6 + cc]])


def phase_E_ctx(P, G, io, l, uF, ymix):
    Lc = NCTX
    with P.scope():
        F5 = P.sb("F512", [128, 4, 2, 512], BF16)
        load_cast(P, F5, lambda c0, n: F5.t[:].rearrange("p a b c -> p (a b c)")[:, c0:c0 + n], io["dft512"].rearrange("p a b c -> p (a b c)"), 4096)
        idn = P.sb("ident", [128, 128], F32)
        P.dma("sp", idn.t[:], io["ident"], w=[idn])
        kcf = P.sb("hc_kcf", [128, 4, 512], F32)
        junk = P.sb("hc_junk", [128, Lc], BF16)

        def emit(q, k_):
            g, half = q // 2, q % 2
            o, dr = g // 2, g % 2
            rc = o * 2 + half
            if dr == 0:
                P.op("pool", lambda e: e.tensor_copy(kcf.t[:, rc, 0:Lc], k_.t[:]), r=[k_], w=[kcf])
            else:
                kp = k_.t[:].ap[0][0]
                P.op("pool", lambda e: e.memset(kcf.t[:, rc, Lc:Lc + 1], 0.0), w=[kcf])
                P.op("pool", lambda e: e.tensor_copy(kcf.t[:, rc, Lc + 1:2 * Lc], mkap(k_.t, Lc - 1, [[kp, 128], [-1, Lc - 1]])), r=[k_], w=[kcf])
        hy_filters(P, G, io, l, Lc, io["zTc"], emit, junk)
        kct = P.sb("hc_kct", [128, 4, 512], BF16)
        Ksp = P.sb("hc_Ksp", [128, 4, 2, 512], F32)
        Kspw = P.sb("hc_Kspw", [128, 4, 2, 512], F32)
        xct = P.sb("hc_xct", [128, 2, 768], F32)
        with P.scope():
            pt = [P.ps("hc_pt%d" % i, [128, 512], F32) for i in range(4)]
            pi = 0
            for nc_ in range(4):
                p_ = pt[pi % 4]; pi += 1
                for rc in range(4):
                    P.op("pe", lambda e, p_=p_, rc=rc, nc_=nc_: e.matmul(p_.t[:, rc * 128:(rc + 1) * 128], kcf.t[:, rc, nc_ * 128:(nc_ + 1) * 128], idn.t[:], start=True, stop=True), r=[kcf, idn], w=[p_])
                P.op("act", lambda e, p_=p_, nc_=nc_: e.activation(kct.t[:, nc_, :], p_.t[:], AF.Identity), r=[p_], w=[kct])
            for kc in range(4):
                for ri in range(2):
                    p_ = pt[pi % 4]; pi += 1
                    for nc_ in range(4):
                        P.op("pe", lambda e, p_=p_, kc=kc, ri=ri, nc_=nc_: e.matmul(p_.t[:], F5.t[:, nc_, ri, kc * 128:(kc + 1) * 128], kct.t[:, nc_, :], start=(nc_ == 0), stop=(nc_ == 3)),
                             r=[F5, kct], w=[p_])
                    sc = 1.0 / 512.0
                    P.op("act", lambda e, p_=p_, kc=kc, ri=ri: e.activation(Ksp.t[:, kc, ri, :], p_.t[:], AF.Identity, scale=sc), r=[p_], w=[Ksp])
                    P.op("dve", lambda e, kc=kc, ri=ri: e.tensor_scalar_mul(Kspw.t[:, kc, 1 - ri, :], Ksp.t[:, kc, ri, :], (1.0 if ri == 0 else -1.0)), r=[Ksp], w=[Kspw])
            cw = P.sb("hc_cw", [128, 24], F32)
            P.dma("sp", cw.t[:], io["hy_cw"][l], w=[cw])
            xi = P.sb("hc_xi", [128, 6, Lc + 2], F32)
            xo = P.sb("hc_xo", [128, 6, Lc], F32)
            P.op("pool", lambda e: e.memset(xi.t[:], 0.0), w=[xi])
            P.dma("sp", xi.t[:, :, 1:Lc + 1], uF.t.rearrange("(c p) s -> p c s", p=128)[:, 5:11, 0:Lc], r=G.uF_tok, w=[xi])
            for ch in range(6):
                P.op("dve", lambda e, ch=ch: e.tensor_scalar(xo.t[:, ch, :], xi.t[:, ch, 0:Lc], cw.t[:, ch * 3:ch * 3 + 1], cw.t[:, 18 + ch:19 + ch], ALU.mult, ALU.add), r=[xi, cw], w=[xo])
                for k in (1, 2):
                    P.op("dve", lambda e, ch=ch, k=k: e.scalar_tensor_tensor(xo.t[:, ch, :], xi.t[:, ch, k:k + Lc], cw.t[:, ch * 3 + k:ch * 3 + k + 1], xo.t[:, ch, :], ALU.mult, ALU.add),
                         r=[xi, cw, xo], w=[xo])
            for nc_ in range(2):
                for hh in range(2):
                    p_ = pt[pi % 4]; pi += 1
                    for c3 in range(3):
                        ch = hh * 3 + c3
                        P.op("pe", lambda e, p_=p_, c3=c3, ch=ch, nc_=nc_: e.matmul(p_.t[:, c3 * 128:(c3 + 1) * 128], xo.t[:, ch, nc_ * 128:(nc_ + 1) * 128], idn.t[:], start=True, stop=True), r=[xo, idn], w=[p_])
                    P.op("act", lambda e, p_=p_, nc_=nc_, hh=hh: e.activation(xct.t[:, nc_, hh * 384:(hh + 1) * 384], p_.t[:, 0:384], AF.Identity), r=[p_], w=[xct])
        with P.scope():
            skp = P.sb("hc_skip", [128, 512], F32)
            P.dma("sp", skp.t[:], io["hy_skip"][l].broadcast_to([128, 512]), w=[skp])
            px = [P.ps("hc_px%d" % i, [128, 2, 256], F32) for i in range(2)]
            py = [P.ps("hc_py%d" % i, [128, 512], F32) for i in range(2)]
            Ysp = P.sb("hc_Ysp", [128, 4, 2, 256], BF16)
            z16 = P.sb("hc_z16", [128, 2, 256], BF16)
            t1 = P.sb("hc_t1", [128, 2, 256], F32); t2 = P.sb("hc_t2", [128, 2, 256], F32)
            y1 = P.sb("hc_y1", [128, 2, 256], F32)
            y2 = P.sb("hc_y2", [128, 2, 256], F32)
            tt = P.sb("hc_tt", [128, 256], F32)
            for o in range(2):
                zb = xct if o == 0 else y1
                zap = (lambda nc_: xct.t[:, nc_, 0:256]) if o == 0 else (lambda nc_: y1.t[:, nc_, :])
                for nc_ in range(2):
                    P.op("act", lambda e, nc_=nc_, zap=zap: e.activation(z16.t[:, nc_, :], zap(nc_), AF.Identity), r=[zb], w=[z16])
                for kc in range(4):
                    p_ = px[kc % 2]
                    for ri in range(2):
                        for nc_ in range(2):
                            P.op("pe", lambda e, p_=p_, ri=ri, nc_=nc_, kc=kc: e.matmul(p_.t[:, ri, :], F5.t[:, nc_, ri, kc * 128:(kc + 1) * 128], z16.t[:, nc_, :], start=(nc_ == 0), stop=(nc_ == 1)),
                                 r=[F5, z16], w=[p_])
                    pp_ = p_.t[:].ap[0][0]
                    XrB = mkap(p_.t, 0, [[pp_, 128], [0, 2], [1, 256]])
                    XiB = mkap(p_.t, 256, [[pp_, 128], [0, 2], [1, 256]])
                    P.op("dve", lambda e, XrB=XrB, kc=kc, o=o: e.tensor_tensor(t1.t[:], XrB, Ksp.t[:, kc, :, o * 256:(o + 1) * 256], ALU.mult), r=[p_, Ksp], w=[t1])
                    P.op("dve", lambda e, XiB=XiB, kc=kc, o=o: e.tensor_tensor(t2.t[:], XiB, Kspw.t[:, kc, :, o * 256:(o + 1) * 256], ALU.mult), r=[p_, Kspw], w=[t2])
                    P.op("pool", lambda e, kc=kc: e.tensor_tensor(Ysp.t[:, kc], t1.t[:], t2.t[:], ALU.add), r=[t1, t2], w=[Ysp])
                for nc_ in range(2):
                    p_ = py[nc_]
                    i = 0
                    for kc in range(4):
                        for ri in range(2):
                            P.op("pe", lambda e, p_=p_, kc=kc, ri=ri, nc_=nc_, i=i: e.matmul(p_.t[:, 0:256], F5.t[:, kc, ri, nc_ * 128:(nc_ + 1) * 128], Ysp.t[:, kc, ri, :], start=(i == 0), stop=(i == 7)),
                                 r=[F5, Ysp], w=[p_])
                            i += 1
                    P.op("pool", lambda e, nc_=nc_, o=o, zap=zap: e.tensor_tensor(tt.t[:], zap(nc_), skp.t[:, o * 256:(o + 1) * 256], ALU.mult), r=[zb, skp], w=[tt])
                    P.op("dve", lambda e, p_=p_: e.tensor_tensor(tt.t[:], p_.t[:, 0:256], tt.t[:], ALU.add), r=[p_, tt], w=[tt])
                    dst = y1 if o == 0 else y2
                    P.op("pool", lambda e, nc_=nc_, o=o, dst=dst: e.tensor_tensor(dst.t[:, nc_, :], tt.t[:], xct.t[:, nc_, 256 * (o + 1):256 * (o + 2)], ALU.mult), r=[tt, xct], w=[dst])
            yb = P.sb("hc_yb", [128, 2, 256], BF16)
            for cc in range(2):
                p_ = py[cc]
                for nc_ in range(2):
                    P.op("pe", lambda e, p_=p_, cc=cc, nc_=nc_: e.matmul(p_.t[:, nc_ * 128:(nc_ + 1) * 128], y2.t[:, nc_, cc * 128:(cc + 1) * 128], idn.t[:], start=True, stop=True), r=[y2, idn], w=[p_])
                P.op("act", lambda e, p_=p_, cc=cc: e.activation(yb.t[:, cc, :], p_.t[:, 0:256], AF.Identity), r=[p_], w=[yb])
                P.dma("sp", ymix.t[768 + cc * 128:768 + (cc + 1) * 128, 0:256], yb.t[:, cc, :], r=[yb], w=[G.ymix_tok[6 + cc]])


def prep_tail(inp, S):
    f = lambda a: np.ascontiguousarray(np.asarray(a, np.float32))
    S["w_out"] = f(inp["w_out"])
    wq = f(inp["peer_wq"])
    Lr = wq.shape[0]
    S["wqT_r"] = np.ascontiguousarray(wq.reshape(Lr, D, 16, 128).transpose(0, 3, 2, 1))
    S["keysT_r"] = np.ascontiguousarray(f(inp["peer_keys"]).transpose(0, 3, 1, 2))
    pu = f(inp["peer_u"]); pv = f(inp["peer_v"])
    for l in range(Lr):
        S["peer_u%d" % l] = pu[l]
        S["peer_v%d" % l] = pv[l]
    S["final_gT"] = chunkT(inp["final_g"], 8)
    S["iota16"] = np.ascontiguousarray(np.tile(np.arange(16, dtype=np.float32)[None, :], (128, 1)))


IO_SHAPES.update({"w_out": [2, D, D], "wqT_r": [2, 128, 16, D], "keysT_r": [2, 128, 2, 128], "peer_u0": [16384, D], "peer_v0": [16384, D], "peer_u1": [16384, D], "peer_v1": [16384, D],
                  "final_gT": [128, 8], "iota16": [128, 16]})


def phase_F(P, G, io, l, need_ctx, ymix, x1T):
    ot = own_tiles(need_ctx)
    c0 = 0 if need_ctx else 256
    SRC = G.src
    x1v = x1T.t.rearrange("(k p) s -> p k s", p=128)
    G.x1_tok = [P.token("x1T%d" % i) for i in range(len(ot))]
    with P.scope():
        wo = P.sb("wo_bf", [128, 8, D], BF16)
        for k in range(8):
            load_cast(P, wo, lambda c0_, n, k=k: wo.t[:, k, c0_:c0_ + n], io["w_out"][l, k * 128:(k + 1) * 128, :], D)
        ym = P.sb("ym", [128, 8, NO], BF16)
        P.dma("sp", ym.t[:, :, c0:NO], ymix.t.rearrange("(k p) s -> p k s", p=128)[:, :, c0:NO], r=G.ymix_tok, w=[ym])
        xt = [P.sb("f_xt%d" % i, [128, 8, 512], F32) for i in range(2)]
        xo = [P.sb("f_xo%d" % i, [128, 8, 512], F32) for i in range(2)]
        pp = [P.ps("f_pp%d" % i, [128, 512], F32) for i in range(3)]
        pi = 0
        for ti, (s0, n, j) in enumerate(ot):
            x_ = xt[ti % 2]; o_ = xo[ti % 2]
            P.dma("act", x_.t[:, :, :n], (SRC["ctx"](n) if j else SRC["own"](s0 - 256, n)), r=SRC["toks"], w=[x_])
            for dm in range(8):
                p_ = pp[pi % 3]; pi += 1
                for k in range(8):
                    P.op("pe", lambda e, p_=p_, k=k, dm=dm, s0=s0, n=n: e.matmul(p_.t[:, :n], wo.t[:, k, dm * 128:(dm + 1) * 128], ym.t[:, k, s0:s0 + n], start=(k == 0), stop=(k == 7)),
                         r=[wo, ym], w=[p_])
                P.op("dve", lambda e, p_=p_, dm=dm, n=n, j=j, x_=x_, o_=o_: e.scalar_tensor_tensor(
                    o_.t[:, dm, :n], p_.t[:, :n], G.mod.t[:, 16 + dm, j:j + 1], x_.t[:, dm, :n], ALU.mult, ALU.add), r=[p_, G.mod, x_], w=[o_])
            P.dma("sp", x1v[:, :, s0:s0 + n], o_.t[:, :, :n], r=[o_], w=[G.x1_tok[ti]])


def cast_peer_tables(P, G, io, l):
    G.Ub = P.dram("peerUb%d" % l, [16384, D], BF16)
    G.Vb = P.dram("peerVb%d" % l, [16384, D], BF16)
    G.tab_tok = [P.token("ptab")]
    with P.scope():
        st = [P.sb("pc_st%d" % i, [128, 2, D], F32) for i in range(2)]
        ob = [P.sb("pc_ob%d" % i, [128, 2, D], BF16) for i in range(2)]
        i = 0
        for nm, dst in (("peer_u%d" % l, G.Ub), ("peer_v%d" % l, G.Vb)):
            src = io[nm].rearrange("(p r) d -> p r d", p=128)
            dv = dst.t.rearrange("(p r) d -> p r d", p=128)
            for r0 in range(0, 128, 2):
                def piece(i=i, src=src, dv=dv, r0=r0):
                    s_ = st[i % 2]; o_ = ob[i % 2]
                    P.dma("sp", s_.t[:], src[:, r0:r0 + 2, :], w=[s_])
                    eng = ("pool", "dve", "pool")[i % 3]
                    P.op(eng, lambda e: e.tensor_copy(o_.t[:], s_.t[:]), r=[s_], w=[o_])
                    P.dma("pool", dv[:, r0:r0 + 2, :], o_.t[:], r=[o_], w=G.tab_tok)
                G.bg.append(piece)
                i += 1
        yield


def phase_G(P, G, io, l, need_ctx, final, x1T, xoT, coT, tile_limit=None):
    x1v = x1T.t.rearrange("(k p) s -> p k s", p=128)
    xov = xoT.t.rearrange("(k p) s -> p k s", p=128)
    cov = coT.t.rearrange("(k p) s -> p k s", p=128) if coT is not None else None
    NEG = -1.0e30
    with P.scope():
        idn = P.sb("g_ident", [128, 128], F32)
        P.dma("sp", idn.t[:], io["ident"], w=[idn])
        io16 = P.sb("g_iota16", [128, 16], F32)
        P.dma("sp", io16.t[:], io["iota16"], w=[io16])
        fg = P.sb("g_fg", [128, 8], F32)
        P.dma("sp", fg.t[:], io["final_gT"], w=[fg])
        Wk = P.sb("g_Wk", [128, 8, 2048], F32)
        with P.scope():
            kT = P.sb("g_keysT", [128, 2, 128], F32)
            P.dma("sp", kT.t[:], io["keysT_r"][l], w=[kT])
            wqs = [P.sb("g_wq%d" % i, [128, D], F32) for i in range(2)]
            pw = [P.ps("g_pw%d" % i, [128, 512], F32) for i in range(8)]
            for g4 in range(4):
                for gg in range(4):
                    g = g4 * 4 + gg
                    wq_ = wqs[g % 2]
                    P.dma("sp" if g % 2 == 0 else "act", wq_.t[:], io["wqT_r"][l, :, g, :], w=[wq_])
                    for dc in range(8):
                        P.op("pe", lambda e, dc=dc, gg=gg, g=g, wq_=wq_: e.matmul(pw[dc].t[:, gg * 128:(gg + 1) * 128], wq_.t[:, dc * 128:(dc + 1) * 128], kT.t[:, g % 2, :], start=True, stop=True),
                             r=[wq_, kT], w=[pw[dc]])
                for dc in range(8):
                    if dc % 2:
                        P.op("act", lambda e, dc=dc, g4=g4: e.activation(Wk.t[:, dc, g4 * 512:(g4 + 1) * 512], pw[dc].t[:], AF.Identity), r=[pw[dc]], w=[Wk])
                    else:
                        P.op("dve", lambda e, dc=dc, g4=g4: e.tensor_copy(Wk.t[:, dc, g4 * 512:(g4 + 1) * 512], pw[dc].t[:]), r=[pw[dc]], w=[Wk])
        NB = 18
        Dg = P.sb("g_Dg", [128, 64, 128], BF16)
        gb = [P.sb("g_gb%d" % i, [128, D], BF16) for i in range(NB)]

        class RB:
            pass
        rbs = []
        for i in range(2):
            r_ = RB()
            r_.xt = P.sb("g_xt%d" % i, [128, 8, 128], F32)
            r_.hxt = P.sb("g_hxt%d" % i, [128, D], F32)
            r_.eidu = P.sb("g_eidu%d" % i, [128, 128], U32)
            r_.gate = P.sb("g_gate%d" % i, [128, 8, 16], F32)
            rbs.append(r_)
        sq = P.sb("g_sq", [128, 8, 128], BF16)
        xn = P.sb("g_xn", [128, 8, 128], F32)
        hx = P.sb("g_hx", [128, 8, 128], F32)
        rs = P.sb("g_rs", [128, 128], F32)
        rs2 = P.sb("g_rs2", [128, 128], F32)
        sq2 = P.sb("g_sq2", [128, 8, 128], BF16)
        junk = P.sb("g_junk", [128, D], BF16)
        sc = P.sb("g_sc", [128, 16, 128], F32)
        sc2 = P.sb("g_sc2", [128, 16, 128], F32)
        cand2 = sc2
        v16 = P.sb("g_v16", [128, 16, 16], F32)
        i16u = P.sb("g_i16u", [128, 16, 16], U32)
        if16 = P.sb("g_if16", [128, 16, 16], F32)
        cand = P.sb("g_cand", [128, 8, 256], F32)
        eq = P.sb("g_eq", [128, 8, 16, 16], F32)
        cs16 = P.sb("g_cs16", [128, 8, 16], F32)
        cp16 = P.sb("g_cp16", [128, 8, 16], U32)
        au = P.sb("g_au", [128, 8, 16], U32); bu = P.sb("g_bu", [128, 8, 16], U32)
        af = P.sb("g_af", [128, 8, 16], F32); bf = P.sb("g_bf", [128, 8, 16], F32)
        s1 = P.sb("g_s1", [128, 8, 16], F32); s2_ = P.sb("g_s2", [128, 8, 16], F32)
        gsum = P.sb("g_gsum", [128, 8], F32)
        dots = P.sb("g_dots", [128, 128], F32)
        wts = P.sb("g_wts", [128, 128], F32)
        acc = P.sb("g_acc", [128, D], F32)
        xo = P.sb("g_xo", [128, 8, 128], F32)
        yo = P.sb("g_yo", [128, 8, 128], F32) if final else xo
        pss = P.ps("g_pss", [128, 512], F32)
        pss2 = P.ps("g_pss2", [128, 512], F32)
        psc = [P.ps("g_psc%d" % i, [128, 512], F32) for i in range(2)]
        ptr = [P.ps("g_ptr%d" % i, [128, 512], F32) for i in range(2)]
        pva = [P.ps("g_pva%d" % i, [128, 512], F32) for i in range(2)]
        ntile = NO // 128
        t0 = 0 if need_ctx else 2
        tend = ntile if tile_limit is None else tile_limit
        out_tok = [P.token("xo%d" % i) for i in range(ntile)]
        G.out_tok = out_tok
        u_tab = G.Ub.t
        v_tab = G.Vb.t
        gi = [0]

        def route(tt, rb):
            o0 = tt * 128
            j = 1 if o0 < 256 else 0
            xt, hxt, eidu, gate = rb.xt, rb.hxt, rb.eidu, rb.gate
            P.dma("sp", xt.t[:], x1v[:, :, o0:o0 + 128], r=G.x1_tok, w=[xt])
            P.op("act", lambda e: e.activation(sq.t[:], xt.t[:], AF.Square), r=[xt], w=[sq])
            yield
            for k in range(8):
                P.op("pe", lambda e, k=k: e.matmul(pss.t[:, 0:128], G.ones_bf.t[:, :], sq.t[:, k, :], start=(k == 0), stop=(k == 7)), r=[sq, G.ones_bf], w=[pss])
            P.op("act", lambda e: e.activation(rs.t[:], pss.t[:, 0:128], AF.Sqrt, bias=G.eps.t[:, 0:1], scale=1.0 / D), r=[pss, G.eps], w=[rs])
            P.op("dve", lambda e: e.reciprocal(rs.t[:], rs.t[:]), r=[rs], w=[rs])
            P.op("dve", lambda e: e.tensor_tensor(xn.t[:], xt.t[:], ins_bc(rs.t[:], 1, 8), ALU.mult), r=[xt, rs], w=[xn])
            yield
            for k in range(8):
                P.op("act", lambda e, k=k, j=j: e.activation(hx.t[:, k, :], xn.t[:, k, :], AF.Identity, scale=G.s2.t[:, k, j:j + 1], bias=G.mod.t[:, 24 + k, j:j + 1]),
                     r=[xn, G.s2, G.mod], w=[hx])
            yield
            for c4 in range(4):
                p_ = psc[c4 % 2]
                for k in range(8):
                    P.op("pe", lambda e, p_=p_, c4=c4, k=k: e.matmul(p_.t[:], hx.t[:, k, :], Wk.t[:, k, c4 * 512:(c4 + 1) * 512], start=(k == 0), stop=(k == 7)), r=[hx, Wk], w=[p_])
                P.op("act", lambda e, p_=p_, c4=c4: e.activation(sc.t[:, c4 * 4:(c4 + 1) * 4, :].rearrange("p a b -> p (a b)"), p_.t[:], AF.Identity), r=[p_], w=[sc])
                yield
            for h2 in range(2):
                for k4 in range(4):
                    k = h2 * 4 + k4
                    P.op("pe", lambda e, k=k, k4=k4, h2=h2: e.matmul(ptr[h2].t[:, k4 * 128:(k4 + 1) * 128], hx.t[:, k, :], idn.t[:], start=True, stop=True), r=[hx, idn], w=[ptr[h2]])
                P.op("act", lambda e, h2=h2: e.activation(hxt.t[:, h2 * 512:(h2 + 1) * 512], ptr[h2].t[:], AF.Identity), r=[ptr[h2]], w=[hxt])
                yield
            for g in range(16):
                P.op("dve", lambda e, g=g: e.max(v16.t[:, g, 0:8], sc.t[:, g, :]), r=[sc], w=[v16])
                P.op("dve", lambda e, g=g: e.max_index(i16u.t[:, g, 0:8], v16.t[:, g, 0:8], sc.t[:, g, :]), r=[sc, v16], w=[i16u])
                yield
                P.op("dve", lambda e, g=g: e.match_replace(sc2.t[:, g, :], v16.t[:, g, 0:8], sc.t[:, g, :], NEG), r=[sc, v16], w=[sc2])
                P.op("dve", lambda e, g=g: e.max(v16.t[:, g, 8:16], sc2.t[:, g, :]), r=[sc2], w=[v16])
                P.op("dve", lambda e, g=g: e.max_index(i16u.t[:, g, 8:16], v16.t[:, g, 8:16], sc2.t[:, g, :]), r=[sc2, v16], w=[i16u])
                yield
            P.op("dve", lambda e: e.tensor_copy(if16.t[:], i16u.t[:]), r=[i16u], w=[if16])
            vp = v16.t[:].ap[0][0]
            bcA = lambda t, off: mkap(t.t, off, [[vp, 128], [32, 8], [1, 16], [0, 16]])
            bcB = lambda t, off: mkap(t.t, off, [[vp, 128], [32, 8], [0, 16], [1, 16]])
            c4d = cand.t[:].rearrange("p h (a b) -> p h a b", b=16)
            P.op("dve", lambda e: e.tensor_tensor(c4d, bcA(v16, 0), bcB(v16, 16), ALU.add), r=[v16], w=[cand])
            yield
            c2v = lambda h: cand2.t[:].rearrange("p a b -> p (a b)")[:, h * 256:(h + 1) * 256]
            for h in range(8):
                P.op("dve", lambda e, h=h: e.max(cs16.t[:, h, 0:8], cand.t[:, h, :]), r=[cand], w=[cs16])
                P.op("dve", lambda e, h=h: e.max_index(cp16.t[:, h, 0:8], cs16.t[:, h, 0:8], cand.t[:, h, :]), r=[cand, cs16], w=[cp16])
                yield
                P.op("dve", lambda e, h=h: e.match_replace(c2v(h), cs16.t[:, h, 0:8], cand.t[:, h, :], NEG), r=[cand, cs16], w=[cand2])
                P.op("dve", lambda e, h=h: e.max(cs16.t[:, h, 8:16], c2v(h)), r=[cand2], w=[cs16])
                P.op("dve", lambda e, h=h: e.max_index(cp16.t[:, h, 8:16], cs16.t[:, h, 8:16], c2v(h)), r=[cand2, cs16], w=[cp16])
                yield
            P.op("dve", lambda e: e.tensor_single_scalar(au.t[:], cp16.t[:], 4, ALU.logical_shift_right), r=[cp16], w=[au])
            P.op("dve", lambda e: e.tensor_single_scalar(bu.t[:], cp16.t[:], 15, ALU.bitwise_and), r=[cp16], w=[bu])
            P.op("dve", lambda e: e.tensor_copy(af.t[:], au.t[:]), r=[au], w=[af])
            P.op("dve", lambda e: e.tensor_copy(bf.t[:], bu.t[:]), r=[bu], w=[bf])
            yield
            for (sel, posf, off) in ((s1, af, 0), (s2_, bf, 16)):
                pf = posf.t[:].ap[0][0]
                posB = mkap(posf.t, 0, [[pf, 128], [16, 8], [1, 16], [0, 16]])
                iotB = mkap(io16.t, 0, [[16, 128], [0, 8], [0, 16], [1, 16]])
                valB = mkap(if16.t, off, [[vp, 128], [32, 8], [0, 16], [1, 16]])
                P.op("dve", lambda e, posB=posB, iotB=iotB: e.tensor_tensor(eq.t[:], posB, iotB, ALU.is_equal), r=[posf, io16], w=[eq])
                P.op("dve", lambda e, valB=valB: e.tensor_tensor(eq.t[:], eq.t[:], valB, ALU.mult), r=[eq, if16], w=[eq])
                P.op("dve", lambda e, sel=sel: e.tensor_reduce(sel.t[:], eq.t[:], AX.X, ALU.add), r=[eq], w=[sel])
                yield
            P.op("dve", lambda e: e.scalar_tensor_tensor(s1.t[:], s1.t[:], 128.0, s2_.t[:], ALU.mult, ALU.add), r=[s1, s2_], w=[s1])
            P.op("dve", lambda e: e.tensor_copy(eidu.t[:], s1.t[:].rearrange("p a b -> p (a b)")), r=[s1], w=[eidu])
            cp_ = cs16.t[:].ap[0][0]
            mxB = mkap(cs16.t, 0, [[cp_, 128], [16, 8], [0, 16]])
            P.op("dve", lambda e, mxB=mxB: e.tensor_tensor(gate.t[:], cs16.t[:], mxB, ALU.subtract), r=[cs16], w=[gate])
            P.op("act", lambda e: e.activation(gate.t[:], gate.t[:], AF.Exp), r=[gate], w=[gate])
            P.op("dve", lambda e: e.tensor_reduce(gsum.t[:], gate.t[:], AX.X, ALU.add), r=[gate], w=[gsum])
            P.op("dve", lambda e: e.reciprocal(gsum.t[:], gsum.t[:]), r=[gsum], w=[gsum])
            P.op("dve", lambda e: e.tensor_tensor(gate.t[:], gate.t[:], ins_bc(gsum.t[:], 2, 16), ALU.mult), r=[gate, gsum], w=[gate])
            yield

        def step(g_):
            if g_ is not None:
                next(g_, None)

        def experts(tt, rb, nxt):
            o0 = tt * 128
            j = 1 if o0 < 256 else 0
            xt, hxt, eidu, gate = rb.xt, rb.hxt, rb.eidu, rb.gate
            for s in range(128):
                ug = gb[gi[0] % NB]; gi[0] += 1
                P.op("pool", lambda e, ug=ug, s=s: e.indirect_dma_start(out=ug.t[:], out_offset=None, in_=u_tab,
                                                                        in_offset=bass.IndirectOffsetOnAxis(ap=eidu.t[:, s:s + 1], axis=0)), r=[eidu] + G.tab_tok, w=[ug], dma=True)
                P.op("dve", lambda e, ug=ug, s=s: e.scalar_tensor_tensor(junk.t[:], ug.t[:], 1.0, hxt.t[:], ALU.mult, ALU.mult, accum_out=dots.t[:, s:s + 1]),
                     r=[ug, hxt], w=[junk, dots])
                if s % 2 == 1:
                    step(nxt)
            P.op("act", lambda e: e.activation(dots.t[:], dots.t[:], AF.Gelu_apprx_tanh), r=[dots], w=[dots])
            P.op("dve", lambda e: e.tensor_tensor(wts.t[:], dots.t[:], gate.t[:].rearrange("p a b -> p (a b)"), ALU.mult), r=[dots, gate], w=[wts])
            wp = wts.t[:].ap[0][0]
            for s in range(128):
                if s % 64 == 0:
                    P.op("dve", lambda e, s=s: e.tensor_tensor(Dg.t[:], mkap(wts.t, s, [[wp, 128], [1, 64], [0, 128]]), mkap(idn.t, 0, [[128, 128], [0, 64], [1, 128]]), ALU.mult),
                         r=[wts, idn], w=[Dg])
                vg = gb[gi[0] % NB]; gi[0] += 1
                P.op("pool", lambda e, vg=vg, s=s: e.indirect_dma_start(out=vg.t[:], out_offset=None, in_=v_tab,
                                                                        in_offset=bass.IndirectOffsetOnAxis(ap=eidu.t[:, s:s + 1], axis=0)), r=[eidu] + G.tab_tok, w=[vg], dma=True)
                for hh in range(2):
                    P.op("pe", lambda e, vg=vg, s=s, hh=hh: e.matmul(pva[hh].t[:], Dg.t[:, s % 64, :], vg.t[:, hh * 512:(hh + 1) * 512], start=(s == 0), stop=(s == 127)),
                         r=[Dg, vg], w=[pva[hh]])
                if s % 2 == 1:
                    step(nxt)
            for hh in range(2):
                P.op("act", lambda e, hh=hh: e.activation(acc.t[:, hh * 512:(hh + 1) * 512], pva[hh].t[:], AF.Identity), r=[pva[hh]], w=[acc])
            if getattr(G, "dbgo", None) is not None and tt == 2:
                dd = G.dbgo
                P.dma("sp", dd["d_acc"], acc.t[:], r=[acc])
            for h2 in range(2):
                for k4 in range(4):
                    k = h2 * 4 + k4
                    P.op("pe", lambda e, k=k, k4=k4, h2=h2: e.matmul(ptr[h2].t[:, k4 * 128:(k4 + 1) * 128], acc.t[:, k * 128:(k + 1) * 128], idn.t[:], start=True, stop=True), r=[acc, idn], w=[ptr[h2]])
                for k4 in range(4):
                    k = h2 * 4 + k4
                    P.op("dve", lambda e, k=k, k4=k4, h2=h2, j=j: e.scalar_tensor_tensor(xo.t[:, k, :], ptr[h2].t[:, k4 * 128:(k4 + 1) * 128], G.mod.t[:, 40 + k, j:j + 1], xt.t[:, k, :],
                                                                                   ALU.mult, ALU.add), r=[ptr[h2], G.mod, xt], w=[xo])
            src = xo
            if final:
                P.op("act", lambda e: e.activation(sq2.t[:], xo.t[:], AF.Square), r=[xo], w=[sq2])
                for k in range(8):
                    P.op("pe", lambda e, k=k: e.matmul(pss2.t[:, 0:128], G.ones_bf.t[:, :], sq2.t[:, k, :], start=(k == 0), stop=(k == 7)), r=[sq2, G.ones_bf], w=[pss2])
                P.op("act", lambda e: e.activation(rs2.t[:], pss2.t[:, 0:128], AF.Sqrt, bias=G.eps.t[:, 0:1], scale=1.0 / D), r=[pss2, G.eps], w=[rs2])
                P.op("dve", lambda e: e.reciprocal(rs2.t[:], rs2.t[:]), r=[rs2], w=[rs2])
                for k in range(8):
                    P.op("dve", lambda e, k=k: e.scalar_tensor_tensor(yo.t[:, k, :], xo.t[:, k, :], fg.t[:, k:k + 1], rs2.t[:], ALU.mult, ALU.mult), r=[xo, fg, rs2], w=[yo])
                src = yo
            if j:
                P.dma("sp", cov[:, :, o0:o0 + 128], src.t[:], r=[src], w=[out_tok[tt]])
            else:
                P.dma("sp", xov[:, :, o0 - 256:o0 - 256 + 128], src.t[:], r=[src], w=[out_tok[tt]])
            if nxt is not None:
                for _ in nxt:
                    pass

        g0 = route(t0, rbs[t0 % 2])
        for _ in g0:
            pass
        for tt in range(t0, tend):
            nxt = route(tt + 1, rbs[(tt + 1) % 2]) if tt + 1 < tend else None
            experts(tt, rbs[tt % 2], nxt)


def build_layer(P, G, io, l, need_ctx, final, xoT, coT, dbg=None):
    uF = P.dram("uF%d" % l, [NFULL * 128, NS], F32)
    uO = P.dram("uO%d" % l, [NOWNC * 128, NO], F32)
    ymix = dbg["ymix"] if dbg and "ymix" in dbg else P.dram("ymix%d" % l, [D, NO], BF16)
    x1T = dbg["x1T"] if dbg and "x1T" in dbg else P.dram("x1T%d" % l, [D, NO], F32)
    KS = P.dram("KS%d" % l, [128, 512, 2, 128], F32)
    KSW = P.dram("KSW%d" % l, [128, 512, 2, 128], F32)
    phase_mod(P, G, io, l)
    phase_B(P, G, io, l, need_ctx, uF, uO)
    phase_C(P, G, io, l, need_ctx, uF, uO, ymix)
    G.bg = []
    cg = cast_peer_tables(P, G, io, l)
    next(cg)
    phase_D(P, G, io, l, need_ctx, uF, uO, ymix)
    while G.bg:
        G.bg.pop(0)()
    for _ in cg:
        pass
    phase_E_filters(P, G, io, l, KS, KSW)
    phase_E_latent(P, G, io, l, uF, ymix, KS, KSW)
    if need_ctx:
        phase_E_ctx(P, G, io, l, uF, ymix)
    phase_F(P, G, io, l, need_ctx, ymix, x1T)
    phase_G(P, G, io, l, need_ctx, final, x1T, xoT, coT)


def src_from_inputs(io):
    xfv = io["xT_full"].rearrange("(k p) s -> p k s", p=128)
    xov = io["xT_own"].rearrange("(k p) s -> p k s", p=128)
    cv = io["cT"].rearrange("(k p) s -> p k s", p=128)
    return {"full": lambda t0, n: xfv[:, :, t0:t0 + n], "own": lambda t0, n: xov[:, :, t0:t0 + n], "ctx": lambda n: cv[:, :, 0:n], "toks": []}


def build_fused_program():
    nc = bass.Bass("TRN2", target_bir_lowering=False)
    io = declare_io(nc)
    P = Prog(nc)
    G = Ctx()
    xoT = Buf("xoT", nc.dram_tensor("xoT", [D, OWN], F32, kind="ExternalOutput").ap())
    setup_globals(P, G)
    fft_consts(P, G, io)
    xo0 = P.dram("xo0", [D, OWN], F32)
    co0 = P.dram("co0", [D, NCTX], F32)
    G.src = src_from_inputs(io)
    build_layer(P, G, io, 0, True, False, xo0, co0)
    toks0 = list(G.out_tok)
    xg = P.dram("xg", [4 * D, OWN], F32)
    for k in range(8):
        P.coll(lambda e, k=k: e.collective_compute("AllGather", ALU.bypass, replica_groups=[[0, 1, 2, 3], [4, 5, 6, 7]],
                                                   ins=[xo0.t[k * 128:(k + 1) * 128, :].opt()], outs=[xg.t[k * 512:(k + 1) * 512, :].opt()]), r=toks0, w=[xg])
    xgv = xg.t.rearrange("(k r p) s -> r p k s", r=4, p=128)
    xo0v = xo0.t.rearrange("(k p) s -> p k s", p=128)
    co0v = co0.t.rearrange("(k p) s -> p k s", p=128)
    G.src = {"full": lambda t0, n: xgv[t0 // OWN][:, :, (t0 % OWN):(t0 % OWN) + n], "own": lambda t0, n: xo0v[:, :, t0:t0 + n],
             "ctx": lambda n: co0v[:, :, 0:n], "toks": [xg] + toks0}
    build_layer(P, G, io, 1, False, True, xoT, None)
    P.finish(G.out_tok)
    return nc


def kernel(**inputs):
    inp = {k: np.asarray(v) for k, v in inputs.items()}
    S = prep_shared(inp)
    nc = build_fused_program()
    maps = []
    for c in range(8):
        m = dict(S)
        m.update(prep_core(inp, c))
        maps.append(m)
    res = run_bass_kernel_spmd(nc, maps, core_ids=list(range(8)))
    x = np.asarray(inp["x"], np.float32)
    out = np.empty_like(x)
    for c in range(8):
        b, j = c // 4, c % 4
        out[b, j * OWN:(j + 1) * OWN] = res.results[c]["xoT"].T
    return out.astype(np.float32)
```

```python
from contextlib import ExitStack
import numpy as np
import concourse.bass as bass
import concourse.mybir as mybir
from concourse.bass_utils import run_bass_kernel_spmd

F32 = mybir.dt.float32
BF16 = mybir.dt.bfloat16
U32 = mybir.dt.uint32
I32 = mybir.dt.int32
AF = mybir.ActivationFunctionType
ALU = mybir.AluOpType
AX = mybir.AxisListType


def mkap(t, offset, pat):
    return bass.AP(t, offset, [list(p) for p in pat])


class Buf:
    __slots__ = ("name", "t", "last_w", "readers", "is_psum")

    def __init__(self, name, t, init_readers=None):
        self.is_psum = False
        self.name = name
        self.t = t
        self.last_w = None
        self.readers = dict(init_readers or {})


class Prog:
    NDS = 24

    def __init__(self, nc):
        self.nc = nc
        self.stack = ExitStack()
        self.eng = {"pe": nc.tensor, "act": nc.scalar, "dve": nc.vector, "pool": nc.gpsimd, "sp": nc.sync}
        self.csem = {k: self.stack.enter_context(nc.semaphore("c_" + k)) for k in self.eng}
        self.cc = {k: 0 for k in self.eng}
        self.dsem = {k: [self.stack.enter_context(nc.semaphore("d_%s%d" % (k, i))) for i in range(self.NDS)]
                     for k in ("sp", "act", "pool")}
        self.dcount = {k: 0 for k in self.dsem}
        self.waited = {k: {} for k in self.eng}
        self.free_events = {}
        self.nins = 0
        self.scopes = []

    def _track(self, b):
        if self.scopes:
            self.scopes[-1][1].append(b)
        return b

    def sb(self, name, shape, dtype):
        st = self.scopes[-1][0] if self.scopes else self.stack
        self.uid = getattr(self, "uid", 0) + 1
        name = "%s_%d" % (name, self.uid)
        t = st.enter_context(self.nc.sbuf_tensor(name, list(shape), dtype))
        return self._track(Buf(name, t, self.free_events))

    def ps(self, name, shape, dtype):
        st = self.scopes[-1][0] if self.scopes else self.stack
        self.uid = getattr(self, "uid", 0) + 1
        name = "%s_%d" % (name, self.uid)
        t = st.enter_context(self.nc.psum_tensor(name, list(shape), dtype))
        b = self._track(Buf(name, t, self.free_events))
        b.is_psum = True
        return b

    def dram(self, name, shape, dtype, kind="Internal"):
        t = self.nc.dram_tensor(name, list(shape), dtype, kind=kind)
        return Buf(name, t.ap())

    def token(self, name):
        return Buf(name, None)

    class _Scope:
        def __init__(self, p):
            self.p = p

        def __enter__(self):
            self.p.scopes.append((ExitStack(), []))
            return self

        def __exit__(self, *a):
            st, bufs = self.p.scopes.pop()
            fe = self.p.free_events
            for b in bufs:
                evs = list(b.readers.items())
                if b.last_w is not None:
                    evs.append(b.last_w)
                for k, v in evs:
                    if fe.get(k, 0) < v:
                        fe[k] = v
            st.close()
            return False

    def scope(self):
        return Prog._Scope(self)

    def _sem(self, key):
        if key[0] == "x":
            return self.xsem
        return self.csem[key[1]] if key[0] == "c" else self.dsem[key[1]][key[2]]

    def _wait(self, eng, key, val):
        if eng == "pe" and key == ("c", "pe"):
            return
        w = self.waited[eng]
        if w.get(key, 0) >= val:
            return
        w[key] = val
        self.eng[eng].wait_ge(self._sem(key), val)

    def op(self, eng, fn, r=(), w=(), dma=False):
        deps = {}
        for b in r:
            if b.last_w is not None:
                k, v = b.last_w
                if deps.get(k, 0) < v:
                    deps[k] = v
            if b.is_psum:
                for k, v in b.readers.items():
                    if k != ("c", eng) and deps.get(k, 0) < v:
                        deps[k] = v
        for b in w:
            if b.last_w is not None:
                k, v = b.last_w
                if deps.get(k, 0) < v:
                    deps[k] = v
            for k, v in b.readers.items():
                if deps.get(k, 0) < v:
                    deps[k] = v
        for k, v in deps.items():
            self._wait(eng, k, v)
        if dma:
            j = self.dcount[eng]
            self.dcount[eng] = j + 1
            slot = j % self.NDS
            key = ("d", eng, slot)
            if j >= self.NDS:
                self._wait(eng, key, 16 * (j // self.NDS))
            ev = (key, 16 * (j // self.NDS + 1))
            fn(self.eng[eng]).then_inc(self._sem(key), 16)
        else:
            self.cc[eng] += 1
            key = ("c", eng)
            ev = (key, self.cc[eng])
            fn(self.eng[eng]).then_inc(self._sem(key), 1)
        self.nins += 1
        for b in r:
            if b.readers.get(ev[0], 0) < ev[1]:
                b.readers[ev[0]] = ev[1]
        for b in w:
            b.last_w = ev
            b.readers = {}
        return ev

    def coll(self, fn, r=(), w=()):
        if not hasattr(self, "xsem"):
            self.xsem = self.stack.enter_context(self.nc.semaphore("x_coll"))
            self.xcount = 0
        deps = {}
        for b in list(r) + list(w):
            if b.last_w is not None:
                k, v = b.last_w
                deps[k] = max(deps.get(k, 0), v)
        for b in w:
            for k, v in b.readers.items():
                deps[k] = max(deps.get(k, 0), v)
        for k, v in deps.items():
            self._wait("pool", k, v)
        self.xcount += 1
        ev = (("x", "coll"), self.xcount)
        fn(self.eng["pool"]).then_inc(self.xsem)
        for b in r:
            b.readers[ev[0]] = max(b.readers.get(ev[0], 0), ev[1])
        for b in w:
            b.last_w = ev
            b.readers = {}
        return ev

    def dma(self, eng, out, in_, r=(), w=(), **kw):
        return self.op(eng, lambda e: e.dma_start(out=out, in_=in_, **kw), r=r, w=w, dma=True)

    def finish(self, out_bufs=()):
        for b in out_bufs:
            if b.last_w is not None:
                self._wait("sp", b.last_w[0], b.last_w[1])
        for k in self.eng:
            if self.cc[k] > 0:
                self._wait("sp", ("c", k), self.cc[k])
        for k in self.dsem:
            j = self.dcount[k]
            for slot in range(self.NDS):
                n = (j - slot + self.NDS - 1) // self.NDS if j > slot else 0
                if n > 0:
                    self._wait("sp", ("d", k, slot), 16 * n)
        self.stack.close()


D = 1024
NL = 8192
NCTX = 256
NS = NL + NCTX
OWN = 2048
NO = OWN + NCTX
EPS = 1e-6
D_LRU = 384
NFULL = 12
NOWNC = 6
MLA_SCALE = 96.0 ** -0.5
R_LRU, R_CKV, R_HY, R_KR, R_KRS = 0, 384, 640, 1408, 1440


def ins_bc(ap, pos, count):
    pat = [list(p) for p in ap.ap]
    pat.insert(pos, [0, count])
    return bass.AP(ap.tensor, ap.offset, pat)


def full_tiles():
    t = [(0, 256, 1)]
    for i in range(16):
        t.append((256 + 512 * i, 512, 0))
    return t


def own_tiles(need_ctx):
    t = [(0, 256, 1)] if need_ctx else []
    for i in range(4):
        t.append((256 + 512 * i, 512, 0))
    return t


class Ctx:
    pass


def load_cast(P, dst, dst_ap_fn, src_ap, ncols_total, rows=128, piece=2048, eng="pool"):
    with P.scope():
        stg = [P.sb("lc_stg%d" % i, [128, piece], F32) for i in range(2)]
        i = 0
        for c0 in range(0, ncols_total, piece):
            n = min(piece, ncols_total - c0)
            st = stg[i % 2]
            i += 1
            P.dma("sp", st.t[:rows, :n], src_ap[:, c0:c0 + n], w=[st])
            P.op(eng, lambda e, st=st, n=n, c0=c0: e.tensor_copy(dst_ap_fn(c0, n), st.t[:rows, :n]), r=[st], w=[dst])


def phase_mod(P, G, io, l):
    nc = P.nc
    G.mod = P.sb("mod", [128, 48, 2], F32)
    G.s1 = P.sb("s1", [128, 8, 2], F32)
    G.s2 = P.sb("s2", [128, 8, 2], F32)
    with P.scope():
        cv = P.sb("cv", [128, 8, 2], F32)
        sv = P.sb("sv", [128, 8, 2], F32)
        bm = P.sb("bm", [128, 48], F32)
        g12 = P.sb("g12", [128, 16], F32)
        tmp = P.sb("modtmp", [128, 8, 2], F32)
        wm = [P.sb("wm%d" % i, [128, 8, 768], F32) for i in range(2)]
        pm = P.ps("pm", [128, 48, 2], F32)
        P.dma("sp", cv.t[:], io["cvec"], w=[cv])
        P.dma("sp", bm.t[:], io["b_modT"][l], w=[bm])
        P.dma("sp", g12.t[:], io["g12T"][l], w=[g12])
        P.op("act", lambda e: e.activation(sv.t[:], cv.t[:], AF.Silu), r=[cv], w=[sv])
        wmv = io["w_mod"][l].rearrange("(k p) n -> p k n", p=128)
        for cb in range(8):
            w = wm[cb % 2]
            P.dma("sp" if cb % 2 == 0 else "act", w.t[:], wmv[:, :, cb * 768:(cb + 1) * 768], w=[w])
            for m in range(6):
                k6 = cb * 6 + m
                for k in range(8):
                    P.op("pe", lambda e, w=w, m=m, k=k, k6=k6: e.matmul(
                        pm.t[:, k6, :], w.t[:, k, m * 128:(m + 1) * 128], sv.t[:, k, :],
                        start=(k == 0), stop=(k == 7)), r=[w, sv], w=[pm])
        P.op("dve", lambda e: e.tensor_tensor(G.mod.t[:], pm.t[:], ins_bc(bm.t[:], 2, 2), ALU.add), r=[pm, bm], w=[G.mod])
        for (s, sc0, gcol) in ((G.s1, 8, 0), (G.s2, 32, 8)):
            P.op("dve", lambda e, sc0=sc0: e.tensor_scalar_add(tmp.t[:], G.mod.t[:, sc0:sc0 + 8, :], 1.0), r=[G.mod], w=[tmp])
            P.op("dve", lambda e, s=s, gcol=gcol: e.tensor_tensor(s.t[:], tmp.t[:], ins_bc(g12.t[:, gcol:gcol + 8], 2, 2), ALU.mult),
                 r=[tmp, g12], w=[s])


def norm_proj(P, G, name, src_fn, tiles, s_buf, sh_idx, w_bf, nch, dst_fn, dst_toks, out_dt=F32):
    with P.scope():
        xt = [P.sb(name + "xt%d" % i, [128, 8, 512], F32) for i in range(2)]
        sq = P.sb(name + "sq", [128, 8, 512], BF16)
        xn = P.sb(name + "xn", [128, 8, 512], F32)
        hx = [P.sb(name + "hx%d" % i, [128, 8, 512], BF16) for i in range(2)]
        rs = P.sb(name + "rs", [128, 512], F32)
        stage = [P.sb(name + "stg%d" % i, [128, nch, 512], out_dt) for i in range(2)]
        ssp = P.ps(name + "ssp", [128, 512], F32)
        pp = [P.ps(name + "pp%d" % i, [128, 512], F32) for i in range(3)]
        mi = 0
        for ti, (s0, n, j) in enumerate(tiles):
            x = xt[ti % 2]
            h = hx[ti % 2]
            stg = stage[ti % 2]
            P.dma("sp", x.t[:, :, :n], src_fn(s0, n, j), r=G.src["toks"], w=[x])
            P.op("act", lambda e, x=x, n=n: e.activation(sq.t[:, :, :n], x.t[:, :, :n], AF.Square), r=[x], w=[sq])
            for k in range(8):
                P.op("pe", lambda e, k=k, n=n: e.matmul(ssp.t[:, :n], G.ones_bf.t[:, :], sq.t[:, k, :n], start=(k == 0), stop=(k == 7)),
                     r=[sq, G.ones_bf], w=[ssp])
            P.op("act", lambda e, n=n: e.activation(rs.t[:, :n], ssp.t[:, :n], AF.Sqrt, bias=G.eps.t[:, 0:1], scale=1.0 / D), r=[ssp, G.eps], w=[rs])
            P.op("dve", lambda e, n=n: e.reciprocal(rs.t[:, :n], rs.t[:, :n]), r=[rs], w=[rs])
            P.op("dve", lambda e, x=x, n=n: e.tensor_tensor(xn.t[:, :, :n], x.t[:, :, :n], ins_bc(rs.t[:, :n], 1, 8), ALU.mult), r=[x, rs], w=[xn])
            for k in range(8):
                P.op("act", lambda e, h=h, k=k, n=n, j=j: e.activation(
                    h.t[:, k, :n], xn.t[:, k, :n], AF.Identity, scale=s_buf.t[:, k, j:j + 1], bias=G.mod.t[:, sh_idx + k, j:j + 1]),
                    r=[xn, s_buf, G.mod], w=[h])
            for m in range(nch):
                p_ = pp[mi % 3]
                for k in range(8):
                    P.op("pe", lambda e, p_=p_, m=m, k=k, n=n, h=h: e.matmul(
                        p_.t[:, :n], w_bf.t[:, k, m * 128:(m + 1) * 128], h.t[:, k, :n], start=(k == 0), stop=(k == 7)),
                        r=[w_bf, h], w=[p_])
                if mi % 2 == 0:
                    P.op("act", lambda e, p_=p_, m=m, n=n, stg=stg: e.activation(stg.t[:, m, :n], p_.t[:, :n], AF.Identity), r=[p_], w=[stg])
                else:
                    P.op("dve", lambda e, p_=p_, m=m, n=n, stg=stg: e.tensor_copy(stg.t[:, m, :n], p_.t[:, :n]), r=[p_], w=[stg])
                mi += 1
            P.dma("sp" if ti % 2 == 0 else "pool", dst_fn(s0, n), stg.t[:, :, :n], r=[stg], w=[dst_toks[ti]])


ROPE_PERM = list(range(8, 16)) + list(range(0, 8)) + list(range(24, 32)) + list(range(16, 24))


def chunkT(v, nch):
    return np.ascontiguousarray(np.asarray(v, np.float32).reshape(nch, 128).T)


def prep_shared(inp):
    f = lambda a: np.ascontiguousarray(np.asarray(a, np.float32))
    S = {}
    w_in = f(inp["w_in"])
    Lr = w_in.shape[0]
    wf = np.zeros((Lr, D, NFULL * 128), np.float32)
    wf[:, :, R_LRU:R_LRU + 384] = w_in[:, :, 0:384]
    wf[:, :, R_CKV:R_CKV + 256] = w_in[:, :, 1152:1408]
    wf[:, :, R_HY:R_HY + 768] = w_in[:, :, 1440:2208]
    wf[:, :, R_KR:R_KR + 32] = w_in[:, :, 1408:1440]
    wf[:, :, R_KRS:R_KRS + 32] = w_in[:, :, 1408:1440][:, :, ROPE_PERM]
    S["w_inF"] = wf
    S["w_inO"] = np.ascontiguousarray(np.concatenate([w_in[:, :, 384:768], w_in[:, :, 768:1152]], axis=2))
    S["w_mod"] = f(inp["w_mod"])
    S["b_modT"] = np.stack([chunkT(inp["b_mod"][l], 48) for l in range(Lr)])
    prep_lru(inp, S)
    prep_mla(inp, S)
    prep_hyena(inp, S)
    prep_tail(inp, S)
    S["g12T"] = np.stack([np.concatenate([chunkT(inp["norm1_g"][l], 8), chunkT(inp["norm2_g"][l], 8)], axis=1) for l in range(Lr)])
    return S


def prep_core(inp, core):
    b, j = core // 4, core % 4
    x = np.asarray(inp["x"], np.float32)
    C = {}
    C["xT_full"] = np.ascontiguousarray(x[b].T)
    C["xT_own"] = np.ascontiguousarray(x[b, j * OWN:(j + 1) * OWN].T)
    C["cT"] = np.ascontiguousarray(np.asarray(inp["ctx"], np.float32)[b].T)
    cv = np.stack([chunkT(inp["c"][b], 8), chunkT(inp["c_ctx"], 8)], axis=2)
    C["cvec"] = np.ascontiguousarray(cv)
    prep_mla_core(C, j)
    C["idx_own"] = np.ascontiguousarray(((np.arange(8)[None, :] * 128 + np.arange(128)[:, None]) * 4 + j).astype(np.uint32))
    return C


IO_SHAPES = {
    "xT_full": [D, NL], "xT_own": [D, OWN], "cT": [D, NCTX], "cvec": [128, 8, 2],
    "w_mod": [2, D, 6144], "b_modT": [2, 128, 48], "g12T": [2, 128, 16],
    "w_inF": [2, D, NFULL * 128], "w_inO": [2, D, NOWNC * 128],
}


def declare_io(nc, names=None):
    io = {}
    for k, shp in IO_SHAPES.items():
        if names is None or k in names:
            io[k] = nc.dram_tensor(k, shp, IO_DT.get(k, F32), kind="ExternalInput").ap()
    return io


def setup_globals(P, G):
    G.ones_bf = P.sb("ones_bf", [128, 128], BF16)
    G.eps = P.sb("eps", [128, 1], F32)
    P.op("pool", lambda e: e.memset(G.ones_bf.t[:], 1.0), w=[G.ones_bf])
    P.op("pool", lambda e: e.memset(G.eps.t[:], EPS), w=[G.eps])
    G.ones_f = P.sb("ones_f", [128, 64], F32)
    P.op("pool", lambda e: e.memset(G.ones_f.t[:], 1.0), w=[G.ones_f])
    G.one = P.sb("one", [128, 1], F32)
    P.op("pool", lambda e: e.memset(G.one.t[:], 1.0), w=[G.one])
    G.ymix_tok = [P.token("ymix%d" % i) for i in range(8)]


def phase_B(P, G, io, l, need_ctx, uF, uO, dbg=False):
    ft = full_tiles()
    G.uF_tok = [P.token("uF%d" % i) for i in range(len(ft))]
    SRC = G.src
    uFv = uF.t.rearrange("(c p) s -> p c s", p=128)
    uOv = uO.t.rearrange("(c p) s -> p c s", p=128)
    with P.scope():
        wF = P.sb("wF_bf", [128, 8, NFULL * 128], BF16)
        for k in range(8):
            load_cast(P, wF, lambda c0, n, k=k: wF.t[:, k, c0:c0 + n], io["w_inF"][l, k * 128:(k + 1) * 128, :], NFULL * 128)
        norm_proj(P, G, "nf", lambda s0, n, j: (SRC["ctx"](n) if j else SRC["full"](s0 - 256, n)), ft, G.s1, 0, wF, NFULL,
                  lambda s0, n: uFv[:, :, s0:s0 + n], G.uF_tok)
    ot = own_tiles(need_ctx)
    G.uO_tok = [P.token("uO%d" % i) for i in range(len(ot))]
    with P.scope():
        wO = P.sb("wO_bf", [128, 8, NOWNC * 128], BF16)
        for k in range(8):
            load_cast(P, wO, lambda c0, n, k=k: wO.t[:, k, c0:c0 + n], io["w_inO"][l, k * 128:(k + 1) * 128, :], NOWNC * 128)
        norm_proj(P, G, "no", lambda s0, n, j: (SRC["ctx"](n) if j else SRC["own"](s0 - 256, n)), ot, G.s1, 0, wO, NOWNC,
                  lambda s0, n: uOv[:, :, s0:s0 + n], G.uO_tok)


def prep_lru(inp, S):
    f = lambda a: np.asarray(a, np.float32)
    Lr = f(inp["lru_wr"]).shape[0]
    wbd = np.zeros((Lr, 128, 12, 128), np.float32)
    for l in range(Lr):
        for d in range(2):
            for gi, nm in enumerate(("lru_wr", "lru_wi")):
                w = f(inp[nm])[l, d]
                for cc in range(3):
                    q = (d * 2 + gi) * 3 + cc
                    wbd[l, 0:64, q, 0:64] = w[2 * cc]
                    wbd[l, 64:128, q, 64:128] = w[2 * cc + 1]
    S["lru_wbd"] = wbd
    vec = np.zeros((Lr, 128, 6 + 6 + 6 + 12 + 3), np.float32)
    for l in range(Lr):
        for d in range(2):
            vec[l, :, d * 3:d * 3 + 3] = chunkT(inp["lru_br"][l, d], 3)
            vec[l, :, 6 + d * 3:6 + d * 3 + 3] = chunkT(inp["lru_bi"][l, d], 3)
            vec[l, :, 12 + d * 3:12 + d * 3 + 3] = chunkT(inp["lru_lambda"][l, d], 3)
        for k in range(4):
            vec[l, :, 18 + k:18 + 12:4] = chunkT(inp["lru_conv_w"][l, k], 3)
        vec[l, :, 30:33] = chunkT(inp["lru_conv_b"][l], 3)
    S["lru_vec"] = vec


IO_SHAPES.update({"lru_wbd": [2, 128, 12, 128], "lru_vec": [2, 128, 33], "idx_own": [128, 8]})
IO_DT = {"idx_own": U32}


def phase_C(P, G, io, l, need_ctx, uF, uO, ymix):
    ft = full_tiles()
    with P.scope():
        wbd = P.sb("lru_wbd", [128, 12, 128], BF16)
        load_cast(P, wbd, lambda c0, n: wbd.t[:].rearrange("p a b -> p (a b)")[:, c0:c0 + n],
                  io["lru_wbd"][l].rearrange("p a b -> p (a b)"), 12 * 128)
        vec = P.sb("lru_vecs", [128, 33], F32)
        P.dma("sp", vec.t[:], io["lru_vec"][l], w=[vec])
        c8 = P.sb("lru_c8", [128, 12], F32)
        P.op("act", lambda e: e.activation(c8.t[:, 0:6], vec.t[:, 12:18], AF.Exp, scale=-1.0), r=[vec], w=[c8])
        P.op("act", lambda e: e.activation(c8.t[:, 0:6], c8.t[:, 0:6], AF.Ln, bias=G.one.t[:, 0:1], scale=1.0), r=[c8, G.one], w=[c8])
        P.op("dve", lambda e: e.tensor_scalar_mul(c8.t[:, 6:12], c8.t[:, 0:6], -16.0), r=[c8], w=[c8])
        P.op("dve", lambda e: e.tensor_scalar_mul(c8.t[:, 0:6], c8.t[:, 0:6], -8.0), r=[c8], w=[c8])
        idx = P.sb("idx_own", [128, 8], U32)
        P.dma("sp", idx.t[:], io["idx_own"], w=[idx])
        hL = [P.dram("hL%d_%d" % (l, d), [384, NL], F32) for d in range(2)]
        hC = [P.dram("hC%d_%d" % (l, d), [384, NCTX], F32) for d in range(2)]
        for cc in range(3):
            with P.scope():
                X1 = P.sb("lruX1", [128, NS + 8], F32)
                xc = P.sb("lruxc", [128, NS], F32)
                xcb = P.sb("lruxcb", [128, NS], BF16)
                Bf = P.sb("lruB", [128, NS], F32)
                T = P.sb("lruT", [128, NS], F32)
                pr = [P.ps("lrupr%d" % i, [128, 512], F32) for i in range(4)]
                P.op("pool", lambda e: e.memset(X1.t[:, 0:264], 0.0), w=[X1])
                P.op("pool", lambda e: e.memset(X1.t[:, NS:NS + 8], 0.0), w=[X1])
                rows = slice(R_LRU + cc * 128, R_LRU + (cc + 1) * 128)
                P.dma("sp", X1.t[:, 2:258], uF.t[rows, 0:256], r=G.uF_tok, w=[X1])
                P.dma("act", X1.t[:, 261:261 + NL], uF.t[rows, 256:NS], r=G.uF_tok, w=[X1])
                for (o0, n, b0) in ((0, 256, 0), (256, NL, 259)):
                    P.op("dve", lambda e, o0=o0, n=n, b0=b0: e.tensor_scalar(
                        xc.t[:, o0:o0 + n], X1.t[:, b0:b0 + n], vec.t[:, 18 + cc * 4:19 + cc * 4], vec.t[:, 30 + cc:31 + cc], ALU.mult, ALU.add),
                        r=[X1, vec], w=[xc])
                    for k in range(1, 4):
                        P.op("dve", lambda e, o0=o0, n=n, b0=b0, k=k: e.scalar_tensor_tensor(
                            xc.t[:, o0:o0 + n], X1.t[:, b0 + k:b0 + k + n], vec.t[:, 18 + cc * 4 + k:19 + cc * 4 + k], xc.t[:, o0:o0 + n],
                            ALU.mult, ALU.add), r=[X1, vec, xc], w=[xc])
                P.op("pool", lambda e: e.tensor_copy(xcb.t[:], xc.t[:]), r=[xc], w=[xcb])
                A = X1
                for d in range(2):
                    pi = 0
                    for (s0, n, j) in ft:
                        for gi, dst in ((0, A), (1, Bf)):
                            p_ = pr[pi % 4]
                            pi += 1
                            q = (d * 2 + gi) * 3 + cc
                            P.op("pe", lambda e, p_=p_, q=q, s0=s0, n=n: e.matmul(p_.t[:, :n], wbd.t[:, q, :], xcb.t[:, s0:s0 + n], start=True, stop=True),
                                 r=[wbd, xcb], w=[p_])
                            P.op("act", lambda e, p_=p_, dst=dst, s0=s0, n=n, gi=gi, d=d: e.activation(
                                dst.t[:, s0:s0 + n], p_.t[:, :n], AF.Sigmoid, bias=vec.t[:, gi * 6 + d * 3 + cc:gi * 6 + d * 3 + cc + 1]),
                                r=[p_, vec], w=[dst])
                    P.op("act", lambda e, d=d: e.activation(T.t[:], A.t[:, 0:NS], AF.Exp, scale=c8.t[:, 6 + d * 3 + cc:7 + d * 3 + cc]), r=[A, c8], w=[T])
                    P.op("act", lambda e, d=d: e.activation(A.t[:, 0:NS], A.t[:, 0:NS], AF.Exp, scale=c8.t[:, d * 3 + cc:d * 3 + cc + 1]), r=[A, c8], w=[A])
                    P.op("act", lambda e: e.activation(T.t[:], T.t[:], AF.Sqrt, bias=G.one.t[:, 0:1], scale=-1.0), r=[T, G.one], w=[T])
                    P.op("dve", lambda e: e.tensor_tensor(Bf.t[:], Bf.t[:], T.t[:], ALU.mult), r=[Bf, T], w=[Bf])
                    P.op("pool", lambda e: e.tensor_tensor(Bf.t[:], Bf.t[:], xc.t[:], ALU.mult), r=[Bf, xc], w=[Bf])
                    ps_ = Bf.t[:].ap[0][0]
                    if d == 0:
                        P.op("dve", lambda e: e.tensor_tensor_scan(Bf.t[:], A.t[:, 0:NS], Bf.t[:], 0.0, ALU.mult, ALU.add), r=[A, Bf], w=[Bf])
                    else:
                        pa = A.t[:].ap[0][0]
                        rv = lambda t, pst, lo, n: mkap(t.t, lo + n - 1, [[pst, 128], [-1, n]])
                        P.op("dve", lambda e: e.tensor_tensor_scan(rv(Bf, ps_, 0, 256), rv(A, pa, 0, 256), rv(Bf, ps_, 0, 256), 0.0, ALU.mult, ALU.add),
                             r=[A, Bf], w=[Bf])
                        P.op("dve", lambda e: e.tensor_tensor_scan(rv(Bf, ps_, 256, NL), rv(A, pa, 256, NL), rv(Bf, ps_, 256, NL), Bf.t[:, 0:1],
                                                                   ALU.mult, ALU.add), r=[A, Bf], w=[Bf])
                    P.dma("sp", hC[d].t[cc * 128:(cc + 1) * 128, :], Bf.t[:, 0:256], r=[Bf], w=[hC[d]])
                    P.dma("sp", hL[d].t[cc * 128:(cc + 1) * 128, :], Bf.t[:, 256:NS], r=[Bf], w=[hL[d]])
        for cc in range(3):
            with P.scope():
                g0 = P.sb("lrug0", [128, NO], F32)
                g1 = P.sb("lrug1", [128, NO], F32)
                gt = P.sb("lrugt", [128, NO], F32)
                yo = P.sb("lruyo", [128, NO], BF16)
                c0 = 0 if need_ctx else 256
                for d, g in ((0, g0), (1, g1)):
                    if need_ctx:
                        P.dma("sp", g.t[:, 0:256], hC[d].t[cc * 128:(cc + 1) * 128, :], r=[hC[d]], w=[g])
                    P.op("pool", lambda e, g=g, d=d: e.indirect_dma_start(
                        out=g.t[:, 256:NO], out_offset=None, in_=hL[d].t.rearrange("c (q s) -> (c q) s", q=4),
                        in_offset=bass.IndirectOffsetOnAxis(ap=idx.t[:, cc:cc + 1], axis=0)), r=[hL[d], idx], w=[g], dma=True)
                P.dma("sp", gt.t[:, c0:NO], uO.t[cc * 128:(cc + 1) * 128, c0:NO], r=G.uO_tok, w=[gt])
                P.op("act", lambda e: e.activation(gt.t[:, c0:NO], gt.t[:, c0:NO], AF.Gelu_apprx_tanh), r=[gt], w=[gt])
                P.op("dve", lambda e: e.tensor_tensor(g0.t[:, c0:NO], g0.t[:, c0:NO], g1.t[:, c0:NO], ALU.add), r=[g0, g1], w=[g0])
                P.op("dve", lambda e: e.tensor_tensor(yo.t[:, c0:NO], g0.t[:, c0:NO], gt.t[:, c0:NO], ALU.mult), r=[g0, gt], w=[yo])
                P.dma("sp", ymix.t[cc * 128:(cc + 1) * 128, c0:NO], yo.t[:, c0:NO], r=[yo], w=[G.ymix_tok[cc]])


def rope_tables(pos):
    pos = np.asarray(pos)
    inv = (np.float32(10000.0) ** (-np.arange(8, dtype=np.float32) / np.float32(8))).astype(np.float32)
    row = (pos // 64).astype(np.float32)
    col = (pos % 64).astype(np.float32)
    Cc = np.ones((32, len(pos)), np.float32)
    Ss = np.zeros((32, len(pos)), np.float32)
    for r in range(32):
        ang = (row if r < 16 else col) * inv[r % 8]
        c, s = np.cos(ang.astype(np.float32)), np.sin(ang.astype(np.float32))
        Cc[r] = np.where(pos >= 0, c, 1.0)
        Ss[r] = np.where(pos >= 0, (-s if (r % 16) < 8 else s), 0.0)
    return Cc.astype(np.float32), Ss.astype(np.float32)


def prep_mla(inp, S):
    f = lambda a: np.asarray(a, np.float32)
    wkvb = f(inp["mla_wkvb"])
    Lr = wkvb.shape[0]
    w4 = wkvb.reshape(Lr, 256, 6, 2, 64)
    S["wkvb_r"] = np.ascontiguousarray(np.concatenate([w4[:, :, :, 0, :].reshape(Lr, 256, 384), w4[:, :, :, 1, :].reshape(Lr, 256, 384)], axis=2))
    wqb = f(inp["mla_wqb"]).reshape(Lr, 384, 6, 96)
    sw = wqb.copy()
    sw[..., 64:96] = wqb[..., 64:96][..., ROPE_PERM]
    S["wq_ext"] = np.ascontiguousarray(np.stack([wqb, sw], axis=3).reshape(Lr, 384, 6 * 2 * 96))
    S["mla_g"] = np.stack([np.concatenate([chunkT(inp["mla_q_norm_g"][l], 3), chunkT(inp["mla_kv_norm_g"][l], 2)], axis=1) for l in range(Lr)])
    pos = np.concatenate([-np.ones(NCTX, np.int64), np.arange(NL)])
    Cc, Ss = rope_tables(pos)
    S["ropeK"] = np.ascontiguousarray(np.stack([Cc, Ss]))


def prep_mla_core(C, j):
    pos = np.concatenate([-np.ones(NCTX, np.int64), np.arange(j * OWN, (j + 1) * OWN)])
    Cc, Ss = rope_tables(pos)
    Cq = np.ones((96, NO), np.float32); Cq[64:96] = Cc
    Sq = np.zeros((96, NO), np.float32); Sq[64:96] = Ss
    C["ropeQ"] = np.ascontiguousarray(np.stack([Cq, Sq]))


IO_SHAPES.update({"wkvb_r": [2, 256, 768], "wq_ext": [2, 384, 1152], "mla_g": [2, 128, 5], "ropeK": [2, 32, NS], "ropeQ": [2, 96, NO]})


def rms_fm(P, G, name, src_fn, tiles, nk, dim, gains, gcol, dst, src_toks):
    with P.scope():
        xt = [P.sb(name + "x%d" % i, [128, nk, 512], F32) for i in range(2)]
        sq = P.sb(name + "sq", [128, nk, 512], BF16)
        rs = P.sb(name + "rs", [128, 512], F32)
        ssp = P.ps(name + "ss", [128, 512], F32)
        for ti, (s0, n, j) in enumerate(tiles):
            x = xt[ti % 2]
            P.dma("sp" if ti % 2 == 0 else "act", x.t[:, :, :n], src_fn(s0, n), r=src_toks, w=[x])
            P.op("act", lambda e, x=x, n=n: e.activation(sq.t[:, :, :n], x.t[:, :, :n], AF.Square), r=[x], w=[sq])
            for k in range(nk):
                P.op("pe", lambda e, k=k, n=n: e.matmul(ssp.t[:, :n], G.ones_bf.t[:, :], sq.t[:, k, :n], start=(k == 0), stop=(k == nk - 1)),
                     r=[sq, G.ones_bf], w=[ssp])
            P.op("act", lambda e, n=n: e.activation(rs.t[:, :n], ssp.t[:, :n], AF.Sqrt, bias=G.eps.t[:, 0:1], scale=1.0 / dim), r=[ssp, G.eps], w=[rs])
            P.op("dve", lambda e, n=n: e.reciprocal(rs.t[:, :n], rs.t[:, :n]), r=[rs], w=[rs])
            for k in range(nk):
                P.op("dve", lambda e, x=x, k=k, n=n, s0=s0: e.scalar_tensor_tensor(
                    dst.t[:, k, s0:s0 + n], x.t[:, k, :n], gains.t[:, gcol + k:gcol + k + 1], rs.t[:, :n], ALU.mult, ALU.mult),
                    r=[x, gains, rs], w=[dst])


def phase_D(P, G, io, l, need_ctx, uF, uO, ymix):
    ft = full_tiles()
    ot = own_tiles(need_ctx)
    with P.scope():
        mg = P.sb("mla_g", [128, 5], F32)
        P.dma("sp", mg.t[:], io["mla_g"][l], w=[mg])
        wkv = P.sb("wkv_bf", [128, 2, 768], BF16)
        for k in range(2):
            load_cast(P, wkv, lambda c0, n, k=k: wkv.t[:, k, c0:c0 + n], io["wkvb_r"][l, k * 128:(k + 1) * 128, :], 768)
        wq = P.sb("wq_bf", [128, 3, 1152], BF16)
        for k in range(3):
            load_cast(P, wq, lambda c0, n, k=k: wq.t[:, k, c0:c0 + n], io["wq_ext"][l, k * 128:(k + 1) * 128, :], 1152)
        ckvn = P.sb("ckvn", [128, 2, NS], BF16)
        uFv = uF.t.rearrange("(c p) s -> p c s", p=128)
        uOv = uO.t.rearrange("(c p) s -> p c s", p=128)
        rms_fm(P, G, "kvn", lambda s0, n: uFv[:, 3:5, s0:s0 + n], ft, 2, 256, mg, 3, ckvn, G.uF_tok)
        cqn = P.sb("cqn", [128, 3, NO], BF16)
        rms_fm(P, G, "qn", lambda s0, n: uOv[:, 3:6, s0:s0 + n], ot, 3, 384, mg, 0, cqn, G.uO_tok)
        krb = P.sb("krb", [96, NS], BF16)
        with P.scope():
            n = 2112
            a = P.sb("kr_a", [96, n], F32); b_ = P.sb("kr_b", [96, n], F32)
            tc_ = P.sb("kr_c", [96, n], F32); ts_ = P.sb("kr_s", [96, n], F32)
            for c0 in range(0, NS, 2112):
                P.dma("sp", a.t[64:96, :], uF.t[R_KR:R_KR + 32, c0:c0 + n], r=G.uF_tok, w=[a])
                P.dma("act", b_.t[64:96, :], uF.t[R_KRS:R_KRS + 32, c0:c0 + n], r=G.uF_tok, w=[b_])
                P.dma("sp", tc_.t[64:96, :], io["ropeK"][0, :, c0:c0 + n], w=[tc_])
                P.dma("act", ts_.t[64:96, :], io["ropeK"][1, :, c0:c0 + n], w=[ts_])
                P.op("dve", lambda e, a=a, tc_=tc_: e.tensor_tensor(a.t[64:96, :], a.t[64:96, :], tc_.t[64:96, :], ALU.mult), r=[a, tc_], w=[a])
                P.op("pool", lambda e, b_=b_, ts_=ts_: e.tensor_tensor(b_.t[64:96, :], b_.t[64:96, :], ts_.t[64:96, :], ALU.mult), r=[b_, ts_], w=[b_])
                P.op("dve", lambda e, a=a, b_=b_, c0=c0, n=n: e.tensor_tensor(krb.t[64:96, c0:c0 + n], a.t[64:96, :], b_.t[64:96, :], ALU.add), r=[a, b_], w=[krb])
        tq = P.sb("ropeQ", [96, 2, NO], F32)
        P.dma("sp", tq.t[:, 0, :], io["ropeQ"][0], w=[tq])
        P.dma("sp", tq.t[:, 1, :], io["ropeQ"][1], w=[tq])
        for pr in range(3):
            with P.scope():
                Kh = [P.sb("Kh%d" % i, [96, NS], BF16) for i in range(2)]
                Vp = P.sb("Vp", [128, 66, 2, 65], BF16)
                Qh = [P.sb("Qh%d" % i, [96, NO], BF16) for i in range(2)]
                with P.scope():
                    pk = [P.ps("pk%d" % i, [128, 512], F32) for i in range(4)]
                    t1 = P.sb("qt1", [96, 512], F32); t2 = P.sb("qt2", [96, 512], F32)
                    pi = 0
                    for i in range(2):
                        h = 2 * pr + i
                        for (s0, n, j) in ft:
                            p_ = pk[pi % 4]; pi += 1
                            for k in range(2):
                                P.op("pe", lambda e, p_=p_, k=k, h=h, s0=s0, n=n: e.matmul(p_.t[0:64, :n], wkv.t[:, k, h * 64:(h + 1) * 64], ckvn.t[:, k, s0:s0 + n],
                                                                                     start=(k == 0), stop=(k == 1)), r=[wkv, ckvn], w=[p_])
                            if pi % 2:
                                P.op("act", lambda e, p_=p_, i=i, s0=s0, n=n: e.activation(Kh[i].t[0:64, s0:s0 + n], p_.t[0:64, :n], AF.Identity), r=[p_], w=[Kh[i]])
                            else:
                                P.op("dve", lambda e, p_=p_, i=i, s0=s0, n=n: e.tensor_copy(Kh[i].t[0:64, s0:s0 + n], p_.t[0:64, :n]), r=[p_], w=[Kh[i]])
                        P.op("pool", lambda e, i=i: e.tensor_copy(Kh[i].t[64:96, :], krb.t[64:96, :]), r=[krb], w=[Kh[i]])
                        for (s0, n, j) in ot:
                            pa = pk[pi % 4]; pi += 1
                            pb = pk[pi % 4]; pi += 1
                            for v, p_ in ((0, pa), (1, pb)):
                                for k in range(3):
                                    c0 = (h * 2 + v) * 96
                                    P.op("pe", lambda e, p_=p_, k=k, c0=c0, s0=s0, n=n: e.matmul(p_.t[0:96, :n], wq.t[:, k, c0:c0 + 96], cqn.t[:, k, s0:s0 + n],
                                                                                          start=(k == 0), stop=(k == 2)), r=[wq, cqn], w=[p_])
                            P.op("dve", lambda e, pa=pa, s0=s0, n=n: e.tensor_tensor(t1.t[:, :n], pa.t[0:96, :n], tq.t[:, 0, s0:s0 + n], ALU.mult), r=[pa, tq], w=[t1])
                            P.op("dve", lambda e, pb=pb, s0=s0, n=n: e.tensor_tensor(t2.t[:, :n], pb.t[0:96, :n], tq.t[:, 1, s0:s0 + n], ALU.mult), r=[pb, tq], w=[t2])
                            P.op("pool", lambda e, i=i, s0=s0, n=n: e.tensor_tensor(Qh[i].t[:, s0:s0 + n], t1.t[:, :n], t2.t[:, :n], ALU.add), r=[t1, t2], w=[Qh[i]])
                    P.op("pool", lambda e: e.memset(Vp.t[:, :, :, 64:65], 1.0), w=[Vp])
                    for kc4 in range(0, 66, 4):
                        p_ = pk[pi % 4]; pi += 1
                        nk4 = min(4, 66 - kc4)
                        for q in range(nk4):
                            kc = kc4 + q
                            for k in range(2):
                                P.op("pe", lambda e, p_=p_, q=q, k=k, kc=kc: e.matmul(p_.t[:, q * 128:(q + 1) * 128], ckvn.t[:, k, kc * 128:(kc + 1) * 128],
                                                                                wkv.t[:, k, 384 + pr * 128:384 + (pr + 1) * 128], start=(k == 0), stop=(k == 1)),
                                     r=[wkv, ckvn], w=[p_])
                        src4 = p_.t[:, :nk4 * 128].rearrange("p (a b c) -> p a b c", b=2, c=64)
                        if (kc4 // 4) % 2:
                            P.op("act", lambda e, src4=src4, kc4=kc4, nk4=nk4: e.activation(Vp.t[:, kc4:kc4 + nk4, :, 0:64], src4, AF.Identity), r=[p_], w=[Vp])
                        else:
                            P.op("dve", lambda e, src4=src4, kc4=kc4, nk4=nk4: e.tensor_copy(Vp.t[:, kc4:kc4 + nk4, :, 0:64], src4), r=[p_], w=[Vp])
                with P.scope():
                    st = [P.ps("att_st%d" % i, [128, 512], F32) for i in range(4)]
                    pn = [P.ps("att_n%d" % i, [128, 512], F32) for i in range(2)]
                    pbc = P.ps("att_bc", [64, 512], F32)
                    pt = [P.sb("att_pt%d" % i, [128, 512], BF16) for i in range(4)]
                    rec = P.sb("att_rec", [128, 512], F32)
                    rbc = P.sb("att_rbc", [64, 512], F32)
                    ob = [P.sb("att_o%d" % i, [64, 512], BF16) for i in range(2)]
                    ci = 0
                    qi = 0
                    for i in range(2):
                        h = 2 * pr + i
                        for (s0, n, j) in ot:
                            nkc = 2 if j else 66
                            pn_, o_ = pn[qi % 2], ob[qi % 2]
                            qi += 1
                            def emit_pv(kc, p_, pn_=pn_, i=i, n=n, nkc=nkc):
                                P.op("pe", lambda e: e.matmul(pn_.t[0:65, :n], Vp.t[:, kc, i, :], p_.t[:, :n], start=(kc == 0), stop=(kc == nkc - 1)), r=[Vp, p_], w=[pn_])
                            prev = None
                            for kc in range(nkc):
                                if kc % 8 == 7 and G.bg:
                                    G.bg.pop(0)()
                                s_ = st[ci % 4]; p_ = pt[ci % 4]; ci += 1
                                P.op("pe", lambda e, s_=s_, i=i, kc=kc, s0=s0, n=n: e.matmul(s_.t[:, :n], Kh[i].t[0:96, kc * 128:(kc + 1) * 128], Qh[i].t[0:96, s0:s0 + n],
                                                                                       start=True, stop=True), r=[Kh[i], Qh[i]], w=[s_])
                                P.op("act", lambda e, s_=s_, p_=p_, n=n: e.activation(p_.t[:, :n], s_.t[:, :n], AF.Exp, scale=MLA_SCALE), r=[s_], w=[p_])
                                if prev is not None:
                                    emit_pv(*prev)
                                prev = (kc, p_)
                            emit_pv(*prev)
                            P.op("dve", lambda e, pn_=pn_, n=n: e.reciprocal(rec.t[64:65, :n], pn_.t[64:65, :n]), r=[pn_], w=[rec])
                            P.op("pe", lambda e, n=n: e.matmul(pbc.t[:, :n], G.ones_f.t[64:65, 0:64], rec.t[64:65, :n], start=True, stop=True), r=[G.ones_f, rec], w=[pbc])
                            P.op("act", lambda e, n=n: e.activation(rbc.t[:, :n], pbc.t[:, :n], AF.Identity), r=[pbc], w=[rbc])
                            P.op("dve", lambda e, pn_=pn_, o_=o_, n=n: e.tensor_tensor(o_.t[:, :n], pn_.t[0:64, :n], rbc.t[:, :n], ALU.mult), r=[pn_, rbc], w=[o_])
                            P.dma("sp", ymix.t[384 + h * 64:384 + (h + 1) * 64, s0:s0 + n], o_.t[:, :n], r=[o_], w=[G.ymix_tok[3 + pr]])


NFFT = 16384
MAGIC = 12582912.0
TWO_PI = 6.283185307179586


def prep_hyena(inp, S):
    f = lambda a: np.asarray(a, np.float32)
    Lr = f(inp["hy_f_w1"]).shape[0]
    n = np.arange(128)
    ang = 2.0 * np.pi * np.outer(n, n) / 128.0
    Fr, Fi = np.cos(ang), -np.sin(ang)
    S["dft128"] = np.ascontiguousarray(np.stack([Fr, Fi, Fr, -Fi], axis=1).astype(np.float32))
    th = 2.0 * np.pi * np.outer(n, n) / NFFT
    c, s = np.cos(th), np.sin(th)
    S["tw128"] = np.ascontiguousarray(np.stack([c, c, s, -s, s], axis=1).astype(np.float32))
    S["ident"] = np.eye(128, dtype=np.float32)

    def zfeat(L):
        t = (np.arange(L, dtype=np.float32) / np.float32(L)).astype(np.float32)
        bands = np.linspace(1e-4, 7, 8, dtype=np.float32)
        wpos = (np.float32(2.0 * np.pi) * t[:, None] * bands[None, :]).astype(np.float32)
        z = np.concatenate([t[:, None], np.cos(wpos), np.sin(wpos)], axis=-1).astype(np.float32)
        return np.ascontiguousarray(z.T)
    S["zT"] = zfeat(NL)
    S["zTc"] = zfeat(NCTX)
    S["hy_w1"] = f(inp["hy_f_w1"])
    S["hy_w2"] = f(inp["hy_f_w2"])
    S["hy_w3"] = f(inp["hy_f_w3"])
    hv = np.zeros((Lr, 128, 3), np.float32)
    hv[:, 0:64, 0] = f(inp["hy_f_freq"]); hv[:, 0:64, 1] = f(inp["hy_f_b1"]); hv[:, 0:64, 2] = f(inp["hy_f_b2"])
    S["hy_vec"] = hv
    S["hy_decT"] = np.stack([chunkT(f(inp["hy_decay"])[l].reshape(1024), 8) for l in range(Lr)])
    cw = f(inp["hy_conv_w"]); cb = f(inp["hy_conv_b"])
    S["hy_cw"] = np.stack([np.concatenate([np.stack([chunkT(cw[l, k], 6) for k in range(3)], axis=2).reshape(128, 18), chunkT(cb[l], 6)], axis=1) for l in range(Lr)])
    S["hy_skip"] = np.ascontiguousarray(f(inp["hy_skip"]).reshape(Lr, 1, 512))
    k5 = np.arange(512)
    a5 = 2.0 * np.pi * np.outer(k5, k5) / 512.0
    F5 = np.stack([np.cos(a5), -np.sin(a5)], axis=1).astype(np.float32)
    S["dft512"] = np.ascontiguousarray(F5.reshape(4, 128, 2, 512).transpose(1, 0, 2, 3))


IO_SHAPES.update({"dft128": [128, 4, 128], "tw128": [128, 5, 128], "ident": [128, 128], "zT": [17, NL], "zTc": [17, NCTX],
                  "hy_w1": [2, 17, 64], "hy_w2": [2, 64, 64], "hy_w3": [2, 64, 1024], "hy_vec": [2, 128, 3], "hy_decT": [2, 128, 8],
                  "hy_cw": [2, 128, 24], "hy_skip": [2, 1, 512], "dft512": [128, 4, 2, 512]})


def hy_sin_layer(P, G, name, ps_in, n, hv, fcol, dst_ap, tmp, dst_buf):
    a, kq = tmp
    P.op("dve", lambda e: e.tensor_scalar(a.t[0:64, :n], ps_in.t[0:64, :n], hv.t[0:64, 0:1], hv.t[0:64, fcol:fcol + 1], ALU.mult, ALU.add), r=[ps_in, hv], w=[a])
    P.op("dve", lambda e: e.tensor_scalar(kq.t[0:64, :n], a.t[0:64, :n], 1.0 / TWO_PI, MAGIC, ALU.mult, ALU.add), r=[a], w=[kq])
    P.op("dve", lambda e: e.tensor_scalar(kq.t[0:64, :n], kq.t[0:64, :n], MAGIC, -TWO_PI, ALU.subtract, ALU.mult), r=[kq], w=[kq])
    P.op("dve", lambda e: e.tensor_tensor(a.t[0:64, :n], a.t[0:64, :n], kq.t[0:64, :n], ALU.add), r=[a, kq], w=[a])
    P.op("dve", lambda e: e.tensor_scalar(a.t[0:64, :n], a.t[0:64, :n], -3.14159, 3.14159, ALU.max, ALU.min), r=[a], w=[a])
    P.op("act", lambda e: e.activation(dst_ap, a.t[0:64, :n], AF.Sin), r=[a], w=[dst_buf])


def hy_filters(P, G, io, l, L, zT_ap, emit_chunk, junk):
    with P.scope():
        hv = P.sb("hy_vec", [128, 5], F32)
        P.dma("sp", hv.t[:, 0:3], io["hy_vec"][l], w=[hv])
        P.op("dve", lambda e: e.tensor_tensor(hv.t[:, 3:5], hv.t[:, 1:3], mkap(hv.t, 0, [[5, 128], [0, 2]]), ALU.mult), r=[hv], w=[hv])
        dec = P.sb("hy_dec", [128, 8], F32)
        P.dma("sp", dec.t[:], io["hy_decT"][l], w=[dec])
        P.op("dve", lambda e: e.tensor_scalar_mul(dec.t[:], dec.t[:], -1.0), r=[dec], w=[dec])
        w1 = P.sb("hy_w1", [17, 64], F32); w2 = P.sb("hy_w2", [64, 64], F32); w3 = P.sb("hy_w3", [64, 1024], F32)
        P.dma("sp", w1.t[:], io["hy_w1"][l], w=[w1]); P.dma("sp", w2.t[:], io["hy_w2"][l], w=[w2]); P.dma("sp", w3.t[:], io["hy_w3"][l], w=[w3])
        zts = [P.sb("hy_zt%d" % i, [17, 512], F32) for i in range(2)]
        tb = P.sb("hy_tb", [128, L], F32)
        P.dma("sp", tb.t[:], zT_ap[0:1, :].broadcast_to([128, L]), w=[tb])
        h2 = P.sb("hy_h2", [64, L], F32)
        tiles = [(s0, min(512, L - s0)) for s0 in range(0, L, 512)]
        with P.scope():
            h1 = P.sb("hy_h1", [64, L], F32)
            a = P.sb("hy_a", [64, 512], F32); kq = P.sb("hy_kq", [64, 512], F32)
            pp = [P.ps("hy_pp%d" % i, [64, 512], F32) for i in range(2)]
            for ti, (s0, n) in enumerate(tiles):
                p_ = pp[ti % 2]
                zt = zts[ti % 2]
                P.dma("sp", zt.t[:, :n], zT_ap[:, s0:s0 + n], w=[zt])
                P.op("pe", lambda e, p_=p_, zt=zt, n=n: e.matmul(p_.t[:, :n], w1.t[:, :], zt.t[:, :n], start=True, stop=True), r=[w1, zt], w=[p_])
                hy_sin_layer(P, G, "s1", p_, n, hv, 3, h1.t[:, s0:s0 + n], (a, kq), h1)
            for ti, (s0, n) in enumerate(tiles):
                p_ = pp[ti % 2]
                P.op("pe", lambda e, p_=p_, s0=s0, n=n: e.matmul(p_.t[:, :n], w2.t[:, :], h1.t[:, s0:s0 + n], start=True, stop=True), r=[w2, h1], w=[p_])
                hy_sin_layer(P, G, "s2", p_, n, hv, 4, h2.t[:, s0:s0 + n], (a, kq), h2)
        with P.scope():
            kk = [P.sb("hy_kk%d" % i, [128, L], F32) for i in range(2)]
            et = [P.sb("hy_et%d" % i, [128, 512], F32) for i in range(2)]
            ss = P.sb("hy_ss", [128, 1], F32)
            pk = [P.ps("hy_pk%d" % i, [128, 512], F32) for i in range(2)]
            for q in range(8):
                k_ = kk[q % 2]
                for ti, (s0, n) in enumerate(tiles):
                    p_ = pk[ti % 2]; e_ = et[ti % 2]
                    P.op("pe", lambda e, p_=p_, s0=s0, n=n, q=q: e.matmul(p_.t[:, :n], w3.t[:, q * 128:(q + 1) * 128], h2.t[:, s0:s0 + n], start=True, stop=True), r=[w3, h2], w=[p_])
                    P.op("act", lambda e, e_=e_, s0=s0, n=n, q=q: e.activation(e_.t[:, :n], tb.t[:, s0:s0 + n], AF.Exp, scale=dec.t[:, q:q + 1]), r=[tb, dec], w=[e_])
                    P.op("dve", lambda e, p_=p_, e_=e_, k_=k_, s0=s0, n=n: e.tensor_tensor(k_.t[:, s0:s0 + n], p_.t[:, :n], e_.t[:, :n], ALU.mult), r=[p_, e_], w=[k_])
                P.op("act", lambda e, k_=k_: e.activation(junk.t[:], k_.t[:], AF.Square, accum_out=ss.t[:, 0:1]), r=[k_], w=[junk, ss])
                P.op("act", lambda e: e.activation(ss.t[:], ss.t[:], AF.Sqrt, bias=G.eps.t[:, 0:1], scale=1.0), r=[ss, G.eps], w=[ss])
                P.op("dve", lambda e: e.reciprocal(ss.t[:], ss.t[:]), r=[ss], w=[ss])
                P.op("dve", lambda e, k_=k_: e.tensor_scalar(k_.t[:], k_.t[:], ss.t[:, 0:1], None, ALU.mult), r=[k_, ss], w=[k_])
                emit_chunk(q, k_)


def fft_consts(P, G, io):
    G.DF = P.sb("dft_bf", [128, 4, 128], BF16)
    load_cast(P, G.DF, lambda c0, n: G.DF.t[:].rearrange("p a b -> p (a b)")[:, c0:c0 + n], io["dft128"].rearrange("p a b -> p (a b)"), 512)
    G.TW = P.sb("tw", [128, 5, 128], F32)
    P.dma("sp", G.TW.t[:], io["tw128"], w=[G.TW])


def cplx_mul_tab(P, G, src_ps, nch, tab, i1, i2, t1, t2, dst):
    tp = tab.t[:].ap[0][0]
    sp_ = src_ps.t[:].ap[0][0]
    tabA = mkap(tab.t, i1 * 128, [[tp, 128], [0, nch], [128, 2], [1, 128]])
    tabB = mkap(tab.t, i2 * 128, [[tp, 128], [0, nch], [128, 2], [1, 128]])
    srcA = mkap(src_ps.t, 0, [[sp_, 128], [256, nch], [128, 2], [1, 128]])
    srcSw = mkap(src_ps.t, 128, [[sp_, 128], [256, nch], [-128, 2], [1, 128]])
    P.op("dve", lambda e: e.tensor_tensor(t1.t[:, 0:nch], srcA, tabA, ALU.mult), r=[src_ps, tab], w=[t1])
    P.op("dve", lambda e: e.tensor_tensor(t2.t[:, 0:nch], srcSw, tabB, ALU.mult), r=[src_ps, tab], w=[t2])
    P.op("pool", lambda e: e.tensor_tensor(dst.t[:, 0:nch], t1.t[:, 0:nch], t2.t[:, 0:nch], ALU.add), r=[t1, t2], w=[dst])


def fft_fwd_group(P, G, xin_fn, K, nch, PA, PXr, PXi, t1, t2, Ab):
    DF = G.DF
    for c in range(nch):
        ap, bufs = xin_fn(c)
        P.op("pe", lambda e, c=c, ap=ap: e.matmul(PA.t[:, c].rearrange("p a b -> p (a b)"), ap, DF.t[0:K, 0:2, :].rearrange("p a b -> p (a b)"), start=True, stop=True),
             r=bufs + [DF], w=[PA])
    cplx_mul_tab(P, G, PA, nch, G.TW, 0, 2, t1, t2, Ab)
    Ar = Ab.t[:, 0:nch, 0, :]
    Ai = Ab.t[:, 0:nch, 1, :]
    P.op("pe", lambda e: e.matmul(PXr.t[:, 0:nch, :], DF.t[:, 0, :], Ar, start=True, stop=False), r=[DF, Ab], w=[PXr])
    P.op("pe", lambda e: e.matmul(PXr.t[:, 0:nch, :], DF.t[:, 3, :], Ai, start=False, stop=True), r=[DF, Ab], w=[PXr])
    P.op("pe", lambda e: e.matmul(PXi.t[:, 0:nch, :], DF.t[:, 1, :], Ar, start=True, stop=False), r=[DF, Ab], w=[PXi])
    P.op("pe", lambda e: e.matmul(PXi.t[:, 0:nch, :], DF.t[:, 0, :], Ai, start=False, stop=True), r=[DF, Ab], w=[PXi])


def phase_E_filters(P, G, io, l, KS, KSW):
    kc = P.dram("hy_kc%d" % l, [512, NFFT], BF16)
    kc_tok = [P.token("kc%d" % q) for q in range(8)]
    with P.scope():
        ob = [P.sb("hy_ob0", [128, NL], BF16)] * 2

        def emit(q, k_):
            g, half = q // 2, q % 2
            o, dr = g // 2, g % 2
            o_ = ob[q % 2]
            rows = slice(o * 256 + half * 128, o * 256 + (half + 1) * 128)
            if dr == 0:
                P.op("pool", lambda e: e.tensor_copy(o_.t[:], k_.t[:]), r=[k_], w=[o_])
                P.dma("sp", kc.t[rows, 0:NL], o_.t[:], r=[o_], w=[kc_tok[q]])
            else:
                kp = k_.t[:].ap[0][0]
                P.op("pool", lambda e: e.memset(o_.t[:, 0:1], 0.0), w=[o_])
                P.op("pool", lambda e: e.tensor_copy(o_.t[:, 1:NL], mkap(k_.t, NL - 1, [[kp, 128], [-1, NL - 1]])), r=[k_], w=[o_])
                P.dma("sp", kc.t[rows, NL:2 * NL], o_.t[:], r=[o_], w=[kc_tok[q]])
        hy_filters(P, G, io, l, NL, io["zT"], emit, ob[0])
    G.KS_tok = [P.token("KS%d" % i) for i in range(32)]
    with P.scope():
        kin = [P.sb("hyf_kin%d" % i, [128, 16, 128], BF16) for i in range(2)]
        PA = P.ps("hyf_PA", [128, 4, 2, 128], F32)
        PXr = P.ps("hyf_PXr", [128, 4, 128], F32)
        PXi = P.ps("hyf_PXi", [128, 4, 128], F32)
        t1 = P.sb("hyf_t1", [128, 4, 2, 128], F32); t2 = P.sb("hyf_t2", [128, 4, 2, 128], F32)
        Ab = P.sb("hyf_Ab", [128, 4, 2, 128], BF16)
        ks = [P.sb("hyf_ks%d" % i, [128, 16, 2, 128], F32) for i in range(2)]
        ksw = [P.sb("hyf_ksw%d" % i, [128, 16, 2, 128], F32) for i in range(2)]
        kcv = kc.t.rearrange("r (a b) -> a r b", b=128)
        for g16 in range(32):
            ki = kin[g16 % 2]; ks_ = ks[g16 % 2]; ksw_ = ksw[g16 % 2]
            P.dma("sp" if g16 % 2 == 0 else "act", ki.t[:], kcv[:, g16 * 16:(g16 + 1) * 16, :], r=kc_tok, w=[ki])
            for g4 in range(4):
                fft_fwd_group(P, G, lambda c, g4=g4, ki=ki: (ki.t[:, g4 * 4 + c, :], [ki]), 128, 4, PA, PXr, PXi, t1, t2, Ab)
                sl = slice(g4 * 4, g4 * 4 + 4)
                sc = 1.0 / NFFT
                P.op("act", lambda e, sl=sl, ks_=ks_: e.activation(ks_.t[:, sl, 0, :], PXr.t[:], AF.Identity, scale=sc), r=[PXr], w=[ks_])
                P.op("act", lambda e, sl=sl, ks_=ks_: e.activation(ks_.t[:, sl, 1, :], PXi.t[:], AF.Identity, scale=sc), r=[PXi], w=[ks_])
                P.op("act", lambda e, sl=sl, ksw_=ksw_: e.activation(ksw_.t[:, sl, 0, :], PXi.t[:], AF.Identity, scale=-sc), r=[PXi], w=[ksw_])
                P.op("act", lambda e, sl=sl, ksw_=ksw_: e.activation(ksw_.t[:, sl, 1, :], PXr.t[:], AF.Identity, scale=sc), r=[PXr], w=[ksw_])
            P.dma("sp", KS.t[:, g16 * 16:(g16 + 1) * 16], ks_.t[:], r=[ks_], w=[G.KS_tok[g16]])
            P.dma("act", KSW.t[:, g16 * 16:(g16 + 1) * 16], ksw_.t[:], r=[ksw_], w=[G.KS_tok[g16]])


def phase_E_latent(P, G, io, l, uF, ymix, KS, KSW):
    hyU = P.dram("hyU%d" % l, [768, NL], F32)
    hyU_tok = [P.token("hyU%d" % i) for i in range(6)]
    hyY = P.dram("hyY%d" % l, [256, NL], BF16)
    hyY_tok = [P.token("hyY")]
    with P.scope():
        cw = P.sb("hy_cw", [128, 24], F32)
        P.dma("sp", cw.t[:], io["hy_cw"][l], w=[cw])
        for ch in range(6):
            with P.scope():
                xi = P.sb("hyc_x", [128, NL + 2], F32)
                xo = P.sb("hyc_o", [128, NL], F32)
                P.op("pool", lambda e: e.memset(xi.t[:, 0:1], 0.0), w=[xi])
                P.op("pool", lambda e: e.memset(xi.t[:, NL + 1:NL + 2], 0.0), w=[xi])
                P.dma("sp", xi.t[:, 1:NL + 1], uF.t[R_HY + ch * 128:R_HY + (ch + 1) * 128, 256:NS], r=G.uF_tok, w=[xi])
                P.op("dve", lambda e: e.tensor_scalar(xo.t[:], xi.t[:, 0:NL], cw.t[:, ch * 3:ch * 3 + 1], cw.t[:, 18 + ch:19 + ch], ALU.mult, ALU.add), r=[xi, cw], w=[xo])
                for k in (1, 2):
                    P.op("dve", lambda e, k=k: e.scalar_tensor_tensor(xo.t[:], xi.t[:, k:k + NL], cw.t[:, ch * 3 + k:ch * 3 + k + 1], xo.t[:], ALU.mult, ALU.add), r=[xi, cw, xo], w=[xo])
                P.dma("sp", hyU.t[ch * 128:(ch + 1) * 128, :], xo.t[:], r=[xo], w=[hyU_tok[ch]])
    with P.scope():
        skp = P.sb("hy_skip", [64, 512], F32)
        P.dma("sp", skp.t[:], io["hy_skip"][l].broadcast_to([64, 512]), w=[skp])
        uv = hyU.t.rearrange("(b c) (a n) -> a b c n", b=3, n=128)
        yv = hyY.t.rearrange("c (a n) -> a c n", n=128)
        NG = 8
        xin = [P.sb("hye_xin%d" % i, [64, 3, NG, 128], F32) for i in range(2)]
        yo = [P.sb("hye_yo%d" % i, [64, NG, 128], BF16) for i in range(2)]
        kt = [P.sb("hye_k%d" % i, [128, 2, NG, 2, 128], F32) for i in range(2)]
        kwt = [P.sb("hye_kw%d" % i, [128, 2, NG, 2, 128], F32) for i in range(2)]
        DF = G.DF

        class HS:
            pass
        sets = []
        for si in range(2):
            h_ = HS()
            h_.vb = P.sb("hye_vb%d" % si, [64, 4, 128], BF16)
            h_.y1f = P.sb("hye_y1f%d" % si, [64, 4, 128], F32)
            h_.y1b = P.sb("hye_y1b%d" % si, [64, 4, 128], BF16)
            h_.tt = P.sb("hye_tt%d" % si, [64, 4, 128], F32)
            h_.t1 = P.sb("hye_t1%d" % si, [128, 4, 2, 128], F32)
            h_.t2 = P.sb("hye_t2%d" % si, [128, 4, 2, 128], F32)
            h_.Ab = P.sb("hye_Ab%d" % si, [128, 4, 2, 128], BF16)
            h_.Yb = P.sb("hye_Yb%d" % si, [128, 4, 2, 128], BF16)
            h_.Bb = P.sb("hye_Bb%d" % si, [128, 4, 2, 128], BF16)
            h_.P2 = P.ps("hye_P2%d" % si, [128, 4, 2, 128], F32)
            h_.PXr = P.ps("hye_PXr%d" % si, [128, 4, 128], F32)
            h_.PXi = P.ps("hye_PXi%d" % si, [128, 4, 128], F32)
            sets.append(h_)

        def chain(h_, x_, k_, kw_, yo_, c0, g4):
            sl = slice(g4 * 4, g4 * 4 + 4)
            P2, PXr, PXi, t1, t2 = h_.P2, h_.PXr, h_.PXi, h_.t1, h_.t2
            PY = P2.t[0:64, 0:2].rearrange("p a b c -> p (a b) c")
            P.op("pool", lambda e: e.tensor_copy(h_.vb.t[:], x_.t[:, 0, sl, :]), r=[x_], w=[h_.vb])
            yield
            for o in range(2):
                src = h_.vb if o == 0 else h_.y1b
                for c in range(4):
                    P.op("pe", lambda e, c=c: e.matmul(P2.t[:, c].rearrange("p a b -> p (a b)"), src.t[:, c, :], DF.t[0:64, 0:2, :].rearrange("p a b -> p (a b)"), start=True, stop=True),
                         r=[src, DF], w=[P2])
                yield
                cplx_mul_tab(P, G, P2, 4, G.TW, 0, 2, t1, t2, h_.Ab)
                yield
                Ar = h_.Ab.t[:, :, 0, :]
                Ai = h_.Ab.t[:, :, 1, :]
                P.op("pe", lambda e: e.matmul(PXr.t[:], DF.t[:, 0, :], Ar, start=True, stop=False), r=[DF, h_.Ab], w=[PXr])
                P.op("pe", lambda e: e.matmul(PXr.t[:], DF.t[:, 3, :], Ai, start=False, stop=True), r=[DF, h_.Ab], w=[PXr])
                P.op("pe", lambda e: e.matmul(PXi.t[:], DF.t[:, 1, :], Ar, start=True, stop=False), r=[DF, h_.Ab], w=[PXi])
                P.op("pe", lambda e: e.matmul(PXi.t[:], DF.t[:, 0, :], Ai, start=False, stop=True), r=[DF, h_.Ab], w=[PXi])
                yield
                XrB = mkap(PXr.t, 0, [[PXr.t[:].ap[0][0], 128], [128, 4], [0, 2], [1, 128]])
                XiB = mkap(PXi.t, 0, [[PXi.t[:].ap[0][0], 128], [128, 4], [0, 2], [1, 128]])
                P.op("dve", lambda e, o=o: e.tensor_tensor(t1.t[:], XrB, k_.t[:, o, sl], ALU.mult), r=[PXr, k_], w=[t1])
                P.op("dve", lambda e, o=o: e.tensor_tensor(t2.t[:], XiB, kw_.t[:, o, sl], ALU.mult), r=[PXi, kw_], w=[t2])
                P.op("pool", lambda e: e.tensor_tensor(h_.Yb.t[:], t1.t[:], t2.t[:], ALU.add), r=[t1, t2], w=[h_.Yb])
                yield
                for c in range(4):
                    P.op("pe", lambda e, c=c: e.matmul(P2.t[:, c].rearrange("p a b -> p (a b)"), h_.Yb.t[:, c, 0, :], DF.t[:, 2:4, :].rearrange("p a b -> p (a b)"),
                                                      start=True, stop=False), r=[h_.Yb, DF], w=[P2])
                    P.op("pe", lambda e, c=c: e.matmul(P2.t[:, c].rearrange("p a b -> p (a b)"), h_.Yb.t[:, c, 1, :], DF.t[:, 1:3, :].rearrange("p a b -> p (a b)"),
                                                      start=False, stop=True), r=[h_.Yb, DF], w=[P2])
                yield
                cplx_mul_tab(P, G, P2, 4, G.TW, 0, 3, t1, t2, h_.Bb)
                yield
                P.op("pe", lambda e: e.matmul(PY, DF.t[:, 0, 0:64], h_.Bb.t[:, :, 0, :], start=True, stop=False), r=[DF, h_.Bb], w=[P2])
                P.op("pe", lambda e: e.matmul(PY, DF.t[:, 1, 0:64], h_.Bb.t[:, :, 1, :], start=False, stop=True), r=[DF, h_.Bb], w=[P2])
                yield
                zsrc = (x_, x_.t[:, 0, sl, :]) if o == 0 else (h_.y1f, h_.y1f.t[:])
                skb = mkap(skp.t, o * 256 + c0 + g4 * 4, [[512, 64], [1, 4], [0, 128]])
                P.op("pool", lambda e, zsrc=zsrc, skb=skb: e.tensor_tensor(h_.tt.t[:], zsrc[1], skb, ALU.mult), r=[zsrc[0], skp], w=[h_.tt])
                P.op("dve", lambda e: e.tensor_tensor(h_.tt.t[:], PY, h_.tt.t[:], ALU.add), r=[P2, h_.tt], w=[h_.tt])
                if o == 0:
                    P.op("pool", lambda e: e.tensor_tensor(h_.y1f.t[:], h_.tt.t[:], x_.t[:, 1, sl, :], ALU.mult), r=[h_.tt, x_], w=[h_.y1f])
                    P.op("act", lambda e: e.activation(h_.y1b.t[:], h_.y1f.t[:], AF.Identity), r=[h_.y1f], w=[h_.y1b])
                else:
                    P.op("pool", lambda e: e.tensor_tensor(yo_.t[:, sl, :], h_.tt.t[:], x_.t[:, 2, sl, :], ALU.mult), r=[h_.tt, x_], w=[yo_])
                yield

        for gi in range(256 // NG):
            c0 = gi * NG
            x_ = xin[gi % 2]; k_ = kt[gi % 2]; kw_ = kwt[gi % 2]; yo_ = yo[gi % 2]
            for b in range(3):
                P.dma("sp" if b != 1 else "act", x_.t[:, b], uv[:, b, c0:c0 + NG, :], r=hyU_tok, w=[x_])
            for o in range(2):
                P.dma("sp", k_.t[:, o], KS.t[:, o * 256 + c0:o * 256 + c0 + NG], r=G.KS_tok, w=[k_])
                P.dma("act", kw_.t[:, o], KSW.t[:, o * 256 + c0:o * 256 + c0 + NG], r=G.KS_tok, w=[kw_])
            gens = [chain(sets[g4], x_, k_, kw_, yo_, c0, g4) for g4 in range(2)]
            live = list(gens)
            while live:
                for g_ in list(live):
                    try:
                        next(g_)
                    except StopIteration:
                        live.remove(g_)
            P.dma("sp", yv[:, c0:c0 + NG, :], yo_.t[:], r=[yo_], w=hyY_tok)
    with P.scope():
        idx = P.sb("idx_own_e", [128, 8], U32)
        P.dma("sp", idx.t[:], io["idx_own"], w=[idx])
        for cc in range(2):
            g = P.sb("hye_g%d" % cc, [128, OWN], BF16)
            P.op("pool", lambda e, g=g, cc=cc: e.indirect_dma_start(
                out=g.t[:], out_offset=None, in_=hyY.t.rearrange("c (q s) -> (c q) s", q=4),
                in_offset=bass.IndirectOffsetOnAxis(ap=idx.t[:, cc:cc + 1], axis=0)), r=hyY_tok + [idx], w=[g], dma=True)
            P.dma("sp", ymix.t[768 + cc * 128:768 + (cc + 1) * 128, 256:NO], g.t[:], r=[g], w=[G.ymix_tok[6 + cc]])


def phase_E_ctx(P, G, io, l, uF, ymix):
    Lc = NCTX
    with P.scope():
        F5 = P.sb("F512", [128, 4, 2, 512], BF16)
        load_cast(P, F5, lambda c0, n: F5.t[:].rearrange("p a b c -> p (a b c)")[:, c0:c0 + n], io["dft512"].rearrange("p a b c -> p (a b c)"), 4096)
        idn = P.sb("ident", [128, 128], F32)
        P.dma("sp", idn.t[:], io["ident"], w=[idn])
        kcf = P.sb("hc_kcf", [128, 4, 512], F32)
        junk = P.sb("hc_junk", [128, Lc], BF16)

        def emit(q, k_):
            g, half = q // 2, q % 2
            o, dr = g // 2, g % 2
            rc = o * 2 + half
            if dr == 0:
                P.op("pool", lambda e: e.tensor_copy(kcf.t[:, rc, 0:Lc], k_.t[:]), r=[k_], w=[kcf])
            else:
                kp = k_.t[:].ap[0][0]
                P.op("pool", lambda e: e.memset(kcf.t[:, rc, Lc:Lc + 1], 0.0), w=[kcf])
                P.op("pool", lambda e: e.tensor_copy(kcf.t[:, rc, Lc + 1:2 * Lc], mkap(k_.t, Lc - 1, [[kp, 128], [-1, Lc - 1]])), r=[k_], w=[kcf])
        hy_filters(P, G, io, l, Lc, io["zTc"], emit, junk)
        kct = P.sb("hc_kct", [128, 4, 512], BF16)
        Ksp = P.sb("hc_Ksp", [128, 4, 2, 512], F32)
        Kspw = P.sb("hc_Kspw", [128, 4, 2, 512], F32)
        xct = P.sb("hc_xct", [128, 2, 768], F32)
        with P.scope():
            pt = [P.ps("hc_pt%d" % i, [128, 512], F32) for i in range(4)]
            pi = 0
            for nc_ in range(4):
                p_ = pt[pi % 4]; pi += 1
                for rc in range(4):
                    P.op("pe", lambda e, p_=p_, rc=rc, nc_=nc_: e.matmul(p_.t[:, rc * 128:(rc + 1) * 128], kcf.t[:, rc, nc_ * 128:(nc_ + 1) * 128], idn.t[:], start=True, stop=True), r=[kcf, idn], w=[p_])
                P.op("act", lambda e, p_=p_, nc_=nc_: e.activation(kct.t[:, nc_, :], p_.t[:], AF.Identity), r=[p_], w=[kct])
            for kc in range(4):
                for ri in range(2):
                    p_ = pt[pi % 4]; pi += 1
                    for nc_ in range(4):
                        P.op("pe", lambda e, p_=p_, kc=kc, ri=ri, nc_=nc_: e.matmul(p_.t[:], F5.t[:, nc_, ri, kc * 128:(kc + 1) * 128], kct.t[:, nc_, :], start=(nc_ == 0), stop=(nc_ == 3)),
                             r=[F5, kct], w=[p_])
                    sc = 1.0 / 512.0
                    P.op("act", lambda e, p_=p_, kc=kc, ri=ri: e.activation(Ksp.t[:, kc, ri, :], p_.t[:], AF.Identity, scale=sc), r=[p_], w=[Ksp])
                    P.op("dve", lambda e, kc=kc, ri=ri: e.tensor_scalar_mul(Kspw.t[:, kc, 1 - ri, :], Ksp.t[:, kc, ri, :], (1.0 if ri == 0 else -1.0)), r=[Ksp], w=[Kspw])
            cw = P.sb("hc_cw", [128, 24], F32)
            P.dma("sp", cw.t[:], io["hy_cw"][l], w=[cw])
            xi = P.sb("hc_xi", [128, 6, Lc + 2], F32)
            xo = P.sb("hc_xo", [128, 6, Lc], F32)
            P.op("pool", lambda e: e.memset(xi.t[:], 0.0), w=[xi])
            P.dma("sp", xi.t[:, :, 1:Lc + 1], uF.t.rearrange("(c p) s -> p c s", p=128)[:, 5:11, 0:Lc], r=G.uF_tok, w=[xi])
            for ch in range(6):
                P.op("dve", lambda e, ch=ch: e.tensor_scalar(xo.t[:, ch, :], xi.t[:, ch, 0:Lc], cw.t[:, ch * 3:ch * 3 + 1], cw.t[:, 18 + ch:19 + ch], ALU.mult, ALU.add), r=[xi, cw], w=[xo])
                for k in (1, 2):
                    P.op("dve", lambda e, ch=ch, k=k: e.scalar_tensor_tensor(xo.t[:, ch, :], xi.t[:, ch, k:k + Lc], cw.t[:, ch * 3 + k:ch * 3 + k + 1], xo.t[:, ch, :], ALU.mult, ALU.add),
                         r=[xi, cw, xo], w=[xo])
            for nc_ in range(2):
                for hh in range(2):
                    p_ = pt[pi % 4]; pi += 1
                    for c3 in range(3):
                        ch = hh * 3 + c3
                        P.op("pe", lambda e, p_=p_, c3=c3, ch=ch, nc_=nc_: e.matmul(p_.t[:, c3 * 128:(c3 + 1) * 128], xo.t[:, ch, nc_ * 128:(nc_ + 1) * 128], idn.t[:], start=True, stop=True), r=[xo, idn], w=[p_])
                    P.op("act", lambda e, p_=p_, nc_=nc_, hh=hh: e.activation(xct.t[:, nc_, hh * 384:(hh + 1) * 384], p_.t[:, 0:384], AF.Identity), r=[p_], w=[xct])
        with P.scope():
            skp = P.sb("hc_skip", [128, 512], F32)
            P.dma("sp", skp.t[:], io["hy_skip"][l].broadcast_to([128, 512]), w=[skp])
            px = [P.ps("hc_px%d" % i, [128, 2, 256], F32) for i in range(2)]
            py = [P.ps("hc_py%d" % i, [128, 512], F32) for i in range(2)]
            Ysp = P.sb("hc_Ysp", [128, 4, 2, 256], BF16)
            z16 = P.sb("hc_z16", [128, 2, 256], BF16)
            t1 = P.sb("hc_t1", [128, 2, 256], F32); t2 = P.sb("hc_t2", [128, 2, 256], F32)
            y1 = P.sb("hc_y1", [128, 2, 256], F32)
            y2 = P.sb("hc_y2", [128, 2, 256], F32)
            tt = P.sb("hc_tt", [128, 256], F32)
            for o in range(2):
                zb = xct if o == 0 else y1
                zap = (lambda nc_: xct.t[:, nc_, 0:256]) if o == 0 else (lambda nc_: y1.t[:, nc_, :])
                for nc_ in range(2):
                    P.op("act", lambda e, nc_=nc_, zap=zap: e.activation(z16.t[:, nc_, :], zap(nc_), AF.Identity), r=[zb], w=[z16])
                for kc in range(4):
                    p_ = px[kc % 2]
                    for ri in range(2):
                        for nc_ in range(2):
                            P.op("pe", lambda e, p_=p_, ri=ri, nc_=nc_, kc=kc: e.matmul(p_.t[:, ri, :], F5.t[:, nc_, ri, kc * 128:(kc + 1) * 128], z16.t[:, nc_, :], start=(nc_ == 0), stop=(nc_ == 1)),
                                 r=[F5, z16], w=[p_])
                    pp_ = p_.t[:].ap[0][0]
                    XrB = mkap(p_.t, 0, [[pp_, 128], [0, 2], [1, 256]])
                    XiB = mkap(p_.t, 256, [[pp_, 128], [0, 2], [1, 256]])
                    P.op("dve", lambda e, XrB=XrB, kc=kc, o=o: e.tensor_tensor(t1.t[:], XrB, Ksp.t[:, kc, :, o * 256:(o + 1) * 256], ALU.mult), r=[p_, Ksp], w=[t1])
                    P.op("dve", lambda e, XiB=XiB, kc=kc, o=o: e.tensor_tensor(t2.t[:], XiB, Kspw.t[:, kc, :, o * 256:(o + 1) * 256], ALU.mult), r=[p_, Kspw], w=[t2])
                    P.op("pool", lambda e, kc=kc: e.tensor_tensor(Ysp.t[:, kc], t1.t[:], t2.t[:], ALU.add), r=[t1, t2], w=[Ysp])
                for nc_ in range(2):
                    p_ = py[nc_]
                    i = 0
                    for kc in range(4):
                        for ri in range(2):
                            P.op("pe", lambda e, p_=p_, kc=kc, ri=ri, nc_=nc_, i=i: e.matmul(p_.t[:, 0:256], F5.t[:, kc, ri, nc_ * 128:(nc_ + 1) * 128], Ysp.t[:, kc, ri, :], start=(i == 0), stop=(i == 7)),
                                 r=[F5, Ysp], w=[p_])
                            i += 1
                    P.op("pool", lambda e, nc_=nc_, o=o, zap=zap: e.tensor_tensor(tt.t[:], zap(nc_), skp.t[:, o * 256:(o + 1) * 256], ALU.mult), r=[zb, skp], w=[tt])
                    P.op("dve", lambda e, p_=p_: e.tensor_tensor(tt.t[:], p_.t[:, 0:256], tt.t[:], ALU.add), r=[p_, tt], w=[tt])
                    dst = y1 if o == 0 else y2
                    P.op("pool", lambda e, nc_=nc_, o=o, dst=dst: e.tensor_tensor(dst.t[:, nc_, :], tt.t[:], xct.t[:, nc_, 256 * (o + 1):256 * (o + 2)], ALU.mult), r=[tt, xct], w=[dst])
            yb = P.sb("hc_yb", [128, 2, 256], BF16)
            for cc in range(2):
                p_ = py[cc]
                for nc_ in range(2):
                    P.op("pe", lambda e, p_=p_, cc=cc, nc_=nc_: e.matmul(p_.t[:, nc_ * 128:(nc_ + 1) * 128], y2.t[:, nc_, cc * 128:(cc + 1) * 128], idn.t[:], start=True, stop=True), r=[y2, idn], w=[p_])
                P.op("act", lambda e, p_=p_, cc=cc: e.activation(yb.t[:, cc, :], p_.t[:, 0:256], AF.Identity), r=[p_], w=[yb])
                P.dma("sp", ymix.t[768 + cc * 128:768 + (cc + 1) * 128, 0:256], yb.t[:, cc, :], r=[yb], w=[G.ymix_tok[6 + cc]])


def prep_tail(inp, S):
    f = lambda a: np.ascontiguousarray(np.asarray(a, np.float32))
    S["w_out"] = f(inp["w_out"])
    wq = f(inp["peer_wq"])
    Lr = wq.shape[0]
    S["wqT_r"] = np.ascontiguousarray(wq.reshape(Lr, D, 16, 128).transpose(0, 3, 2, 1))
    S["keysT_r"] = np.ascontiguousarray(f(inp["peer_keys"]).transpose(0, 3, 1, 2))
    pu = f(inp["peer_u"]); pv = f(inp["peer_v"])
    for l in range(Lr):
        S["peer_u%d" % l] = pu[l]
        S["peer_v%d" % l] = pv[l]
    S["final_gT"] = chunkT(inp["final_g"], 8)
    S["iota16"] = np.ascontiguousarray(np.tile(np.arange(16, dtype=np.float32)[None, :], (128, 1)))


IO_SHAPES.update({"w_out": [2, D, D], "wqT_r": [2, 128, 16, D], "keysT_r": [2, 128, 2, 128], "peer_u0": [16384, D], "peer_v0": [16384, D], "peer_u1": [16384, D], "peer_v1": [16384, D],
                  "final_gT": [128, 8], "iota16": [128, 16]})


def phase_F(P, G, io, l, need_ctx, ymix, x1T):
    ot = own_tiles(need_ctx)
    c0 = 0 if need_ctx else 256
    SRC = G.src
    x1v = x1T.t.rearrange("(k p) s -> p k s", p=128)
    G.x1_tok = [P.token("x1T%d" % i) for i in range(len(ot))]
    with P.scope():
        wo = P.sb("wo_bf", [128, 8, D], BF16)
        for k in range(8):
            load_cast(P, wo, lambda c0_, n, k=k: wo.t[:, k, c0_:c0_ + n], io["w_out"][l, k * 128:(k + 1) * 128, :], D)
        ym = P.sb("ym", [128, 8, NO], BF16)
        P.dma("sp", ym.t[:, :, c0:NO], ymix.t.rearrange("(k p) s -> p k s", p=128)[:, :, c0:NO], r=G.ymix_tok, w=[ym])
        xt = [P.sb("f_xt%d" % i, [128, 8, 512], F32) for i in range(2)]
        xo = [P.sb("f_xo%d" % i, [128, 8, 512], F32) for i in range(2)]
        pp = [P.ps("f_pp%d" % i, [128, 512], F32) for i in range(3)]
        pi = 0
        for ti, (s0, n, j) in enumerate(ot):
            x_ = xt[ti % 2]; o_ = xo[ti % 2]
            P.dma("act", x_.t[:, :, :n], (SRC["ctx"](n) if j else SRC["own"](s0 - 256, n)), r=SRC["toks"], w=[x_])
            for dm in range(8):
                p_ = pp[pi % 3]; pi += 1
                for k in range(8):
                    P.op("pe", lambda e, p_=p_, k=k, dm=dm, s0=s0, n=n: e.matmul(p_.t[:, :n], wo.t[:, k, dm * 128:(dm + 1) * 128], ym.t[:, k, s0:s0 + n], start=(k == 0), stop=(k == 7)),
                         r=[wo, ym], w=[p_])
                P.op("dve", lambda e, p_=p_, dm=dm, n=n, j=j, x_=x_, o_=o_: e.scalar_tensor_tensor(
                    o_.t[:, dm, :n], p_.t[:, :n], G.mod.t[:, 16 + dm, j:j + 1], x_.t[:, dm, :n], ALU.mult, ALU.add), r=[p_, G.mod, x_], w=[o_])
            P.dma("sp", x1v[:, :, s0:s0 + n], o_.t[:, :, :n], r=[o_], w=[G.x1_tok[ti]])


def cast_peer_tables(P, G, io, l):
    G.UVb = P.dram("peerUVb%d" % l, [16384, 2 * D], BF16)
    G.tab_tok = [P.token("ptab")]
    with P.scope():
        st = [P.sb("pc_st%d" % i, [128, 2, D], F32) for i in range(2)]
        ob = [P.sb("pc_ob%d" % i, [128, 2, D], BF16) for i in range(2)]
        i = 0
        for nm, half in (("peer_u%d" % l, 0), ("peer_v%d" % l, 1)):
            src = io[nm].rearrange("(p r) d -> p r d", p=128)
            dv = G.UVb.t.rearrange("(p r) d -> p r d", p=128)[:, :, half * D:(half + 1) * D]
            for r0 in range(0, 128, 2):
                def piece(i=i, src=src, dv=dv, r0=r0):
                    s_ = st[i % 2]; o_ = ob[i % 2]
                    P.dma("sp", s_.t[:], src[:, r0:r0 + 2, :], w=[s_])
                    eng = ("pool", "dve", "pool")[i % 3]
                    P.op(eng, lambda e: e.tensor_copy(o_.t[:], s_.t[:]), r=[s_], w=[o_])
                    P.dma("pool", dv[:, r0:r0 + 2, :], o_.t[:], r=[o_], w=G.tab_tok)
                G.bg.append(piece)
                i += 1
        yield


def phase_G(P, G, io, l, need_ctx, final, x1T, xoT, coT, tile_limit=None):
    x1v = x1T.t.rearrange("(k p) s -> p k s", p=128)
    xov = xoT.t.rearrange("(k p) s -> p k s", p=128)
    cov = coT.t.rearrange("(k p) s -> p k s", p=128) if coT is not None else None
    NEG = -1.0e30
    with P.scope():
        idn = P.sb("g_ident", [128, 128], F32)
        P.dma("sp", idn.t[:], io["ident"], w=[idn])
        io16 = P.sb("g_iota16", [128, 16], F32)
        P.dma("sp", io16.t[:], io["iota16"], w=[io16])
        fg = P.sb("g_fg", [128, 8], F32)
        P.dma("sp", fg.t[:], io["final_gT"], w=[fg])
        Wk = P.sb("g_Wk", [128, 8, 2048], F32)
        with P.scope():
            kT = P.sb("g_keysT", [128, 2, 128], F32)
            P.dma("sp", kT.t[:], io["keysT_r"][l], w=[kT])
            wqs = [P.sb("g_wq%d" % i, [128, D], F32) for i in range(2)]
            pw = [P.ps("g_pw%d" % i, [128, 512], F32) for i in range(8)]
            for g4 in range(4):
                for gg in range(4):
                    g = g4 * 4 + gg
                    wq_ = wqs[g % 2]
                    P.dma("sp" if g % 2 == 0 else "act", wq_.t[:], io["wqT_r"][l, :, g, :], w=[wq_])
                    for dc in range(8):
                        P.op("pe", lambda e, dc=dc, gg=gg, g=g, wq_=wq_: e.matmul(pw[dc].t[:, gg * 128:(gg + 1) * 128], wq_.t[:, dc * 128:(dc + 1) * 128], kT.t[:, g % 2, :], start=True, stop=True),
                             r=[wq_, kT], w=[pw[dc]])
                for dc in range(8):
                    if dc % 2:
                        P.op("act", lambda e, dc=dc, g4=g4: e.activation(Wk.t[:, dc, g4 * 512:(g4 + 1) * 512], pw[dc].t[:], AF.Identity), r=[pw[dc]], w=[Wk])
                    else:
                        P.op("dve", lambda e, dc=dc, g4=g4: e.tensor_copy(Wk.t[:, dc, g4 * 512:(g4 + 1) * 512], pw[dc].t[:]), r=[pw[dc]], w=[Wk])
        NB = 12
        SB = 8
        Dgs = [P.sb("g_Dg%d" % i, [128, SB, 128], BF16) for i in range(2)]
        gb = [P.sb("g_gb%d" % i, [128, 2 * D], BF16) for i in range(NB)]

        class RB:
            pass
        rbs = []
        for i in range(2):
            r_ = RB()
            r_.xt = P.sb("g_xt%d" % i, [128, 8, 128], F32)
            r_.hxt = P.sb("g_hxt%d" % i, [128, D], F32)
            r_.eidu = P.sb("g_eidu%d" % i, [128, 128], U32)
            r_.gate = P.sb("g_gate%d" % i, [128, 8, 16], F32)
            rbs.append(r_)
        sq = P.sb("g_sq", [128, 8, 128], BF16)
        xn = P.sb("g_xn", [128, 8, 128], F32)
        hx = P.sb("g_hx", [128, 8, 128], F32)
        rs = P.sb("g_rs", [128, 128], F32)
        rs2 = P.sb("g_rs2", [128, 128], F32)
        sq2 = P.sb("g_sq2", [128, 8, 128], BF16)
        junk = P.sb("g_junk", [128, D], BF16)
        sc = P.sb("g_sc", [128, 16, 128], F32)
        sc2 = P.sb("g_sc2", [128, 16, 128], F32)
        cand2 = sc2
        v16 = P.sb("g_v16", [128, 16, 16], F32)
        i16u = P.sb("g_i16u", [128, 16, 16], U32)
        if16 = P.sb("g_if16", [128, 16, 16], F32)
        cand = P.sb("g_cand", [128, 8, 256], F32)
        eq = P.sb("g_eq", [128, 8, 16, 16], F32)
        cs16 = P.sb("g_cs16", [128, 8, 16], F32)
        cp16 = P.sb("g_cp16", [128, 8, 16], U32)
        au = P.sb("g_au", [128, 8, 16], U32); bu = P.sb("g_bu", [128, 8, 16], U32)
        af = P.sb("g_af", [128, 8, 16], F32); bf = P.sb("g_bf", [128, 8, 16], F32)
        s1 = P.sb("g_s1", [128, 8, 16], F32); s2_ = P.sb("g_s2", [128, 8, 16], F32)
        gsum = P.sb("g_gsum", [128, 8], F32)
        dotsb = [P.sb("g_dots%d" % i, [128, 8], F32) for i in range(2)]
        wtsb = [P.sb("g_wts%d" % i, [128, 8], F32) for i in range(2)]
        acc = P.sb("g_acc", [128, D], F32)
        xo = P.sb("g_xo", [128, 8, 128], F32)
        yo = P.sb("g_yo", [128, 8, 128], F32) if final else xo
        pss = P.ps("g_pss", [128, 512], F32)
        pss2 = P.ps("g_pss2", [128, 512], F32)
        psc = [P.ps("g_psc%d" % i, [128, 512], F32) for i in range(2)]
        ptr = [P.ps("g_ptr%d" % i, [128, 512], F32) for i in range(2)]
        pva = [P.ps("g_pva%d" % i, [128, 512], F32) for i in range(2)]
        ntile = NO // 128
        t0 = 0 if need_ctx else 2
        tend = ntile if tile_limit is None else tile_limit
        out_tok = [P.token("xo%d" % i) for i in range(ntile)]
        G.out_tok = out_tok
        uv_tab = G.UVb.t
        gi = [0]

        def route(tt, rb):
            o0 = tt * 128
            j = 1 if o0 < 256 else 0
            xt, hxt, eidu, gate = rb.xt, rb.hxt, rb.eidu, rb.gate
            P.dma("sp", xt.t[:], x1v[:, :, o0:o0 + 128], r=G.x1_tok, w=[xt])
            P.op("act", lambda e: e.activation(sq.t[:], xt.t[:], AF.Square), r=[xt], w=[sq])
            yield
            for k in range(8):
                P.op("pe", lambda e, k=k: e.matmul(pss.t[:, 0:128], G.ones_bf.t[:, :], sq.t[:, k, :], start=(k == 0), stop=(k == 7)), r=[sq, G.ones_bf], w=[pss])
            P.op("act", lambda e: e.activation(rs.t[:], pss.t[:, 0:128], AF.Sqrt, bias=G.eps.t[:, 0:1], scale=1.0 / D), r=[pss, G.eps], w=[rs])
            P.op("dve", lambda e: e.reciprocal(rs.t[:], rs.t[:]), r=[rs], w=[rs])
            P.op("dve", lambda e: e.tensor_tensor(xn.t[:], xt.t[:], ins_bc(rs.t[:], 1, 8), ALU.mult), r=[xt, rs], w=[xn])
            yield
            for k in range(8):
                P.op("act", lambda e, k=k, j=j: e.activation(hx.t[:, k, :], xn.t[:, k, :], AF.Identity, scale=G.s2.t[:, k, j:j + 1], bias=G.mod.t[:, 24 + k, j:j + 1]),
                     r=[xn, G.s2, G.mod], w=[hx])
            yield
            for c4 in range(4):
                p_ = psc[c4 % 2]
                for k in range(8):
                    P.op("pe", lambda e, p_=p_, c4=c4, k=k: e.matmul(p_.t[:], hx.t[:, k, :], Wk.t[:, k, c4 * 512:(c4 + 1) * 512], start=(k == 0), stop=(k == 7)), r=[hx, Wk], w=[p_])
                P.op("act", lambda e, p_=p_, c4=c4: e.activation(sc.t[:, c4 * 4:(c4 + 1) * 4, :].rearrange("p a b -> p (a b)"), p_.t[:], AF.Identity), r=[p_], w=[sc])
                yield
            for h2 in range(2):
                for k4 in range(4):
                    k = h2 * 4 + k4
                    P.op("pe", lambda e, k=k, k4=k4, h2=h2: e.matmul(ptr[h2].t[:, k4 * 128:(k4 + 1) * 128], hx.t[:, k, :], idn.t[:], start=True, stop=True), r=[hx, idn], w=[ptr[h2]])
                P.op("act", lambda e, h2=h2: e.activation(hxt.t[:, h2 * 512:(h2 + 1) * 512], ptr[h2].t[:], AF.Identity), r=[ptr[h2]], w=[hxt])
                yield
            for g in range(16):
                P.op("dve", lambda e, g=g: e.max(v16.t[:, g, 0:8], sc.t[:, g, :]), r=[sc], w=[v16])
                P.op("dve", lambda e, g=g: e.max_index(i16u.t[:, g, 0:8], v16.t[:, g, 0:8], sc.t[:, g, :]), r=[sc, v16], w=[i16u])
                yield
                P.op("dve", lambda e, g=g: e.match_replace(sc2.t[:, g, :], v16.t[:, g, 0:8], sc.t[:, g, :], NEG), r=[sc, v16], w=[sc2])
                P.op("dve", lambda e, g=g: e.max(v16.t[:, g, 8:16], sc2.t[:, g, :]), r=[sc2], w=[v16])
                P.op("dve", lambda e, g=g: e.max_index(i16u.t[:, g, 8:16], v16.t[:, g, 8:16], sc2.t[:, g, :]), r=[sc2, v16], w=[i16u])
                yield
            P.op("dve", lambda e: e.tensor_copy(if16.t[:], i16u.t[:]), r=[i16u], w=[if16])
            vp = v16.t[:].ap[0][0]
            bcA = lambda t, off: mkap(t.t, off, [[vp, 128], [32, 8], [1, 16], [0, 16]])
            bcB = lambda t, off: mkap(t.t, off, [[vp, 128], [32, 8], [0, 16], [1, 16]])
            c4d = cand.t[:].rearrange("p h (a b) -> p h a b", b=16)
            P.op("dve", lambda e: e.tensor_tensor(c4d, bcA(v16, 0), bcB(v16, 16), ALU.add), r=[v16], w=[cand])
            yield
            c2v = lambda h: cand2.t[:].rearrange("p a b -> p (a b)")[:, h * 256:(h + 1) * 256]
            for h in range(8):
                P.op("dve", lambda e, h=h: e.max(cs16.t[:, h, 0:8], cand.t[:, h, :]), r=[cand], w=[cs16])
                P.op("dve", lambda e, h=h: e.max_index(cp16.t[:, h, 0:8], cs16.t[:, h, 0:8], cand.t[:, h, :]), r=[cand, cs16], w=[cp16])
                yield
                P.op("dve", lambda e, h=h: e.match_replace(c2v(h), cs16.t[:, h, 0:8], cand.t[:, h, :], NEG), r=[cand, cs16], w=[cand2])
                P.op("dve", lambda e, h=h: e.max(cs16.t[:, h, 8:16], c2v(h)), r=[cand2], w=[cs16])
                P.op("dve", lambda e, h=h: e.max_index(cp16.t[:, h, 8:16], cs16.t[:, h, 8:16], c2v(h)), r=[cand2, cs16], w=[cp16])
                yield
            P.op("dve", lambda e: e.tensor_single_scalar(au.t[:], cp16.t[:], 4, ALU.logical_shift_right), r=[cp16], w=[au])
            P.op("dve", lambda e: e.tensor_single_scalar(bu.t[:], cp16.t[:], 15, ALU.bitwise_and), r=[cp16], w=[bu])
            P.op("dve", lambda e: e.tensor_copy(af.t[:], au.t[:]), r=[au], w=[af])
            P.op("dve", lambda e: e.tensor_copy(bf.t[:], bu.t[:]), r=[bu], w=[bf])
            yield
            for (sel, posf, off) in ((s1, af, 0), (s2_, bf, 16)):
                pf = posf.t[:].ap[0][0]
                posB = mkap(posf.t, 0, [[pf, 128], [16, 8], [1, 16], [0, 16]])
                iotB = mkap(io16.t, 0, [[16, 128], [0, 8], [0, 16], [1, 16]])
                valB = mkap(if16.t, off, [[vp, 128], [32, 8], [0, 16], [1, 16]])
                P.op("dve", lambda e, posB=posB, iotB=iotB: e.tensor_tensor(eq.t[:], posB, iotB, ALU.is_equal), r=[posf, io16], w=[eq])
                P.op("dve", lambda e, valB=valB: e.tensor_tensor(eq.t[:], eq.t[:], valB, ALU.mult), r=[eq, if16], w=[eq])
                P.op("dve", lambda e, sel=sel: e.tensor_reduce(sel.t[:], eq.t[:], AX.X, ALU.add), r=[eq], w=[sel])
                yield
            P.op("dve", lambda e: e.scalar_tensor_tensor(s1.t[:], s1.t[:], 128.0, s2_.t[:], ALU.mult, ALU.add), r=[s1, s2_], w=[s1])
            P.op("dve", lambda e: e.tensor_copy(eidu.t[:], s1.t[:].rearrange("p a b -> p (a b)")), r=[s1], w=[eidu])
            cp_ = cs16.t[:].ap[0][0]
            mxB = mkap(cs16.t, 0, [[cp_, 128], [16, 8], [0, 16]])
            P.op("dve", lambda e, mxB=mxB: e.tensor_tensor(gate.t[:], cs16.t[:], mxB, ALU.subtract), r=[cs16], w=[gate])
            P.op("act", lambda e: e.activation(gate.t[:], gate.t[:], AF.Exp), r=[gate], w=[gate])
            P.op("dve", lambda e: e.tensor_reduce(gsum.t[:], gate.t[:], AX.X, ALU.add), r=[gate], w=[gsum])
            P.op("dve", lambda e: e.reciprocal(gsum.t[:], gsum.t[:]), r=[gsum], w=[gsum])
            P.op("dve", lambda e: e.tensor_tensor(gate.t[:], gate.t[:], ins_bc(gsum.t[:], 2, 16), ALU.mult), r=[gate, gsum], w=[gate])
            yield

        def step(g_):
            if g_ is not None:
                next(g_, None)

        def experts(tt, rb, nxt):
            o0 = tt * 128
            j = 1 if o0 < 256 else 0
            xt, hxt, eidu, gate = rb.xt, rb.hxt, rb.eidu, rb.gate
            gflat = gate.t[:].rearrange("p a b -> p (a b)")
            for blk in range(128 // SB):
                b0 = blk * SB
                gs = []
                dots = dotsb[blk % 2]; wts = wtsb[blk % 2]
                wp = wts.t[:].ap[0][0]
                for s in range(b0, b0 + SB):
                    g_ = gb[gi[0] % NB]; gi[0] += 1
                    gs.append(g_)
                    P.op("pool", lambda e, g_=g_, s=s: e.indirect_dma_start(out=g_.t[:], out_offset=None, in_=uv_tab,
                                                                            in_offset=bass.IndirectOffsetOnAxis(ap=eidu.t[:, s:s + 1], axis=0)), r=[eidu] + G.tab_tok, w=[g_], dma=True)
                    P.op("dve", lambda e, g_=g_, s=s: e.scalar_tensor_tensor(junk.t[:], g_.t[:, 0:D], 1.0, hxt.t[:], ALU.mult, ALU.mult, accum_out=dots.t[:, s - b0:s - b0 + 1]),
                         r=[g_, hxt], w=[junk, dots])
                    if s % 2 == 1:
                        step(nxt)
                Dg = Dgs[blk % 2]
                P.op("act", lambda e, dots=dots: e.activation(dots.t[:], dots.t[:], AF.Gelu_apprx_tanh), r=[dots], w=[dots])
                P.op("dve", lambda e, b0=b0, dots=dots, wts=wts: e.tensor_tensor(wts.t[:], dots.t[:], gflat[:, b0:b0 + SB], ALU.mult), r=[dots, gate], w=[wts])
                P.op("dve", lambda e, b0=b0, Dg=Dg: e.tensor_tensor(Dg.t[:], mkap(wts.t, 0, [[wp, 128], [1, SB], [0, 128]]), mkap(idn.t, 0, [[128, 128], [0, SB], [1, 128]]), ALU.mult),
                     r=[wts, idn], w=[Dg])
                for si, s in enumerate(range(b0, b0 + SB)):
                    for hh in range(2):
                        P.op("pe", lambda e, g_=gs[si], si=si, s=s, hh=hh, Dg=Dg: e.matmul(pva[hh].t[:], Dg.t[:, si, :], g_.t[:, D + hh * 512:D + (hh + 1) * 512], start=(s == 0), stop=(s == 127)),
                             r=[Dg, g_], w=[pva[hh]])
            for hh in range(2):
                P.op("act", lambda e, hh=hh: e.activation(acc.t[:, hh * 512:(hh + 1) * 512], pva[hh].t[:], AF.Identity), r=[pva[hh]], w=[acc])
            if getattr(G, "dbgo", None) is not None and tt == 2:
                dd = G.dbgo
                P.dma("sp", dd["d_acc"], acc.t[:], r=[acc])
            for h2 in range(2):
                for k4 in range(4):
                    k = h2 * 4 + k4
                    P.op("pe", lambda e, k=k, k4=k4, h2=h2: e.matmul(ptr[h2].t[:, k4 * 128:(k4 + 1) * 128], acc.t[:, k * 128:(k + 1) * 128], idn.t[:], start=True, stop=True), r=[acc, idn], w=[ptr[h2]])
                for k4 in range(4):
                    k = h2 * 4 + k4
                    P.op("dve", lambda e, k=k, k4=k4, h2=h2, j=j: e.scalar_tensor_tensor(xo.t[:, k, :], ptr[h2].t[:, k4 * 128:(k4 + 1) * 128], G.mod.t[:, 40 + k, j:j + 1], xt.t[:, k, :],
                                                                                   ALU.mult, ALU.add), r=[ptr[h2], G.mod, xt], w=[xo])
            src = xo
            if final:
                P.op("act", lambda e: e.activation(sq2.t[:], xo.t[:], AF.Square), r=[xo], w=[sq2])
                for k in range(8):
                    P.op("pe", lambda e, k=k: e.matmul(pss2.t[:, 0:128], G.ones_bf.t[:, :], sq2.t[:, k, :], start=(k == 0), stop=(k == 7)), r=[sq2, G.ones_bf], w=[pss2])
                P.op("act", lambda e: e.activation(rs2.t[:], pss2.t[:, 0:128], AF.Sqrt, bias=G.eps.t[:, 0:1], scale=1.0 / D), r=[pss2, G.eps], w=[rs2])
                P.op("dve", lambda e: e.reciprocal(rs2.t[:], rs2.t[:]), r=[rs2], w=[rs2])
                for k in range(8):
                    P.op("dve", lambda e, k=k: e.scalar_tensor_tensor(yo.t[:, k, :], xo.t[:, k, :], fg.t[:, k:k + 1], rs2.t[:], ALU.mult, ALU.mult), r=[xo, fg, rs2], w=[yo])
                src = yo
            if j:
                P.dma("sp", cov[:, :, o0:o0 + 128], src.t[:], r=[src], w=[out_tok[tt]])
            else:
                P.dma("sp", xov[:, :, o0 - 256:o0 - 256 + 128], src.t[:], r=[src], w=[out_tok[tt]])
            if nxt is not None:
                for _ in nxt:
                    pass

        g0 = route(t0, rbs[t0 % 2])
        for _ in g0:
            pass
        for tt in range(t0, tend):
            nxt = route(tt + 1, rbs[(tt + 1) % 2]) if tt + 1 < tend else None
            experts(tt, rbs[tt % 2], nxt)


def build_layer(P, G, io, l, need_ctx, final, xoT, coT, dbg=None):
    uF = P.dram("uF%d" % l, [NFULL * 128, NS], F32)
    uO = P.dram("uO%d" % l, [NOWNC * 128, NO], F32)
    ymix = dbg["ymix"] if dbg and "ymix" in dbg else P.dram("ymix%d" % l, [D, NO], BF16)
    x1T = dbg["x1T"] if dbg and "x1T" in dbg else P.dram("x1T%d" % l, [D, NO], F32)
    KS = P.dram("KS%d" % l, [128, 512, 2, 128], F32)
    KSW = P.dram("KSW%d" % l, [128, 512, 2, 128], F32)
    phase_mod(P, G, io, l)
    phase_B(P, G, io, l, need_ctx, uF, uO)
    phase_C(P, G, io, l, need_ctx, uF, uO, ymix)
    G.bg = []
    cg = cast_peer_tables(P, G, io, l)
    next(cg)
    phase_D(P, G, io, l, need_ctx, uF, uO, ymix)
    while G.bg:
        G.bg.pop(0)()
    for _ in cg:
        pass
    phase_E_filters(P, G, io, l, KS, KSW)
    phase_E_latent(P, G, io, l, uF, ymix, KS, KSW)
    if need_ctx:
        phase_E_ctx(P, G, io, l, uF, ymix)
    phase_F(P, G, io, l, need_ctx, ymix, x1T)
    phase_G(P, G, io, l, need_ctx, final, x1T, xoT, coT)


def src_from_inputs(io):
    xfv = io["xT_full"].rearrange("(k p) s -> p k s", p=128)
    xov = io["xT_own"].rearrange("(k p) s -> p k s", p=128)
    cv = io["cT"].rearrange("(k p) s -> p k s", p=128)
    return {"full": lambda t0, n: xfv[:, :, t0:t0 + n], "own": lambda t0, n: xov[:, :, t0:t0 + n], "ctx": lambda n: cv[:, :, 0:n], "toks": []}


def build_fused_program():
    nc = bass.Bass("TRN2", target_bir_lowering=False)
    io = declare_io(nc)
    P = Prog(nc)
    G = Ctx()
    xoT = Buf("xoT", nc.dram_tensor("xoT", [D, OWN], F32, kind="ExternalOutput").ap())
    setup_globals(P, G)
    fft_consts(P, G, io)
    xo0 = P.dram("xo0", [D, OWN], F32)
    co0 = P.dram("co0", [D, NCTX], F32)
    G.src = src_from_inputs(io)
    build_layer(P, G, io, 0, True, False, xo0, co0)
    toks0 = list(G.out_tok)
    xg = P.dram("xg", [4 * D, OWN], F32)
    for k in range(8):
        P.coll(lambda e, k=k: e.collective_compute("AllGather", ALU.bypass, replica_groups=[[0, 1, 2, 3], [4, 5, 6, 7]],
                                                   ins=[xo0.t[k * 128:(k + 1) * 128, :].opt()], outs=[xg.t[k * 512:(k + 1) * 512, :].opt()]), r=toks0, w=[xg])
    xgv = xg.t.rearrange("(k r p) s -> r p k s", r=4, p=128)
    xo0v = xo0.t.rearrange("(k p) s -> p k s", p=128)
    co0v = co0.t.rearrange("(k p) s -> p k s", p=128)
    G.src = {"full": lambda t0, n: xgv[t0 // OWN][:, :, (t0 % OWN):(t0 % OWN) + n], "own": lambda t0, n: xo0v[:, :, t0:t0 + n],
             "ctx": lambda n: co0v[:, :, 0:n], "toks": [xg] + toks0}
    build_layer(P, G, io, 1, False, True, xoT, None)
    P.finish(G.out_tok)
    return nc


def kernel(**inputs):
    inp = {k: np.asarray(v) for k, v in inputs.items()}
    S = prep_shared(inp)
    nc = build_fused_program()
    maps = []
    for c in range(8):
        m = dict(S)
        m.update(prep_core(inp, c))
        maps.append(m)
    res = run_bass_kernel_spmd(nc, maps, core_ids=list(range(8)))
    x = np.asarray(inp["x"], np.float32)
    out = np.empty_like(x)
    for c in range(8):
        b, j = c // 4, c % 4
        out[b, j * OWN:(j + 1) * OWN] = res.results[c]["xoT"].T
    return out.astype(np.float32)
```
